# Optimizing a Trainium2 kernel written in Bass

```python
import jax
import jax.numpy as jnp
from jax import lax
import numpy as np

D_MODEL = 1024
BATCH = 16
SEQ = 2048
DEPTH = 2

GRID_W = 64
CTX_LEN = 256
HEAD_DIM = 64
D_MIX = 1024
CONV_CH = 256
CONV_WIDTH = 31
NA_HEADS = 6
NA_WIN_H = 8
NA_WIN_W = 16
GQA_HEADS = 6
GQA_KV_HEADS = 2
ROPE_THETA = 10000.0
Q_BLOCK = 128
PEER_HEADS = 8
PEER_N_KEYS = 128
PEER_N_EXPERTS = PEER_N_KEYS * PEER_N_KEYS
PEER_D_KEY = 128
PEER_TOPK = 16
PEER_CHUNK = 128
EPS = 1e-6

NA_W = NA_HEADS * HEAD_DIM
GQA_W = GQA_HEADS * HEAD_DIM
GQA_KV_W = GQA_KV_HEADS * HEAD_DIM
OFF_A_VAL = 0
OFF_A_GATE = OFF_A_VAL + CONV_CH
OFF_NA_Q = OFF_A_GATE + CONV_CH
OFF_G_Q = OFF_NA_Q + NA_W
KV_START = OFF_G_Q + GQA_W
OFF_NA_K = KV_START
OFF_NA_V = OFF_NA_K + NA_W
OFF_G_K = OFF_NA_V + NA_W
OFF_G_V = OFF_G_K + GQA_KV_W
IN_COLS = OFF_G_V + GQA_KV_W

kernel_name = 'hymba_conformer_natten_gqa_peer_dit'


def _cols(z, off, width, base=0):
    return z[..., off - base: off - base + width]


def rms_norm(x, g):
    xf = x.astype(jnp.float32)
    y = xf * lax.rsqrt(jnp.mean(xf * xf, axis=-1, keepdims=True) + EPS)
    return (y * g.astype(jnp.float32)).astype(x.dtype)


def layer_norm(x, g, b):
    xf = x.astype(jnp.float32)
    mu = jnp.mean(xf, axis=-1, keepdims=True)
    var = jnp.mean(jnp.square(xf - mu), axis=-1, keepdims=True)
    y = (xf - mu) * lax.rsqrt(var + EPS)
    return (y * g.astype(jnp.float32) + b.astype(jnp.float32)).astype(x.dtype)


def modulate(h, shift, scale):
    return h * (1 + scale) + shift


def split_heads(z, n):
    B, L, _ = z.shape
    return z.reshape(B, L, n, HEAD_DIM).transpose(0, 2, 1, 3)


def merge_heads(o):
    B, H, L, d = o.shape
    return o.transpose(0, 2, 1, 3).reshape(B, L, H * d)


def axial_rope_tables(T, dtype):
    t = jnp.arange(T, dtype=jnp.int32)
    row = (t // GRID_W).astype(jnp.float32)
    col = (t % GRID_W).astype(jnp.float32)
    n_freq = HEAD_DIM // 4
    inv_freq = ROPE_THETA ** (-jnp.arange(n_freq, dtype=jnp.float32) / n_freq)
    ang = jnp.concatenate([row[:, None] * inv_freq, col[:, None] * inv_freq], axis=-1)
    return jnp.cos(ang).astype(dtype), jnp.sin(ang).astype(dtype)


def apply_rope(x, cos, sin):
    half = HEAD_DIM // 2
    x1, x2 = x[..., :half], x[..., half:]
    return jnp.concatenate([x1 * cos - x2 * sin, x1 * sin + x2 * cos], axis=-1)


def dense_attention(qh, kh, vh):
    B, Hq, L, d = qh.shape
    Hk = kh.shape[1]
    qg = qh.reshape(B, Hk, Hq // Hk, L, d) * (d ** -0.5)
    s = jnp.einsum('bkgqd,bksd->bkgqs', qg, kh).astype(jnp.float32)
    p = jax.nn.softmax(s, axis=-1).astype(vh.dtype)
    o = jnp.einsum('bkgqs,bksd->bkgqd', p, vh)
    return merge_heads(o.reshape(B, Hq, L, d))


def conformer_conv(val, gate, w_dw, b_dw, ln_g, ln_b):
    u = val * jax.nn.sigmoid(gate)
    pad = CONV_WIDTH // 2
    y = lax.conv_general_dilated(u, w_dw[:, None, :], window_strides=(1,), padding=[(pad, pad)],
                                 dimension_numbers=('NWC', 'WIO', 'NWC'),
                                 feature_group_count=CONV_CH) + b_dw
    return jax.nn.silu(layer_norm(y, ln_g, ln_b))


def neighbourhood_attention(q, k, v, kc, vc, rel_bias):
    B, T, _ = q.shape
    rows = T // GRID_W
    kh = min(NA_WIN_H, rows)
    n_loc = kh * NA_WIN_W

    def grid(z):
        return z.reshape(B, rows, GRID_W, NA_HEADS, HEAD_DIM).transpose(0, 3, 1, 2, 4)

    kg, vg = grid(k), grid(v)
    q_rows = grid(q * (HEAD_DIM ** -0.5)).transpose(2, 0, 1, 3, 4)
    kc_h, vc_h = split_heads(kc, NA_HEADS), split_heads(vc, NA_HEADS)
    cols = jnp.arange(GRID_W, dtype=jnp.int32)
    c0 = jnp.clip(cols - NA_WIN_W // 2, 0, GRID_W - NA_WIN_W)
    col_idx = c0[:, None] + jnp.arange(NA_WIN_W, dtype=jnp.int32)[None, :]
    col_bias_idx = col_idx - cols[:, None] + (NA_WIN_W - 1)

    def row_step(args):
        r, qr = args
        r0 = jnp.clip(r - kh // 2, 0, rows - kh)
        band_k = lax.dynamic_slice_in_dim(kg, r0, kh, axis=2)
        band_v = lax.dynamic_slice_in_dim(vg, r0, kh, axis=2)
        k_win = jnp.take(band_k, col_idx, axis=3)
        v_win = jnp.take(band_v, col_idx, axis=3)
        row_bias_idx = r0 + jnp.arange(kh, dtype=jnp.int32) - r + (NA_WIN_H - 1)
        bias = jnp.take(jnp.take(rel_bias, row_bias_idx, axis=1), col_bias_idx, axis=2)
        s_loc = jnp.einsum('bhqd,bhiqjd->bhqij', qr, k_win).astype(jnp.float32)
        s_loc = s_loc + bias.transpose(0, 2, 1, 3)[None].astype(jnp.float32)
        s_loc = s_loc.reshape(B, NA_HEADS, GRID_W, n_loc)
        s_ctx = jnp.einsum('bhqd,bhcd->bhqc', qr, kc_h).astype(jnp.float32)
        p = jax.nn.softmax(jnp.concatenate([s_loc, s_ctx], axis=-1), axis=-1).astype(qr.dtype)
        p_loc = p[..., :n_loc].reshape(B, NA_HEADS, GRID_W, kh, NA_WIN_W)
        p_ctx = p[..., n_loc:]
        return (jnp.einsum('bhqij,bhiqjd->bhqd', p_loc, v_win)
                + jnp.einsum('bhqc,bhcd->bhqd', p_ctx, vc_h))

    out = lax.map(row_step, (jnp.arange(rows, dtype=jnp.int32), q_rows))
    return out.transpose(1, 0, 3, 2, 4).reshape(B, T, NA_W)


def gqa_latent(q, k, v, kc_h, vc_h, q_g, k_g, cos, sin):
    B, T, _ = q.shape
    G = GQA_HEADS // GQA_KV_HEADS
    n_blk = T // Q_BLOCK
    qh = apply_rope(rms_norm(split_heads(q, GQA_HEADS), q_g), cos, sin) * (HEAD_DIM ** -0.5)
    kh = apply_rope(rms_norm(split_heads(k, GQA_KV_HEADS), k_g), cos, sin)
    k_all = jnp.concatenate([kh, kc_h], axis=2)
    v_all = jnp.concatenate([split_heads(v, GQA_KV_HEADS), vc_h], axis=2)
    qb = qh.reshape(B, GQA_KV_HEADS, G, n_blk, Q_BLOCK, HEAD_DIM).transpose(3, 0, 1, 2, 4, 5)

    def block_step(qq):
        s = jnp.einsum('bkgqd,bksd->bkgqs', qq, k_all).astype(jnp.float32)
        p = jax.nn.softmax(s, axis=-1).astype(v_all.dtype)
        return jnp.einsum('bkgqs,bksd->bkgqd', p, v_all)

    out = lax.map(block_step, qb)
    return out.transpose(1, 0, 4, 2, 3, 5).reshape(B, T, GQA_W)


def peer_ffn(h, w_q, sub_keys, u, v):
    B, L, D = h.shape
    chunks = h.reshape(-1, PEER_CHUNK, D)
    n_cand = PEER_TOPK * PEER_TOPK

    def chunk_step(xc):
        q = (xc @ w_q).reshape(PEER_CHUNK, PEER_HEADS, 2, PEER_D_KEY)
        s = jnp.einsum('chpk,hpnk->chpn', q, sub_keys).astype(jnp.float32)
        top_s, top_i = lax.top_k(s, PEER_TOPK)
        cand_s = top_s[:, :, 0, :, None] + top_s[:, :, 1, None, :]
        cand_i = top_i[:, :, 0, :, None] * PEER_N_KEYS + top_i[:, :, 1, None, :]
        best_s, best_pos = lax.top_k(cand_s.reshape(PEER_CHUNK, PEER_HEADS, n_cand), PEER_TOPK)
        idx = jnp.take_along_axis(cand_i.reshape(PEER_CHUNK, PEER_HEADS, n_cand), best_pos, axis=-1)
        g = jax.nn.softmax(best_s, axis=-1).astype(xc.dtype)
        act = jax.nn.gelu(jnp.einsum('cd,chkd->chk', xc, jnp.take(u, idx, axis=0)))
        return jnp.einsum('chk,chkd->cd', g * act, jnp.take(v, idx, axis=0))

    return lax.map(chunk_step, chunks).reshape(B, L, D)


def setup_inputs(seed: int = 0) -> dict:
    key = jax.random.key(seed)
    ks = jax.random.split(key, 24)
    f32 = jnp.float32

    def nrm(k, shape, s):
        return jax.random.normal(k, shape, f32) * s

    L, D = DEPTH, D_MODEL
    return {
        'x': nrm(ks[0], (BATCH, SEQ, D), 1.0),
        'c': nrm(ks[1], (BATCH, D), 1.0),
        'ctx': nrm(ks[2], (BATCH, CTX_LEN, D), 1.0),
        'c_ctx': nrm(ks[3], (D,), 1.0),
        'norm1_g': 1.0 + nrm(ks[4], (L, D), 0.05),
        'norm2_g': 1.0 + nrm(ks[5], (L, D), 0.05),
        'w_ada': nrm(ks[6], (L, D, 6 * D), 0.5 * D ** -0.5),
        'b_ada': nrm(ks[7], (L, 6 * D), 0.02),
        'w_in': nrm(ks[8], (L, D, IN_COLS), D ** -0.5),
        'conv_w': nrm(ks[9], (L, CONV_WIDTH, CONV_CH), CONV_WIDTH ** -0.5),
        'conv_b': nrm(ks[10], (L, CONV_CH), 0.02),
        'conv_ln_g': 1.0 + nrm(ks[11], (L, CONV_CH), 0.05),
        'conv_ln_b': nrm(ks[12], (L, CONV_CH), 0.02),
        'na_rel_bias': nrm(ks[13], (L, NA_HEADS, 2 * NA_WIN_H - 1, 2 * NA_WIN_W - 1), 0.1),
        'gqa_q_norm': 1.0 + nrm(ks[14], (L, HEAD_DIM), 0.05),
        'gqa_k_norm': 1.0 + nrm(ks[15], (L, HEAD_DIM), 0.05),
        'w_out': nrm(ks[16], (L, D_MIX, D), D_MIX ** -0.5),
        'peer_w_q': nrm(ks[17], (L, D, PEER_HEADS * 2 * PEER_D_KEY), D ** -0.5),
        'peer_keys': nrm(ks[18], (L, PEER_HEADS, 2, PEER_N_KEYS, PEER_D_KEY), PEER_D_KEY ** -0.5),
        'peer_u': nrm(ks[19], (L, PEER_N_EXPERTS, D), D ** -0.5),
        'peer_v': nrm(ks[20], (L, PEER_N_EXPERTS, D), 0.5),
        'final_norm_g': 1.0 + nrm(ks[21], (D,), 0.05),
    }


def reference(x, c, ctx, c_ctx, norm1_g, norm2_g, w_ada, b_ada, w_in, conv_w, conv_b, conv_ln_g,
              conv_ln_b, na_rel_bias, gqa_q_norm, gqa_k_norm, w_out, peer_w_q, peer_keys, peer_u,
              peer_v, final_norm_g):
    T = x.shape[1]
    cos, sin = axial_rope_tables(T, x.dtype)
    silu_c = jax.nn.silu(c)
    silu_cc = jax.nn.silu(c_ctx)
    for l in range(DEPTH):
        last = l == DEPTH - 1
        mod = (silu_c @ w_ada[l] + b_ada[l])[:, None, :]
        sh1, sc1, g1, sh2, sc2, g2 = jnp.split(mod, 6, axis=-1)
        mod_c = silu_cc @ w_ada[l] + b_ada[l]
        csh1, csc1, cg1, csh2, csc2, cg2 = jnp.split(mod_c, 6, axis=-1)

        h = modulate(rms_norm(x, norm1_g[l]), sh1, sc1)
        hc = modulate(rms_norm(ctx, norm1_g[l]), csh1, csc1)
        z = h @ w_in[l]
        zc_kv = hc @ w_in[l][:, KV_START:]
        kc_na = _cols(zc_kv, OFF_NA_K, NA_W, KV_START)
        vc_na = _cols(zc_kv, OFF_NA_V, NA_W, KV_START)
        kc_g = rms_norm(split_heads(_cols(zc_kv, OFF_G_K, GQA_KV_W, KV_START), GQA_KV_HEADS), gqa_k_norm[l])
        vc_g = split_heads(_cols(zc_kv, OFF_G_V, GQA_KV_W, KV_START), GQA_KV_HEADS)

        a = conformer_conv(_cols(z, OFF_A_VAL, CONV_CH), _cols(z, OFF_A_GATE, CONV_CH),
                           conv_w[l], conv_b[l], conv_ln_g[l], conv_ln_b[l])
        b = neighbourhood_attention(_cols(z, OFF_NA_Q, NA_W), _cols(z, OFF_NA_K, NA_W),
                                    _cols(z, OFF_NA_V, NA_W), kc_na, vc_na, na_rel_bias[l])
        g = gqa_latent(_cols(z, OFF_G_Q, GQA_W), _cols(z, OFF_G_K, GQA_KV_W), _cols(z, OFF_G_V, GQA_KV_W),
                       kc_g, vc_g, gqa_q_norm[l], gqa_k_norm[l], cos, sin)
        x = x + g1 * (jnp.concatenate([a, b, g], axis=-1) @ w_out[l])

        x = x + g2 * peer_ffn(modulate(rms_norm(x, norm2_g[l]), sh2, sc2),
                              peer_w_q[l], peer_keys[l], peer_u[l], peer_v[l])

        if not last:
            zc_q = hc @ w_in[l][:, :KV_START]
            ac = conformer_conv(_cols(zc_q, OFF_A_VAL, CONV_CH), _cols(zc_q, OFF_A_GATE, CONV_CH),
                                conv_w[l], conv_b[l], conv_ln_g[l], conv_ln_b[l])
            bc = dense_attention(split_heads(_cols(zc_q, OFF_NA_Q, NA_W), NA_HEADS),
                                 split_heads(kc_na, NA_HEADS), split_heads(vc_na, NA_HEADS))
            qc_g = rms_norm(split_heads(_cols(zc_q, OFF_G_Q, GQA_W), GQA_HEADS), gqa_q_norm[l])
            gc = dense_attention(qc_g, kc_g, vc_g)
            ctx = ctx + cg1 * (jnp.concatenate([ac, bc, gc], axis=-1) @ w_out[l])
            ctx = ctx + cg2 * peer_ffn(modulate(rms_norm(ctx, norm2_g[l]), csh2, csc2),
                                       peer_w_q[l], peer_keys[l], peer_u[l], peer_v[l])
    return rms_norm(x, final_norm_g)
```

```python
import numpy as np
import concourse.bass as bass
import concourse.mybir as mybir
from concourse.bass_utils import run_bass_kernel_spmd
from contextlib import ExitStack

F32 = mybir.dt.float32
BF16 = mybir.dt.bfloat16
U32 = mybir.dt.uint32
I32 = mybir.dt.int32
AF = mybir.ActivationFunctionType
ALU = mybir.AluOpType
AX = mybir.AxisListType


ALLTOK = []


class Tok:
    __slots__ = ("name", "writers", "readers", "base")

    def __init__(self, name):
        self.name = name
        self.writers = []
        self.readers = []
        self.base = []
        ALLTOK.append(self)


class Op:
    __slots__ = ("eng", "fn", "deps", "dma", "signal", "count", "sem", "val",
                 "prev_val", "idx", "barrier")

    def __init__(self, eng, fn, dma):
        self.eng = eng
        self.fn = fn
        self.dma = dma
        self.deps = []
        self.signal = False
        self.count = 0
        self.sem = None
        self.val = 0
        self.prev_val = 0
        self.barrier = False


ENGS = ("pe", "act", "dve", "pool", "sp")
NSLOT = 12


class Prog:
    def __init__(self, nc):
        self.nc = nc
        self.ops = []
        self.last = {e: None for e in ENGS}
        self.dma_outstanding = []

    def tok(self, name="t"):
        return Tok(name)

    def toks(self, name, n):
        return [Tok("%s%d" % (name, i)) for i in range(n)]

    def op(self, eng, fn, reads=(), writes=(), pw=(), dma=False):
        o = Op(eng, fn, dma)
        deps = []
        for t in reads:
            deps.extend(t.writers)
        for t in writes:
            deps.extend(t.writers)
            deps.extend(t.readers)
        for t in pw:
            if t.readers:
                t.base = list(t.readers) + list(t.writers)
                t.writers = []
                t.readers = []
            deps.extend(t.base)
        for t in reads:
            t.readers.append(o)
        for t in writes:
            t.writers = [o]
            t.readers = []
            t.base = [o]
        for t in pw:
            t.writers.append(o)
        o.idx = len(self.ops)
        seen = set()
        best = {}
        for d in deps:
            if d is o or id(d) in seen:
                continue
            seen.add(id(d))
            if d.dma:
                o.deps.append(d)
            else:
                b = best.get(d.eng)
                if b is None or d.idx > b.idx:
                    best[d.eng] = d
        o.deps.extend(best.values())
        self.ops.append(o)
        self.last[eng] = o
        return o

    def barrier(self):
        b = Op(None, None, False)
        b.barrier = True
        b.deps = [o for o in self.ops if False]
        b.idx = len(self.ops)
        self.ops.append(b)
        for t in ALLTOK:
            t.writers = []
            t.readers = []
            t.base = []

    def emit(self):
        nc = self.nc
        ops = self.ops
        last = {e: None for e in ENGS}
        dmas_since = []
        pending = {e: [] for e in ENGS}
        for o in ops:
            if o.barrier:
                extra = [x for x in last.values() if x is not None] + dmas_since
                for e in ENGS:
                    pending[e] = list(extra)
                dmas_since = []
                continue
            if pending[o.eng]:
                seen = set(id(d) for d in o.deps)
                for d in pending[o.eng]:
                    if id(d) not in seen and d is not o:
                        o.deps.append(d)
                pending[o.eng] = []
            last[o.eng] = o
            if o.dma:
                dmas_since.append(o)
        real = [o for o in ops if not o.barrier]
        for o in real:
            for d in o.deps:
                if not d.dma:
                    if d.eng == "pe" and o.eng == "pe" and not o.dma:
                        continue
                    d.signal = True
        cnt = {e: 0 for e in ENGS}
        for o in real:
            if o.dma:
                continue
            if o.signal:
                cnt[o.eng] += 1
            o.count = cnt[o.eng]
        stack = ExitStack()
        sems = {e: stack.enter_context(nc.semaphore("s_" + e)) for e in ENGS}
        dsem = {}
        dval = {}
        dn = {e: 0 for e in ENGS}
        for o in real:
            if not o.dma:
                continue
            q = o.eng
            j = dn[q] % NSLOT
            dn[q] += 1
            key = (q, j)
            if key not in dsem:
                dsem[key] = stack.enter_context(nc.semaphore("d_%s%d" % (q, j)))
                dval[key] = 0
            o.sem = dsem[key]
            o.prev_val = dval[key]
            dval[key] += 16
            o.val = dval[key]
        per_eng = {e: [o for o in real if o.eng == e] for e in ENGS}

        def run(e, eng):
            waited = {}

            def wait(sem, v):
                k = id(sem)
                if waited.get(k, 0) >= v:
                    return
                waited[k] = v
                eng.wait_ge(sem, v)

            for o in per_eng[e]:
                for d in o.deps:
                    if d.dma:
                        wait(d.sem, d.val)
                    else:
                        if d.eng == "pe" and e == "pe" and not o.dma:
                            continue
                        wait(sems[d.eng], d.count)
                if o.dma:
                    if o.prev_val:
                        wait(o.sem, o.prev_val)
                    ins = o.fn(eng)
                    ins.then_inc(o.sem, 16)
                else:
                    ins = o.fn(eng)
                    if o.signal:
                        ins.then_inc(sems[e], 1)
            if e == "sp":
                for key, s in dsem.items():
                    wait(s, dval[key])
                for e2 in ENGS:
                    if e2 != "sp" and cnt[e2]:
                        wait(sems[e2], cnt[e2])

        with nc.Block() as block:
            @block.tensor
            def _(eng):
                run("pe", eng)

            @block.scalar
            def _(eng):
                run("act", eng)

            @block.vector
            def _(eng):
                run("dve", eng)

            @block.gpsimd
            def _(eng):
                run("pool", eng)

            @block.sync
            def _(eng):
                run("sp", eng)
        stack.close()


DEPTH = 2
NB = 2
TL = 2048
TC = 256
TB = TL + TC
NT = NB * TB
NTILE = NT // 128
GT = 256
NGB = TB // GT
EPS = 1e-6
ULAT0 = 15
UCTX0 = 15 + TL + 15 + 15
UW = UCTX0 + TC + 15
PG = 384
NPG = NT // PG
NEG = -1.0e30


def na_tiles(j):
    res = []
    for m in range(16):
        pat = []
        anyv = False
        for a in range(2):
            row = []
            for b in range(2):
                r = 2 * j + b
                r0 = min(max(r - 4, 0), 24)
                kr = 2 * m + a
                v = (r0 <= kr <= r0 + 7)
                row.append(v)
                anyv = anyv or v
            pat.append(tuple(row))
        if anyv:
            res.append((m, tuple(pat)))
    return res


def na_combos():
    combos = []
    for j in range(16):
        for m, pat in na_tiles(j):
            key = (m - j, pat)
            if key not in combos:
                combos.append(key)
    return combos


class K:
    def __init__(self, nc, dbg=()):
        self.nc = nc
        self.P = Prog(nc)
        self.dbg = dbg
        self.D = {}
        self.nm = 0

    def din(self, name, shape, dt=F32):
        self.D[name] = self.nc.dram_tensor(name, list(shape), dt, kind="ExternalInput").ap()

    def sb(self, st, shape, dt, name=None):
        self.nm += 1
        return st.enter_context(self.nc.sbuf_tensor("%s_%d" % (name or "sb", self.nm), list(shape), dt))

    def ps(self, st, shape, dt, name=None):
        self.nm += 1
        return st.enter_context(self.nc.psum_tensor("%s_%d" % (name or "ps", self.nm), list(shape), dt))

    def tok(self, n="t"):
        return Tok(n)

    def mm(self, out, lhsT, rhs, start, stop, reads, w=(), pw=(), sgc=False):
        self.P.op("pe", lambda e: e.matmul(out, lhsT=lhsT, rhs=rhs, start=start, stop=stop, skip_group_check=sgc),
                  reads=reads, writes=w, pw=pw)

    def tr(self, out, in_, ident, reads, w=(), pw=()):
        self.P.op("pe", lambda e: e.transpose(out, in_, ident), reads=reads, writes=w, pw=pw)

    def act(self, out, in_, func, reads, w=(), pw=(), scale=None, bias=None):
        kw = {}
        if scale is not None:
            kw["scale"] = scale
        if bias is not None:
            kw["bias"] = bias
        self.P.op("act", lambda e: e.activation(out=out, in_=in_, func=func, **kw),
                  reads=reads, writes=w, pw=pw)

    def tt(self, eng, out, in0, in1, op, reads, w=(), pw=()):
        self.P.op(eng, lambda e: e.tensor_tensor(out=out, in0=in0, in1=in1, op=op),
                  reads=reads, writes=w, pw=pw)

    def ts(self, eng, out, in0, s1, op0, reads, w=(), pw=(), s2=None, op1=None):
        if op1 is None:
            self.P.op(eng, lambda e: e.tensor_scalar(out=out, in0=in0, scalar1=s1, scalar2=None, op0=op0),
                      reads=reads, writes=w, pw=pw)
        else:
            self.P.op(eng, lambda e: e.tensor_scalar(out=out, in0=in0, scalar1=s1, scalar2=s2, op0=op0, op1=op1),
                      reads=reads, writes=w, pw=pw)

    def stt(self, out, in0, scalar, in1, op0, op1, reads, w=(), pw=()):
        self.P.op("dve", lambda e: e.scalar_tensor_tensor(out=out, in0=in0, scalar=scalar, in1=in1, op0=op0, op1=op1),
                  reads=reads, writes=w, pw=pw)

    def cp(self, eng, out, in_, reads, w=(), pw=()):
        if eng == "act":
            self.P.op("act", lambda e: e.copy(out=out, in_=in_), reads=reads, writes=w, pw=pw)
        else:
            self.P.op(eng, lambda e: e.tensor_copy(out=out, in_=in_), reads=reads, writes=w, pw=pw)

    def recip(self, out, in_, reads, w=(), pw=()):
        self.P.op("dve", lambda e: e.reciprocal(out=out, in_=in_), reads=reads, writes=w, pw=pw)

    def memset(self, eng, ap, val, w=(), pw=(), reads=()):
        self.P.op(eng, lambda e: e.memset(ap, val), reads=reads, writes=w, pw=pw)

    def dma(self, q, out, in_, reads, w=(), pw=()):
        self.P.op(q, lambda e: e.dma_start(out=out, in_=in_), reads=reads, writes=w, pw=pw, dma=True)

    def declare(self):
        din = self.din
        din("x", [NB, TL, 1024]); din("ctx", [NB, TC, 1024]); din("ccT", [128, 8, 3])
        din("w_ada", [DEPTH, 1024, 6144]); din("b_adaT", [DEPTH, 128, 48])
        din("n1gT", [DEPTH, 128, 8]); din("n2gT", [DEPTH, 128, 8]); din("fngT", [128, 8])
        din("w_fm", [DEPTH, 1024, 2304]); din("w_v", [DEPTH, 1024, 512])
        din("gq2", [DEPTH, 128, 2]); din("gk2", [DEPTH, 128, 2])
        din("conv_wT", [DEPTH, 128, 2, 31]); din("conv_bT", [DEPTH, 128, 2])
        din("conv_lngT", [DEPTH, 128, 2]); din("conv_lnbT", [DEPTH, 128, 2])
        din("rb_exp", [DEPTH, 6, 15, 64, 64])
        din("w_out", [DEPTH, 1024, 1024]); din("w_q", [DEPTH, 1024, 2048])
        din("keysT", [DEPTH, 128, 16, 128]); din("uT", [DEPTH, 1024, 16384]); din("pv", [DEPTH, 16384, 1024])
        din("ropeC", [128, TB]); din("ropeS", [128, TB]); din("colmask2", [128, 128])
        din("blockmask", [128, 8]); din("blockones", [128, 128])
        nc = self.nc
        self.out = nc.dram_tensor("out", [NB, TL, 1024], F32, kind="ExternalOutput").ap()
        if self.dbg:
            self.xT_d = nc.dram_tensor("xT_d", [8, 128, NT], F32, kind="ExternalOutput").ap()
        else:
            self.xT_d = nc.dram_tensor("xT_d", [8, 128, NT], F32).ap()
        self.xc_d = nc.dram_tensor("xc_d", [8, 128, NT], BF16).ap()
        self.G_d = nc.dram_tensor("G_d", [16384, NT // 2], BF16).ap()
        self.ub_d = nc.dram_tensor("ub_d", [1024, 16384], BF16).ap()
        self.vb_d = nc.dram_tensor("vb_d", [16384, 1024], BF16).ap()
        self.xT_tok = [Tok("xT%d" % i) for i in range(NTILE)]
        self.xc_tok = [Tok("xc%d" % i) for i in range(NTILE)]
        self.G_tok = [Tok("G%d" % i) for i in range(NTILE)]
        self.dbg_out = {}

    def dbg_tensor(self, name, shape, dt=F32):
        ap = self.nc.dram_tensor(name, list(shape), dt, kind="ExternalOutput").ap()
        self.dbg_out[name] = ap
        return ap

    def consts(self, st):
        D = self.D
        c = {}
        self.c = c
        c["ident_f"] = self.sb(st, [128, 128], F32, "identf")
        c["ident_b"] = self.sb(st, [128, 128], BF16, "identb")
        c["ones_b"] = self.sb(st, [128, 128], BF16, "onesb")
        c["bones_b"] = self.sb(st, [128, 128], BF16, "bonesb")
        c["iota_f"] = self.sb(st, [128, 128], F32, "iotaf")
        c["iota_b"] = self.sb(st, [128, 128], BF16, "iotab")
        c["bmask_b"] = self.sb(st, [128, 8], BF16, "bmaskb")
        c["cmask_b"] = self.sb(st, [128, 128], BF16, "cmaskb")
        c["ropeC"] = self.sb(st, [128, TB], BF16, "ropeC")
        c["ropeS"] = self.sb(st, [128, TB], BF16, "ropeS")
        c["siluT"] = self.sb(st, [128, 8, 3], F32, "siluT")
        c["fng"] = self.sb(st, [128, 8], F32, "fng")
        c["modT"] = self.sb(st, [128, 48, 3], F32, "modT")
        c["A1"] = self.sb(st, [128, 8, 3], F32, "A1")
        c["A2"] = self.sb(st, [128, 8, 3], F32, "A2")
        c["eps"] = self.sb(st, [128, 1], F32, "eps")
        tmpf = self.sb(st, [128, 128], F32, "ctmp")
        tmp8 = self.sb(st, [128, 8], F32, "ctmp8")
        T = self.T = {k: Tok(k) for k in ["const", "modT", "A", "ctmp", "ctmp8", "silu"]}
        ct = [T["const"]]
        self.memset("pool", c["ident_f"][:], 0.0, w=[T["ctmp"]])
        self.P.op("pool", lambda e: e.affine_select(out=c["ident_f"][:], in_=c["ident_f"][:], pattern=[[-1, 128]],
                                                    compare_op=ALU.not_equal, fill=1.0, base=0, channel_multiplier=1),
                  reads=[T["ctmp"]], writes=[T["ctmp"]])
        self.cp("dve", c["ident_b"][:], c["ident_f"][:], reads=[T["ctmp"]], pw=ct)
        self.memset("dve", c["ones_b"][:], 1.0, pw=ct)
        self.memset("dve", c["eps"][:], EPS, pw=ct)
        tio = Tok("iota")
        self.P.op("pool", lambda e: e.iota(c["iota_f"][:], pattern=[[1, 128]], base=0, channel_multiplier=0,
                                           allow_small_or_imprecise_dtypes=True), writes=[tio])
        self.cp("dve", c["iota_b"][:], c["iota_f"][:], reads=[tio], pw=ct)
        self.dma("sp", tmpf[:], D["blockones"][:, :], reads=[], w=[T["ctmp8"]])
        self.cp("dve", c["bones_b"][:], tmpf[:], reads=[T["ctmp8"]], pw=ct)
        tmpf2 = self.sb(st, [128, 128], F32, "ctmp2")
        t2 = Tok("ctmp2")
        self.dma("sp", tmpf2[:], D["colmask2"][:, :], reads=[], w=[t2])
        self.cp("dve", c["cmask_b"][:], tmpf2[:], reads=[t2], pw=ct)
        t3 = Tok("ctmp3")
        self.dma("sp", tmp8[:], D["blockmask"][:, :], reads=[], w=[t3])
        self.cp("dve", c["bmask_b"][:], tmp8[:], reads=[t3], pw=ct)
        with ExitStack() as st2:
            rst = [self.sb(st2, [128, TB], F32, "ropest") for _ in range(2)]
            trst = [Tok("ropest") for _ in range(2)]
            for i, nm in enumerate(("ropeC", "ropeS")):
                self.dma("sp", rst[i][:], D[nm][:, :], reads=[], w=[trst[i]])
                self.cp("dve", c[nm][:], rst[i][:], reads=[trst[i]], pw=ct)
            self.P.barrier()
        self.dma("sp", c["fng"][:], D["fngT"][:, :], reads=[], pw=ct)
        t4 = Tok("cc")
        cct = self.sb(st, [128, 8, 3], F32, "cct")
        self.dma("sp", cct[:], D["ccT"][:, :, :], reads=[], w=[t4])
        self.act(c["siluT"][:], cct[:], AF.Silu, reads=[t4], w=[T["silu"]])

    def stage_s0(self):
        D, c, T = self.D, self.c, self.T
        with ExitStack() as st:
            xin = [self.sb(st, [128, 1024], F32, "xin") for _ in range(2)]
            xo = [self.sb(st, [128, 8, 128], F32, "xo") for _ in range(2)]
            pp = [self.ps(st, [128, 4, 128], F32, "s0p") for _ in range(4)]
            txin = [Tok("xin") for _ in range(2)]
            txo = [Tok("xo") for _ in range(2)]
            tpp = [Tok("pp") for _ in range(4)]
            n = 0
            for b in range(NB):
                for ti in range(TB // 128):
                    s = n % 2
                    if ti < 16:
                        src = D["x"][b, ti * 128:(ti + 1) * 128, :]
                    else:
                        src = D["ctx"][b, (ti - 16) * 128:(ti - 15) * 128, :]
                    self.dma("sp", xin[s][:], src, reads=[], w=[txin[s]])
                    for hf in range(2):
                        pi = (n * 2 + hf) % 4
                        for q in range(4):
                            k = hf * 4 + q
                            self.tr(pp[pi][:, q, :], xin[s][:, k * 128:(k + 1) * 128], c["ident_f"][:],
                                    reads=[txin[s], T["const"], T["ctmp"]],
                                    w=[tpp[pi]] if q == 0 else (), pw=() if q == 0 else [tpp[pi]])
                        self.cp("act" if hf == 0 else "dve", xo[s][:, hf * 4:(hf + 1) * 4, :], pp[pi][:],
                                reads=[tpp[pi]], w=([txo[s]] if hf == 0 else []) + [tpp[pi]], pw=() if hf == 0 else [txo[s]])
                    tile = b * (TB // 128) + ti
                    self.dma("pool", self.xT_d.rearrange("k p t -> p k t")[:, :, tile * 128:(tile + 1) * 128], xo[s][:],
                             reads=[txo[s]], w=[self.xT_tok[tile]])
                    n += 1
        self.P.barrier()

    def stage_s1(self, l):
        D, c, T = self.D, self.c, self.T
        with ExitStack() as st:
            wst = [self.sb(st, [128, 8, 512], F32, "wada") for _ in range(2)]
            twst = [Tok("wada") for _ in range(2)]
            bad = self.sb(st, [128, 48], F32, "bada")
            n1g = self.sb(st, [128, 8], F32, "n1g")
            n2g = self.sb(st, [128, 8], F32, "n2g")
            tsm = Tok("s1small")
            tmpa = self.sb(st, [128, 8, 3], F32, "tmpa")
            ttmp = Tok("tmpa")
            modp = self.ps(st, [128, 128, 4], F32, "modp")
            tmodp = Tok("modp")
            self.dma("pool", bad[:], D["b_adaT"][l], reads=[], pw=[tsm])
            self.dma("pool", n1g[:], D["n1gT"][l], reads=[], pw=[tsm])
            self.dma("pool", n2g[:], D["n2gT"][l], reads=[], pw=[tsm])
            wv = D["w_ada"][l].rearrange("(k p) c -> p k c", p=128)
            for jc in range(12):
                s = jc % 2
                self.dma("sp", wst[s][:], wv[:, :, jc * 512:(jc + 1) * 512], reads=[], w=[twst[s]])
                for q in range(4):
                    j = jc * 4 + q
                    for k in range(8):
                        self.mm(modp[:, j, 0:3], wst[s][:, k, q * 128:(q + 1) * 128], c["siluT"][:, k, :],
                                start=(k == 0), stop=(k == 7), reads=[twst[s], T["silu"]],
                                w=[tmodp] if (j == 0 and k == 0) else (), pw=() if (j == 0 and k == 0) else [tmodp])
            self.tt("dve", c["modT"][:], modp[:, 0:48, 0:3], bad[:].unsqueeze(2).to_broadcast([128, 48, 3]), ALU.add,
                    reads=[tmodp, tsm], w=[T["modT"], tmodp])
            for (dst, nrm, base) in ((c["A1"], n1g, 8), (c["A2"], n2g, 32)):
                self.ts("dve", tmpa[:], c["modT"][:, base:base + 8, :], 1.0, ALU.add, reads=[T["modT"]], w=[ttmp])
                self.tt("dve", dst[:], tmpa[:], nrm[:].unsqueeze(2).to_broadcast([128, 8, 3]), ALU.mult,
                        reads=[ttmp, tsm], pw=[T["A"]])
        self.P.barrier()

    def norm_mod(self, st_bufs, xg, txg, hT, thT, A, sh_base, r, n, pfx):
        c = self.c
        B = st_bufs
        self.act(B["sq"][:, :, 0:n], xg, AF.Square, reads=[txg], w=[B["tsq"]])
        for k in range(8):
            self.mm(B["ssp"][:, 0:n], c["ones_b"][:], B["sq"][:, k, 0:n], start=(k == 0), stop=(k == 7),
                    reads=[B["tsq"]], w=[B["tssp"]] if k == 0 else (), pw=() if k == 0 else [B["tssp"]])
        self.act(B["sd"][:, 0:n], B["ssp"][:, 0:n], AF.Sqrt, reads=[B["tssp"]], w=[B["tsd"], B["tssp"]],
                 scale=1.0 / 1024.0, bias=c["eps"][:])
        self.recip(B["rstd"][:, 0:n], B["sd"][:, 0:n], reads=[B["tsd"]], w=[B["trstd"]])
        self.tt("dve", B["tmp"][:, :, 0:n], xg, B["rstd"][:, 0:n].unsqueeze(1).to_broadcast([128, 8, n]), ALU.mult,
                reads=[txg, B["trstd"]], w=[B["ttmp"]])
        for k in range(8):
            self.act(hT[:, k, :], B["tmp"][:, k, 0:n], AF.Identity, reads=[B["ttmp"]],
                     w=[thT] if k == 0 else (), pw=() if k == 0 else [thT],
                     scale=A[:, k, r:r + 1], bias=c["modT"][:, sh_base + k, r:r + 1])

    def norm_bufs(self, st, n):
        B = {}
        B["sq"] = self.sb(st, [128, 8, n], BF16, "sq")
        B["ssp"] = self.ps(st, [128, 512], F32, "ssp")
        B["sd"] = self.sb(st, [128, n], F32, "sd")
        B["rstd"] = self.sb(st, [128, n], F32, "rstd")
        B["tmp"] = self.sb(st, [128, 8, n], F32, "ntmp")
        for k in ["tsq", "tssp", "tsd", "trstd", "ttmp"]:
            B[k] = Tok(k)
        return B

    def load_cast(self, st, dst, src_view, ncols, piece, engs=("act", "pool", "dve"), stg=None):
        if stg is None:
            stg = ([self.sb(st, [128, 8, piece], F32, "wstg") for _ in range(2)], [Tok("wstg") for _ in range(2)])
        stg, tst = stg
        tdst = Tok("wdst")
        npc = ncols // piece
        for i in range(npc):
            s = i % 2
            self.dma("sp", stg[s][:], src_view[:, :, i * piece:(i + 1) * piece], reads=[], w=[tst[s]])
            self.cp(engs[i % len(engs)], dst[:, :, i * piece:(i + 1) * piece], stg[s][:], reads=[tst[s]], pw=[tdst])
        return tdst

    def stage_s2(self, l, b, st_b, BB):
        D, c = self.D, self.c
        with ExitStack() as st:
            wfm = self.sb(st, [128, 8, 2304], BF16, "wfm")
            wv = self.sb(st, [128, 8, 512], BF16, "wv")
            stg = ([self.sb(st, [128, 8, 128], F32, "wstg") for _ in range(2)], [Tok("wstg") for _ in range(2)])
            twfm = self.load_cast(st, wfm, D["w_fm"][l].rearrange("(k p) c -> p k c", p=128), 2304, 128, stg=stg)
            twv = self.load_cast(st, wv, D["w_v"][l].rearrange("(k p) c -> p k c", p=128), 512, 128, stg=stg)
            gq2 = self.sb(st, [128, 2], F32, "gq2")
            gk2 = self.sb(st, [128, 2], F32, "gk2")
            gq2s = self.sb(st, [128, 2], F32, "gq2s")
            tg = Tok("g2")
            tgs = Tok("g2s")
            self.dma("pool", gq2[:], D["gq2"][l], reads=[], pw=[tg])
            self.dma("pool", gk2[:], D["gk2"][l], reads=[], pw=[tg])
            self.ts("dve", gq2s[:], gq2[:], 0.125, ALU.mult, reads=[tg], w=[tgs])
            NBUF = self.norm_bufs(st, GT)
            xg1 = self.sb(st, [128, 8, GT], F32, "xg")
            xg = [xg1, xg1]
            txg1 = Tok("xg")
            txg = [txg1, txg1]
            hT = [self.sb(st, [128, 8, GT], BF16, "hT") for _ in range(2)]
            thT = [Tok("hT") for _ in range(2)]
            zp = [self.ps(st, [128, 2, GT], F32, "zp") for _ in range(4)]
            tzp = [Tok("zp") for _ in range(4)]
            vtp = [self.ps(st, [128, 512], F32, "vtp") for _ in range(2)]
            tvtp = [Tok("vtp") for _ in range(2)]
            ssz = self.ps(st, [128, 2, GT], F32, "ssz")
            tssz = Tok("ssz")
            sg = [self.sb(st, [128, GT], F32, "sg") for _ in range(2)]
            tsg = [Tok("sg") for _ in range(2)]
            sqz = [self.sb(st, [128, GT], BF16, "sqz") for _ in range(2)]
            tsqz = [Tok("sqz") for _ in range(2)]
            sdz = [self.sb(st, [128, GT], F32, "sdz") for _ in range(2)]
            tsdz = [Tok("sdz") for _ in range(2)]
            rz = [self.sb(st, [128, GT], F32, "rz") for _ in range(2)]
            trz = [Tok("rz") for _ in range(2)]
            t1 = [self.sb(st, [128, GT], F32, "t1") for _ in range(2)]
            tt1 = [Tok("t1") for _ in range(2)]
            t2 = [self.sb(st, [128, GT], F32, "t2") for _ in range(2)]
            tt2 = [Tok("t2") for _ in range(2)]
            xTv = self.xT_d.rearrange("k p t -> p k t")
            zc = [0]

            def zpair(ciA, ciB, s, thTs):
                i = zc[0] % 4
                zc[0] += 1
                tk = tzp[i]
                for h2, ci in enumerate((ciA, ciB)):
                    for k in range(8):
                        first = (h2 == 0 and k == 0)
                        self.mm(zp[i][:, h2, :], wfm[:, k, ci * 128:(ci + 1) * 128], hT[s][:, k, :], start=(k == 0), stop=(k == 7),
                                reads=[twfm, thTs], w=[tk] if first else (), pw=() if first else [tk])
                return zp[i][:, 0, :], zp[i][:, 1, :], tk

            nrope = [0]
            for g in range(NGB):
                s = g % 2
                t0 = b * TB + g * GT
                r = b if g < 8 else 2
                pos0 = g * GT
                self.dma("sp", xg[s][:], xTv[:, :, t0:t0 + GT], reads=[], w=[txg[s]])
                self.norm_mod(NBUF, xg[s][:], txg[s], hT[s], thT[s], c["A1"], 0, r, GT, "s2")
                ucol0 = (ULAT0 + pos0) if g < 8 else UCTX0
                for cc in range(2):
                    zv, zg, tk = zpair(cc, 2 + cc, s, thT[s])
                    self.act(sg[cc][:], zg, AF.Sigmoid, reads=[tk], w=[tsg[cc], tk])
                    self.tt("dve", BB["ubuf"][:, cc, ucol0:ucol0 + GT], zv, sg[cc][:], ALU.mult,
                            reads=[tk, tsg[cc]], w=[tk], pw=[BB["tubuf"]])
                for cc in range(3):
                    zq, zk, tk = zpair(4 + cc, 10 + cc, s, thT[s])
                    self.act(BB["qna"][:, cc, pos0:pos0 + GT], zq, AF.Identity, reads=[tk], w=[tk], pw=[BB["tqna"]], scale=0.125)
                    self.cp("dve", BB["knaA"][0:64, cc, pos0:pos0 + GT], zk[0:64, :], reads=[tk], w=[tk], pw=[BB["tkna"]])
                    self.cp("dve", BB["knaB"][64:128, cc, pos0:pos0 + GT], zk[64:128, :], reads=[tk], w=[tk], pw=[BB["tkna"]])
                for cc in range(4):
                    if cc < 3:
                        ci, cis, gg = 7 + cc, 14 + cc, gq2s
                        dst, tdst = BB["qg"][:, cc, pos0:pos0 + GT], BB["tqg"]
                    else:
                        ci, cis, gg = 13, 17, gk2
                        dst, tdst = None, BB["tkg"]
                    i = nrope[0] % 2
                    nrope[0] += 1
                    z, zs, tk = zpair(ci, cis, s, thT[s])
                    self.act(sqz[i][:], z, AF.Square, reads=[tk], w=[tsqz[i], tk])
                    self.mm(ssz[:, i, :], c["bones_b"][:], sqz[i][:], start=True, stop=True, reads=[tsqz[i]], w=[tssz])
                    self.act(sdz[i][:], ssz[:, i, :], AF.Sqrt, reads=[tssz], w=[tsdz[i], tssz], scale=1.0 / 64.0, bias=c["eps"][:])
                    self.recip(rz[i][:], sdz[i][:], reads=[tsdz[i]], w=[trz[i]])
                    self.stt(t1[i][:], z, gg[:, 0:1], c["ropeC"][:, pos0:pos0 + GT], ALU.mult, ALU.mult,
                             reads=[tk, tg, tgs], w=[tt1[i], tk])
                    self.stt(t2[i][:], zs, gg[:, 1:2], c["ropeS"][:, pos0:pos0 + GT], ALU.mult, ALU.mult,
                             reads=[tk, tg, tgs], w=[tt2[i], tk])
                    self.tt("pool", t1[i][:], t1[i][:], t2[i][:], ALU.add, reads=[tt1[i], tt2[i]], w=[tt1[i]])
                    if dst is not None:
                        self.tt("dve", dst, t1[i][:], rz[i][:], ALU.mult, reads=[tt1[i], trz[i]], pw=[tdst])
                    else:
                        self.tt("dve", BB["kgA"][0:64, pos0:pos0 + GT], t1[i][0:64, :], rz[i][0:64, :], ALU.mult,
                                reads=[tt1[i], trz[i]], pw=[tdst])
                        self.tt("dve", BB["kgB"][64:128, pos0:pos0 + GT], t1[i][64:128, :], rz[i][64:128, :], ALU.mult,
                                reads=[tt1[i], trz[i]], pw=[tdst])
                for tt_ in range(2):
                    vi = (g * 2 + tt_) % 2
                    for k in range(8):
                        self.mm(vtp[vi][:], hT[s][:, k, tt_ * 128:(tt_ + 1) * 128], wv[:, k, :], start=(k == 0), stop=(k == 7),
                                reads=[thT[s], twv], w=[tvtp[vi]] if k == 0 else (), pw=() if k == 0 else [tvtp[vi]])
                    self.cp("act", BB["vv"][:, g * 2 + tt_, :, 0:64], vtp[vi][:].rearrange("p (h d) -> p h d", d=64),
                            reads=[tvtp[vi]], w=[tvtp[vi]], pw=[BB["tvv"]])
        self.P.barrier()

    def stage_s3(self, l, b, BB, do_ctx):
        D, c = self.D, self.c
        mixT, tmix = BB["mixT"], BB["tmix"]
        with ExitStack() as st:
            cw = self.sb(st, [128, 2, 31], F32, "cw")
            cb = self.sb(st, [128, 2], F32, "cb")
            lng = self.sb(st, [128, 2], F32, "lng")
            lnb = self.sb(st, [128, 2], F32, "lnb")
            tcs = Tok("convsmall")
            self.dma("pool", cw[:], D["conv_wT"][l], reads=[], pw=[tcs])
            self.dma("pool", cb[:], D["conv_bT"][l], reads=[], pw=[tcs])
            self.dma("pool", lng[:], D["conv_lngT"][l], reads=[], pw=[tcs])
            self.dma("pool", lnb[:], D["conv_lnbT"][l], reads=[], pw=[tcs])
            dg = self.sb(st, [128, 2, 31, 128], BF16, "dg")
            tdg = Tok("dg")
            n = 0
            for cc in range(2):
                for k in range(31):
                    self.ts("dve" if n % 2 == 0 else "pool", dg[:, cc, k, :], c["ident_b"][:], cw[:, cc, k:k + 1], ALU.mult,
                            reads=[tcs], pw=[tdg])
                    n += 1
            yp = [self.ps(st, [128, 2, GT], F32, "yp") for _ in range(2)]
            typ = [Tok("yp") for _ in range(2)]
            lsp = self.ps(st, [128, 2, GT], F32, "lsp")
            tlsp = Tok("lsp")
            ycb = [self.sb(st, [128, 2, GT], F32, "ycb") for _ in range(2)]
            tycb = [Tok("ycb") for _ in range(2)]
            ysq = [self.sb(st, [128, 2, GT], BF16, "ysq") for _ in range(2)]
            tysq = [Tok("ysq") for _ in range(2)]
            yb = [self.sb(st, [128, 2, GT], BF16, "yb") for _ in range(2)]
            tyb = [Tok("yb") for _ in range(2)]
            mu = self.sb(st, [128, GT], F32, "mu")
            musq = self.sb(st, [128, GT], F32, "musq")
            var = self.sb(st, [128, GT], F32, "var")
            rs = self.sb(st, [128, GT], F32, "rs")
            tmu, tmusq, tvar, trs = Tok("mu"), Tok("musq"), Tok("var"), Tok("rs")
            ct = [self.sb(st, [128, GT], F32, "ct") for _ in range(2)]
            tct = [Tok("ct") for _ in range(2)]
            import os
            parts = os.environ.get("S3PARTS", "cma")
            ngr = NGB if do_ctx else 8
            if "c" not in parts:
                ngr = 0
            for g in range(ngr):
                s = g % 2
                pos0 = g * GT
                ub0 = (pos0 if g < 8 else (UCTX0 - 15))
                for cc in range(2):
                    for k in range(31):
                        self.mm(yp[s][:, cc, :], dg[:, cc, k, :], BB["ubuf"][:, cc, ub0 + k:ub0 + k + GT],
                                start=(k == 0), stop=(k == 30), reads=[tdg, BB["tubuf"]],
                                w=[typ[s]] if (cc == 0 and k == 0) else (), pw=() if (cc == 0 and k == 0) else [typ[s]])
                for cc in range(2):
                    self.act(ycb[s][:, cc, :], yp[s][:, cc, :], AF.Identity, reads=[typ[s], tcs],
                             w=([tycb[s]] if cc == 0 else []) + [typ[s]], pw=() if cc == 0 else [tycb[s]], bias=cb[:, cc:cc + 1])
                    self.act(ysq[s][:, cc, :], yp[s][:, cc, :], AF.Square, reads=[typ[s], tcs],
                             w=([tysq[s]] if cc == 0 else []) + [typ[s]], pw=() if cc == 0 else [tysq[s]], bias=cb[:, cc:cc + 1])
                self.cp("pool", yb[s][:], ycb[s][:], reads=[tycb[s]], w=[tyb[s]])
                for cc in range(2):
                    self.mm(lsp[:, 0, :], c["ones_b"][:], yb[s][:, cc, :], start=(cc == 0), stop=(cc == 1),
                            reads=[tyb[s]], w=[tlsp] if cc == 0 else (), pw=() if cc == 0 else [tlsp])
                for cc in range(2):
                    self.mm(lsp[:, 1, :], c["ones_b"][:], ysq[s][:, cc, :], start=(cc == 0), stop=(cc == 1),
                            reads=[tysq[s]], pw=[tlsp])
                self.act(mu[:], lsp[:, 0, :], AF.Identity, reads=[tlsp], w=[tmu, tlsp], scale=1.0 / 256.0)
                self.tt("dve", musq[:], mu[:], mu[:], ALU.mult, reads=[tmu], w=[tmusq])
                self.stt(var[:], lsp[:, 1, :], 1.0 / 256.0, musq[:], ALU.mult, ALU.subtract, reads=[tlsp, tmusq], w=[tvar, tlsp])
                self.act(var[:], var[:], AF.Sqrt, reads=[tvar], w=[tvar], bias=c["eps"][:])
                self.recip(rs[:], var[:], reads=[tvar], w=[trs])
                for cc in range(2):
                    self.tt("dve", ct[cc][:], ycb[s][:, cc, :], mu[:], ALU.subtract, reads=[tycb[s], tmu], w=[tct[cc]])
                    self.tt("pool", ct[cc][:], ct[cc][:], rs[:], ALU.mult, reads=[tct[cc], trs], w=[tct[cc]])
                    self.act(mixT[:, cc, pos0:pos0 + GT], ct[cc][:], AF.Silu, reads=[tct[cc], tcs], pw=[tmix],
                             scale=lng[:, cc:cc + 1], bias=lnb[:, cc:cc + 1])

            combos = na_combos()
            NCB = len(combos)
            MT = self.sb(st, [128, NCB, 6, 128], BF16, "MT")
            tMT = Tok("MT")
            mst = [self.sb(st, [128, 6, 128], F32, "mst") for _ in range(2)]
            tmst = [Tok("mst") for _ in range(2)]
            for ci, (d, pat) in enumerate(combos):
                s = ci % 2
                self.memset("pool", mst[s][:], -30000.0, w=[tmst[s]])
                for a in range(2):
                    for bq in range(2):
                        if not pat[a][bq]:
                            continue
                        delta = 2 * d + a - bq
                        src = D["rb_exp"][l, :, delta + 7, :, :].rearrange("h c q -> c h q")
                        self.dma("sp", mst[s][a * 64:(a + 1) * 64, :, bq * 64:(bq + 1) * 64], src, reads=[], pw=[tmst[s]])
                self.act(MT[:, ci, :, :], mst[s][:], AF.Exp, reads=[tmst[s]], pw=[tMT])
            self.tt("dve", MT[:].rearrange("p c h q -> p (c h) q"), MT[:].rearrange("p c h q -> p (c h) q"),
                    c["cmask_b"][:].unsqueeze(1).to_broadcast([128, NCB * 6, 128]), ALU.mult, reads=[tMT], w=[tMT])

            sp_ = [self.ps(st, [128, 4, 128], F32, "sT") for _ in range(2)]
            tsp = [Tok("sT") for _ in range(2)]
            op_ = [self.ps(st, [128, 4, 128], F32, "oacc") for _ in range(2)]
            top_ = [Tok("oacc") for _ in range(2)]
            trp = self.ps(st, [128, 8, 128], BF16, "trp")
            ttrp = Tok("trp")
            pT = [self.sb(st, [128, 3, 128], BF16, "pT") for _ in range(3)]
            tpT = [Tok("pT") for _ in range(3)]
            rc = [self.sb(st, [128, 3], F32, "rc") for _ in range(2)]
            trc = [Tok("rc") for _ in range(2)]
            otm = [self.sb(st, [128, 768], BF16, "otm") for _ in range(2)]
            totm = [Tok("otm") for _ in range(2)]
            cnt = {"u": 0, "h": 0}

            att = os.environ.get("ATT", "qpnt")

            def attn_half(qt, heads, keylist, ot, tot_, ocol0):
                hi = cnt["h"] % 2
                cnt["h"] += 1
                nk = len(keylist)

                def qk(ki):
                    kaps, vaps, mask = keylist[ki]
                    u = cnt["u"]
                    cnt["u"] += 1
                    si, pi = u % 2, u % 3
                    for h3 in range(3):
                        self.mm(sp_[si][:, h3, :], kaps[h3], heads[h3], start=True, stop=True,
                                reads=[BB["tqna"], BB["tkna"], BB["tqg"], BB["tkg"]],
                                w=[tsp[si]] if h3 == 0 else (), pw=() if h3 == 0 else [tsp[si]])
                    self.act(pT[pi][:], sp_[si][:, 0:3, :], AF.Exp, reads=[tsp[si]], w=[tpT[pi], tsp[si]])
                    if mask is not None:
                        self.tt("dve", pT[pi][:], pT[pi][:], mask, ALU.mult, reads=[tpT[pi], tMT], w=[tpT[pi]])
                    return pi

                pend = qk(0)
                for ki in range(nk):
                    nxt = qk(ki + 1) if ki + 1 < nk else None
                    pi = pend
                    vaps = keylist[ki][1]
                    for h3 in range(3 if "p" in att else 0):
                        first = (ki == 0 and h3 == 0)
                        self.mm(op_[hi][:, h3, 0:65], pT[pi][:, h3, :], vaps[h3], start=first, stop=(ki == nk - 1), sgc=True,
                                reads=[tpT[pi], BB["tvv"]], w=[top_[hi]] if first else (), pw=() if first else [top_[hi]])
                    pend = nxt
                if "n" not in att:
                    return
                self.recip(rc[hi][:], op_[hi][:, 0:3, 64], reads=[top_[hi]], w=[trc[hi], top_[hi]])
                self.tt("dve", ot[:, ocol0:ocol0 + 192].rearrange("p (h d) -> p h d", d=64), op_[hi][:, 0:3, 0:64],
                        rc[hi][:].unsqueeze(2).to_broadcast([128, 3, 64]), ALU.mult, reads=[top_[hi], trc[hi]], w=[top_[hi]], pw=[tot_])

            vv, qna, qg = BB["vv"], BB["qna"], BB["qg"]
            knaAB = [BB["knaA"], BB["knaB"]]
            kgAB = [BB["kgA"], BB["kgB"]]
            nq = 18 if do_ctx else 16
            if "a" not in parts:
                nq = 0
            if "1" in parts:
                nq = 1
            for qt in range(nq):
                oi = qt % 2
                q0 = qt * 128
                if qt < 16:
                    loc = na_tiles(qt)
                    na_keys = [(m, combos.index((m - qt, pat))) for (m, pat) in loc] + [(16, None), (17, None)]
                    g_keys = list(range(18))
                else:
                    na_keys = [(16, None), (17, None)]
                    g_keys = [16, 17]
                for half in range(2):
                    hs = [3 * half + i for i in range(3)]
                    heads = [qna[:, h // 2, q0:q0 + 128] for h in hs]
                    kl = []
                    for (m, cidx) in na_keys:
                        kaps = [knaAB[h % 2][:, h // 2, m * 128:(m + 1) * 128] for h in hs]
                        vaps = [vv[:, m, h, :] for h in hs]
                        mask = None if cidx is None else MT[:, cidx, 3 * half:3 * half + 3, :]
                        kl.append((kaps, vaps, mask))
                    attn_half(qt, heads, kl, otm[oi], totm[oi], half * 192)
                for kvh in range(2):
                    heads = [qg[:, cc, q0:q0 + 128] for cc in range(3)]
                    kl = []
                    for m in g_keys:
                        kaps = [kgAB[kvh][:, m * 128:(m + 1) * 128]] * 3
                        vaps = [vv[:, m, 6 + kvh, :]] * 3
                        kl.append((kaps, vaps, None))
                    attn_half(qt, heads, kl, otm[oi], totm[oi], 384 + kvh * 192)
                if "t" not in att:
                    continue
                for j in range(6):
                    self.tr(trp[:, j, :], otm[oi][:, j * 128:(j + 1) * 128], c["ident_b"][:], reads=[totm[oi]],
                            w=[ttrp] if j == 0 else (), pw=() if j == 0 else [ttrp])
                self.cp("act", mixT[:, 2:8, q0:q0 + 128], trp[:, 0:6, :], reads=[ttrp], w=[ttrp], pw=[tmix])
        self.P.barrier()

    def stage_s4(self, l, b, BB, do_ctx):
        D, c = self.D, self.c
        with ExitStack() as st:
            wo = self.sb(st, [128, 8, 1024], BF16, "wo")
            two = self.load_cast(st, wo, D["w_out"][l].rearrange("(k p) c -> p k c", p=128), 1024, 256)
            xg = [self.sb(st, [128, 8, GT], F32, "xg4") for _ in range(2)]
            txg = [Tok("xg4") for _ in range(2)]
            opp = [self.ps(st, [128, 512], F32, "opp") for _ in range(4)]
            topp = [Tok("opp") for _ in range(4)]
            xTv = self.xT_d.rearrange("k p t -> p k t")
            ngr = NGB if do_ctx else 8
            n = 0
            for g in range(ngr):
                s = g % 2
                t0 = b * TB + g * GT
                pos0 = g * GT
                r = b if g < 8 else 2
                self.dma("sp", xg[s][:], xTv[:, :, t0:t0 + GT], reads=[], w=[txg[s]])
                for kd in range(8):
                    i = n % 4
                    n += 1
                    ap, tk = opp[i][:, 0:GT], topp[i]
                    for kf in range(8):
                        self.mm(ap, wo[:, kf, kd * 128:(kd + 1) * 128], BB["mixT"][:, kf, pos0:pos0 + GT],
                                start=(kf == 0), stop=(kf == 7), reads=[two, BB["tmix"]],
                                w=[tk] if kf == 0 else (), pw=() if kf == 0 else [tk])
                    self.stt(xg[s][:, kd, :], ap, c["modT"][:, 16 + kd, r:r + 1], xg[s][:, kd, :], ALU.mult, ALU.add,
                             reads=[tk, txg[s]], w=[tk], pw=[txg[s]])
                self.dma("pool", xTv[:, :, t0:t0 + GT], xg[s][:], reads=[txg[s]], w=[])
        self.P.barrier()

    def stage_p1(self, l, ntiles=NTILE, tile0=0, gbase=0):
        D, c = self.D, self.c
        with ExitStack() as st:
            wq = self.sb(st, [128, 8, 2048], BF16, "wq")
            twq = self.load_cast(st, wq, D["w_q"][l].rearrange("(k p) c -> p k c", p=128), 2048, 256)
            keysT = self.sb(st, [128, 16, 128], F32, "keysT")
            tkeys = Tok("keysT")
            self.dma("pool", keysT[:], D["keysT"][l], reads=[], w=[tkeys])
            NBUF = self.norm_bufs(st, 128)
            xt = [self.sb(st, [128, 8, 128], F32, "xt") for _ in range(2)]
            txt = [Tok("xt") for _ in range(2)]
            xc = [self.sb(st, [128, 8, 128], BF16, "xc") for _ in range(2)]
            txc = [Tok("xc") for _ in range(2)]
            qT = self.sb(st, [128, 16, 128], F32, "qT")
            tqT = Tok("qT")
            s_sb = self.sb(st, [128, 16, 128], F32, "s_sb")
            ts_sb = Tok("s_sb")
            tmpk = [self.sb(st, [128, 128], F32, "tmpk") for _ in range(2)]
            ttmpk = [Tok("tmpk") for _ in range(2)]
            top = self.sb(st, [128, 16, 16], F32, "top")
            ttop = Tok("top")
            idx = self.sb(st, [128, 16, 16], U32, "idx")
            tidx = Tok("idx")
            idxf = self.sb(st, [128, 2, 8, 16], F32, "idxf")
            tidxf = Tok("idxf")
            cs = self.sb(st, [128, 8, 256], F32, "cs")
            tcs = Tok("cs")
            tmp2 = [self.sb(st, [128, 256], F32, "tmp2") for _ in range(2)]
            ttmp2 = [Tok("tmp2") for _ in range(2)]
            m8 = self.sb(st, [128, 8, 16], F32, "m8")
            tm8 = Tok("m8")
            ee = self.sb(st, [128, 8, 256], F32, "ee")
            tee = Tok("ee")
            msk = self.sb(st, [128, 8, 256], F32, "msk")
            tmsk = Tok("msk")
            zz = self.sb(st, [128, 8], F32, "zz")
            tzz = Tok("zz")
            idxT = self.sb(st, [128, 2, 128], BF16, "idxT")
            tidxT = Tok("idxT")
            Tw = self.sb(st, [128, 16, 128], BF16, "Tw")
            tTw = Tok("Tw")
            G_t = self.sb(st, [128, 128, 128], BF16, "G_t")
            tG_t = Tok("G_t")
            A4 = [self.sb(st, [128, 4, 128], BF16, "A4") for _ in range(2)]
            tA4 = [Tok("A4") for _ in range(2)]
            B4 = [self.sb(st, [128, 4, 128], BF16, "B4") for _ in range(2)]
            tB4 = [Tok("B4") for _ in range(2)]
            W4 = [self.sb(st, [128, 4, 8, 16], BF16, "W4") for _ in range(2)]
            tW4 = [Tok("W4") for _ in range(2)]
            R4 = [self.sb(st, [128, 4, 128], BF16, "R4") for _ in range(2)]
            tR4 = [Tok("R4") for _ in range(2)]
            qsp = [self.ps(st, [128, 4, 128], F32, "qsp") for _ in range(2)]
            tqsp = [Tok("qsp") for _ in range(2)]
            trq, ttrq = qsp, tqsp
            Rp = [self.ps(st, [128, 4, 128], F32, "Rp") for _ in range(2)]
            tRp = [Tok("Rp") for _ in range(2)]
            Gp = [self.ps(st, [128, 4, 128], F32, "Gp") for _ in range(2)]
            tGp = [Tok("Gp") for _ in range(2)]
            xTv = self.xT_d.rearrange("k p t -> p k t")
            xcv = self.xc_d.rearrange("k p t -> p k t")
            Gv = self.G_d.rearrange("(a i) t -> i a t", i=128)
            nq = 0
            for ti in range(tile0, tile0 + ntiles):
                s = ti % 2
                t0 = ti * 128
                bb = ti // (TB // 128)
                r = bb if (ti % (TB // 128)) < 16 else 2
                self.dma("sp", xt[s][:], xTv[:, :, t0:t0 + 128], reads=[], w=[txt[s]])
                self.norm_mod(NBUF, xt[s][:], txt[s], xc[s], txc[s], c["A2"], 24, r, 128, "p1")
                self.dma("pool", xcv[:, :, t0:t0 + 128], xc[s][:], reads=[txc[s]], w=[])
                for bg in range(4):
                    i = nq % 2
                    nq += 1
                    for bl in range(4):
                        blk = bg * 4 + bl
                        for k in range(8):
                            first = (bl == 0 and k == 0)
                            self.mm(qsp[i][:, bl, :], wq[:, k, blk * 128:(blk + 1) * 128], xc[s][:, k, :],
                                    start=(k == 0), stop=(k == 7), reads=[twq, txc[s]],
                                    w=[tqsp[i]] if first else (), pw=() if first else [tqsp[i]])
                    self.cp("act" if bg % 2 == 0 else "dve", qT[:, bg * 4:(bg + 1) * 4, :], qsp[i][:], reads=[tqsp[i]],
                            w=([tqT] if bg == 0 else []) + [tqsp[i]], pw=() if bg == 0 else [tqT])
                for bg in range(4):
                    i = nq % 2
                    nq += 1
                    for bl in range(4):
                        blk = bg * 4 + bl
                        self.mm(qsp[i][:, bl, :], qT[:, blk, :], keysT[:, blk, :], start=True, stop=True,
                                reads=[tqT, tkeys], w=[tqsp[i]] if bl == 0 else (), pw=() if bl == 0 else [tqsp[i]])
                    self.cp("act" if bg % 2 == 0 else "dve", s_sb[:, bg * 4:(bg + 1) * 4, :], qsp[i][:], reads=[tqsp[i]],
                            w=([ts_sb] if bg == 0 else []) + [tqsp[i]], pw=() if bg == 0 else [ts_sb])
                for blk in range(16):
                    j = blk % 2
                    first = blk == 0
                    wtop = dict(w=[ttop]) if first else dict(pw=[ttop])
                    widx = dict(w=[tidx]) if first else dict(pw=[tidx])
                    self.P.op("dve", (lambda e, blk=blk: e.max(out=top[:, blk, 0:8], in_=s_sb[:, blk, :])),
                              reads=[ts_sb], writes=wtop.get("w", ()), pw=wtop.get("pw", ()))
                    self.P.op("dve", (lambda e, blk=blk: e.max_index(out=idx[:, blk, 0:8], in_max=top[:, blk, 0:8],
                                                                    in_values=s_sb[:, blk, :])),
                              reads=[ts_sb, ttop], writes=widx.get("w", ()), pw=widx.get("pw", ()))
                    self.P.op("dve", (lambda e, blk=blk, j=j: e.match_replace(out=tmpk[j][:], in_to_replace=top[:, blk, 0:8],
                                                                            in_values=s_sb[:, blk, :], imm_value=NEG)),
                              reads=[ts_sb, ttop], writes=[ttmpk[j]])
                    self.P.op("dve", (lambda e, blk=blk, j=j: e.max(out=top[:, blk, 8:16], in_=tmpk[j][:])),
                              reads=[ttmpk[j]], pw=[ttop])
                    self.P.op("dve", (lambda e, blk=blk, j=j: e.max_index(out=idx[:, blk, 8:16], in_max=top[:, blk, 8:16],
                                                                         in_values=tmpk[j][:])),
                              reads=[ttmpk[j], ttop], pw=[tidx])
                self.cp("dve", idxf[:].rearrange("p q h i -> p h q i"), idx[:].rearrange("p (h q) i -> p h q i", q=2),
                        reads=[tidx], w=[tidxf])
                top4 = top[:].rearrange("p (h q) i -> p h q i", q=2)
                self.tt("dve", cs[:].rearrange("p h (i j) -> p h i j", j=16),
                        top4[:, :, 0, :].unsqueeze(3).to_broadcast([128, 8, 16, 16]),
                        top4[:, :, 1, :].unsqueeze(2).to_broadcast([128, 8, 16, 16]), ALU.add, reads=[ttop], w=[tcs])
                for h in range(8):
                    j = h % 2
                    self.P.op("dve", (lambda e, h=h: e.max(out=m8[:, h, 0:8], in_=cs[:, h, :])), reads=[tcs],
                              writes=[tm8] if h == 0 else (), pw=() if h == 0 else [tm8])
                    self.P.op("dve", (lambda e, h=h, j=j: e.match_replace(out=tmp2[j][:], in_to_replace=m8[:, h, 0:8],
                                                                        in_values=cs[:, h, :], imm_value=NEG)),
                              reads=[tcs, tm8], writes=[ttmp2[j]])
                    self.P.op("dve", (lambda e, h=h, j=j: e.max(out=m8[:, h, 8:16], in_=tmp2[j][:])), reads=[ttmp2[j]], pw=[tm8])
                self.tt("dve", ee[:], cs[:], m8[:, :, 0:1].to_broadcast([128, 8, 256]), ALU.subtract, reads=[tcs, tm8], w=[tee])
                self.act(ee[:], ee[:], AF.Exp, reads=[tee], w=[tee])
                self.tt("dve", msk[:], cs[:], m8[:, :, 15:16].to_broadcast([128, 8, 256]), ALU.is_ge, reads=[tcs, tm8], w=[tmsk])
                self.tt("dve", ee[:], ee[:], msk[:], ALU.mult, reads=[tee, tmsk], w=[tee])
                self.P.op("dve", lambda e: e.tensor_reduce(out=zz[:], in_=ee[:], axis=AX.X, op=ALU.add), reads=[tee], writes=[tzz])
                self.recip(zz[:], zz[:], reads=[tzz], w=[tzz])
                Wc2 = msk[:].rearrange("p a b -> p (a b)").rearrange("p (i h j) -> p i h j", i=16, h=8)
                self.tt("dve", Wc2.rearrange("p i h j -> p h i j"), ee[:].rearrange("p h (i j) -> p h i j", j=16),
                        zz[:].unsqueeze(2).unsqueeze(3).to_broadcast([128, 8, 16, 16]), ALU.mult, reads=[tee, tzz], w=[tmsk])
                for q2 in range(2):
                    self.tr(trq[0][:, q2, :], idxf[:, q2, :, :].rearrange("p h i -> p (h i)"), c["ident_f"][:], reads=[tidxf],
                            w=[ttrq[0]] if q2 == 0 else (), pw=() if q2 == 0 else [ttrq[0]])
                self.cp("act", idxT[:], trq[0][:, 0:2, :], reads=[ttrq[0]], w=[tidxT, ttrq[0]])
                for ig in range(4):
                    pi = (ig + 1) % 2
                    for i4 in range(4):
                        i = ig * 4 + i4
                        self.tr(trq[pi][:, i4, :], Wc2[:, i, :, :].rearrange("p h j -> p (h j)"), c["ident_f"][:], reads=[tmsk],
                                w=[ttrq[pi]] if i4 == 0 else (), pw=() if i4 == 0 else [ttrq[pi]])
                    self.cp("act" if ig % 2 == 0 else "dve", Tw[:, ig * 4:(ig + 1) * 4, :], trq[pi][:], reads=[ttrq[pi]],
                            w=([tTw] if ig == 0 else []) + [ttrq[pi]], pw=() if ig == 0 else [tTw])
                def r_phase(qd):
                    u = qd % 2
                    tq0 = qd * 4
                    self.tt("dve", A4[u][:], c["iota_b"][:].unsqueeze(1).to_broadcast([128, 4, 128]),
                            idxT[:, 0, tq0:tq0 + 4].unsqueeze(2).to_broadcast([128, 4, 128]), ALU.is_equal,
                            reads=[tidxT], w=[tA4[u]])
                    self.tt("dve", B4[u][:], c["iota_b"][:].unsqueeze(1).to_broadcast([128, 4, 128]),
                            idxT[:, 1, tq0:tq0 + 4].unsqueeze(2).to_broadcast([128, 4, 128]), ALU.is_equal,
                            reads=[tidxT], w=[tB4[u]])
                    self.tt("pool", W4[u][:], c["bmask_b"][:].unsqueeze(1).unsqueeze(3).to_broadcast([128, 4, 8, 16]),
                            Tw[:, :, tq0:tq0 + 4].rearrange("p i t -> p t i").unsqueeze(2).to_broadcast([128, 4, 8, 16]),
                            ALU.mult, reads=[tTw], w=[tW4[u]])
                    for t4 in range(4):
                        self.mm(Rp[u][:, t4, :], W4[u][:, t4, :, :].rearrange("p h i -> p (h i)"), B4[u][:, t4, :],
                                start=True, stop=True, reads=[tW4[u], tB4[u]],
                                w=[tRp[u]] if t4 == 0 else (), pw=() if t4 == 0 else [tRp[u]])
                    self.cp("act", R4[u][:], Rp[u][:], reads=[tRp[u]], w=[tR4[u], tRp[u]])

                def g_phase(qd):
                    u = qd % 2
                    tq0 = qd * 4
                    for t4 in range(4):
                        self.mm(Gp[u][:, t4, :], R4[u][:, t4, :], A4[u][:, t4, :], start=True, stop=True,
                                reads=[tR4[u], tA4[u]], w=[tGp[u]] if t4 == 0 else (), pw=() if t4 == 0 else [tGp[u]])
                    self.cp("act" if qd % 2 == 0 else "dve", G_t[:, :, tq0:tq0 + 4], Gp[u][:].rearrange("p t i -> p i t"),
                            reads=[tGp[u]], w=([tG_t] if qd == 0 else []) + [tGp[u]], pw=() if qd == 0 else [tG_t])

                r_phase(0)
                for qd in range(32):
                    if qd + 1 < 32:
                        r_phase(qd + 1)
                    g_phase(qd)
                for a8 in range(8):
                    self.dma("pool", Gv[:, a8 * 16:(a8 + 1) * 16, t0 - gbase:t0 - gbase + 128], G_t[:, a8 * 16:(a8 + 1) * 16, :],
                             reads=[tG_t], w=[])
        self.P.barrier()

    def stage_p0(self, l):
        D = self.D
        with ExitStack() as st:
            ust = [self.sb(st, [128, 8, 512], F32, "ust0") for _ in range(3)]
            tust = [Tok("ust0") for _ in range(3)]
            ubf = [self.sb(st, [128, 8, 512], BF16, "ubf0") for _ in range(3)]
            tubf = [Tok("ubf0") for _ in range(3)]
            uv = D["uT"][l].rearrange("(k p) e -> p k e", p=128)
            vview = D["pv"][l].rearrange("(a e) d -> e a d", e=128)
            ubv = self.ub_d.rearrange("(k p) e -> p k e", p=128)
            vbv = self.vb_d.rearrange("(a e) d -> e a d", e=128)
            engs = ("act", "dve", "act", "dve", "act", "pool")
            n = 0
            for pc in range(32):
                for which in range(2):
                    s = n % 3
                    if which == 0:
                        src = uv[:, :, pc * 512:(pc + 1) * 512]
                        dst = ubv[:, :, pc * 512:(pc + 1) * 512]
                        sview = ust[s][:]
                        bview = ubf[s][:]
                    else:
                        src = vview[:, pc * 4:(pc + 1) * 4, :]
                        dst = vbv[:, pc * 4:(pc + 1) * 4, :]
                        sview = ust[s][:].rearrange("p k e -> p (k e)").rearrange("p (a d) -> p a d", a=4)
                        bview = ubf[s][:].rearrange("p k e -> p (k e)").rearrange("p (a d) -> p a d", a=4)
                    self.dma("sp", sview, src, reads=[], w=[tust[s]])
                    self.cp(engs[n % 6], bview, sview, reads=[tust[s]], w=[tubf[s]])
                    self.dma("pool" if n % 2 == 0 else "act", dst, bview, reads=[tubf[s]], w=[])
                    n += 1
        self.P.barrier()

    def stage_p2(self, l, nsg=2, sg0=0, gbase=0):
        D, c = self.D, self.c
        NSUB = 3
        SG = NSUB * PG
        NTL = SG // 128
        with ExitStack() as st:
            ubf = [self.sb(st, [128, 8, 512], BF16, "ubf") for _ in range(3)]
            tubf = [Tok("ubf") for _ in range(3)]
            vbf = [self.sb(st, [128, 4, 1024], BF16, "vbf") for _ in range(3)]
            tvbf = [Tok("vbf") for _ in range(3)]
            gpc = [self.sb(st, [128, 4, SG], BF16, "gpc") for _ in range(3)]
            tgpc = [Tok("gpc") for _ in range(3)]
            xcg = self.sb(st, [128, 8, SG], BF16, "xcg")
            txcg = Tok("xcg")
            a_sb = [self.sb(st, [128, PG], BF16, "a_sb") for _ in range(2)]
            ta_sb = [Tok("a_sb") for _ in range(2)]
            ga = [self.sb(st, [128, PG], BF16, "ga") for _ in range(2)]
            tga = [Tok("ga") for _ in range(2)]
            acc = self.sb(st, [128, NTL, 1024], F32, "acc")
            tacc = [Tok("acc") for _ in range(NTL)]
            xt = [self.sb(st, [128, 8, 128], F32, "xt2") for _ in range(2)]
            txt = [Tok("xt2") for _ in range(2)]
            app = [self.ps(st, [128, 512], F32, "app") for _ in range(2)]
            tapp = [Tok("app") for _ in range(2)]
            oacc = [self.ps(st, [128, 1024], F32, "oacc2") for _ in range(3)]
            toacc = [Tok("oacc2") for _ in range(3)]
            uv = self.ub_d.rearrange("(k p) e -> p k e", p=128)
            vview = self.vb_d.rearrange("(a e) d -> e a d", e=128)
            Gv = self.G_d.rearrange("(a i) t -> i a t", i=128)
            xTv = self.xT_d.rearrange("k p t -> p k t")
            xcv = self.xc_d.rearrange("k p t -> p k t")
            NPC = 32
            self._npc = 0
            self._nch = 0
            nch = 0
            ntile = 0
            for sg in range(sg0, sg0 + nsg):
                tok0 = sg * SG
                self.dma("sp", xcg[:], xcv[:, :, tok0:tok0 + SG], reads=[], w=[txcg])
                steps = [(pc, sub, cc) for pc in range(NPC) for sub in range(NSUB) for cc in range(4)]
                pbuf = {}

                def u_phase(step):
                    pc, sub, cc = step
                    if sub == 0 and cc == 0:
                        s = self._npc % 3
                        self._npc += 1
                        pbuf[pc] = s
                        e0 = pc * 512
                        self.dma("sp", ubf[s][:], uv[:, :, e0:e0 + 512], reads=[], w=[tubf[s]])
                        self.dma("sp", vbf[s][:], vview[:, pc * 4:(pc + 1) * 4, :], reads=[], w=[tvbf[s]])
                        self.dma("sp", gpc[s][:], Gv[:, pc * 4:(pc + 1) * 4, tok0 - gbase:tok0 - gbase + SG], reads=[], w=[tgpc[s]])
                    s = pbuf[pc]
                    c0 = sub * PG
                    i = self._nch % 2
                    self._nch += 1
                    for k in range(8):
                        self.mm(app[i][:, 0:PG], ubf[s][:, k, cc * 128:(cc + 1) * 128], xcg[:, k, c0:c0 + PG],
                                start=(k == 0), stop=(k == 7), reads=[tubf[s], txcg],
                                w=[tapp[i]] if k == 0 else (), pw=() if k == 0 else [tapp[i]])
                    self.act(a_sb[i][:], app[i][:, 0:PG], AF.Gelu, reads=[tapp[i]], w=[ta_sb[i], tapp[i]])
                    self.tt("dve", ga[i][:], a_sb[i][:], gpc[s][:, cc, c0:c0 + PG], ALU.mult,
                            reads=[ta_sb[i], tgpc[s]], w=[tga[i]])
                    return i

                def v_phase(step, i):
                    pc, sub, cc = step
                    s = pbuf[pc]
                    for t3 in range(3):
                        for hf in range(2):
                            self.mm(oacc[t3][:, hf * 512:(hf + 1) * 512], ga[i][:, t3 * 128:(t3 + 1) * 128],
                                    vbf[s][:, cc, hf * 512:(hf + 1) * 512], start=(cc == 0), stop=(cc == 3),
                                    reads=[tga[i], tvbf[s]],
                                    w=[toacc[t3]] if (cc == 0 and hf == 0) else (),
                                    pw=() if (cc == 0 and hf == 0) else [toacc[t3]])
                    if cc == 3:
                        for t3 in range(3):
                            tl = sub * 3 + t3
                            if pc == 0:
                                self.cp("dve" if t3 != 1 else "act", acc[:, tl, :], oacc[t3][:], reads=[toacc[t3]],
                                        w=[tacc[tl], toacc[t3]])
                            else:
                                self.tt("dve", acc[:, tl, :], oacc[t3][:], acc[:, tl, :], ALU.add,
                                        reads=[toacc[t3], tacc[tl]], w=[tacc[tl], toacc[t3]])

                pend = u_phase(steps[0])
                for n_ in range(len(steps)):
                    nxt = u_phase(steps[n_ + 1]) if n_ + 1 < len(steps) else None
                    v_phase(steps[n_], pend)
                    pend = nxt
                nch = self._nch
                for tl in range(NTL):
                    ti = sg * NTL + tl
                    s2 = ntile % 2
                    ntile += 1
                    t0 = ti * 128
                    bb = ti // (TB // 128)
                    r = bb if (ti % (TB // 128)) < 16 else 2
                    self.dma("sp", xt[s2][:], xTv[:, :, t0:t0 + 128], reads=[], w=[txt[s2]])
                    for hf in range(2):
                        i = self._nch % 2
                        self._nch += 1
                        for q in range(4):
                            k = hf * 4 + q
                            self.tr(app[i][:, q * 128:(q + 1) * 128], acc[:, tl, k * 128:(k + 1) * 128], c["ident_f"][:],
                                    reads=[tacc[tl]], w=[tapp[i]] if q == 0 else (), pw=() if q == 0 else [tapp[i]])
                        for q in range(4):
                            k = hf * 4 + q
                            self.stt(xt[s2][:, k, :], app[i][:, q * 128:(q + 1) * 128], c["modT"][:, 40 + k, r:r + 1],
                                     xt[s2][:, k, :], ALU.mult, ALU.add, reads=[tapp[i], txt[s2]], w=[tapp[i]], pw=[txt[s2]])
                    self.dma("pool", xTv[:, :, t0:t0 + 128], xt[s2][:], reads=[txt[s2]], w=[])
        self.P.barrier()

    def stage_final(self):
        D, c = self.D, self.c
        with ExitStack() as st:
            xt = [self.sb(st, [128, 8, 128], F32, "xtf") for _ in range(2)]
            txt = [Tok("xtf") for _ in range(2)]
            sq = self.sb(st, [128, 8, 128], BF16, "sqf")
            tsq = Tok("sqf")
            ssp = self.ps(st, [128, 512], F32, "sspf")
            tssp = Tok("sspf")
            sd = self.sb(st, [128, 128], F32, "sdf")
            tsd = Tok("sdf")
            yt = [self.sb(st, [128, 8, 128], F32, "ytf") for _ in range(2)]
            tyt = [Tok("ytf") for _ in range(2)]
            trp = [self.ps(st, [128, 1024], F32, "trpf") for _ in range(2)]
            ttrp = [Tok("trpf") for _ in range(2)]
            yo = [self.sb(st, [128, 1024], F32, "yo") for _ in range(2)]
            tyo = [Tok("yo") for _ in range(2)]
            xTv = self.xT_d.rearrange("k p t -> p k t")
            n = 0
            for b in range(NB):
                for ti in range(16):
                    s = n % 2
                    n += 1
                    t0 = b * TB + ti * 128
                    self.dma("sp", xt[s][:], xTv[:, :, t0:t0 + 128], reads=[], w=[txt[s]])
                    self.act(sq[:], xt[s][:], AF.Square, reads=[txt[s]], w=[tsq])
                    for k in range(8):
                        self.mm(ssp[:, 0:128], c["ones_b"][:], sq[:, k, :], start=(k == 0), stop=(k == 7), reads=[tsq],
                                w=[tssp] if k == 0 else (), pw=() if k == 0 else [tssp])
                    self.act(sd[:], ssp[:, 0:128], AF.Sqrt, reads=[tssp], w=[tsd, tssp], scale=1.0 / 1024.0, bias=c["eps"][:])
                    self.recip(sd[:], sd[:], reads=[tsd], w=[tsd])
                    self.tt("dve", yt[s][:], xt[s][:], sd[:].unsqueeze(1).to_broadcast([128, 8, 128]), ALU.mult,
                            reads=[txt[s], tsd], w=[tyt[s]])
                    self.tt("pool", yt[s][:], yt[s][:], c["fng"][:].unsqueeze(2).to_broadcast([128, 8, 128]), ALU.mult,
                            reads=[tyt[s]], w=[tyt[s]])
                    for k in range(8):
                        self.tr(trp[s][:, k * 128:(k + 1) * 128], yt[s][:, k, :], c["ident_f"][:], reads=[tyt[s]],
                                w=[ttrp[s]] if k == 0 else (), pw=() if k == 0 else [ttrp[s]])
                    self.cp("act", yo[s][:], trp[s][:], reads=[ttrp[s]], w=[tyo[s], ttrp[s]])
                    self.dma("pool", self.out[b, ti * 128:(ti + 1) * 128, :], yo[s][:], reads=[tyo[s]], w=[])

    def batch_bufs(self, st):
        BB = {}
        BB["qna"] = self.sb(st, [128, 3, TB], BF16, "qna")
        BB["knaA"] = self.sb(st, [128, 3, TB], BF16, "knaA")
        BB["knaB"] = self.sb(st, [128, 3, TB], BF16, "knaB")
        BB["qg"] = self.sb(st, [128, 3, TB], BF16, "qg")
        BB["kgA"] = self.sb(st, [128, TB], BF16, "kgA")
        BB["kgB"] = self.sb(st, [128, TB], BF16, "kgB")
        BB["vv"] = self.sb(st, [128, TB // 128, 8, 65], BF16, "vv")
        BB["ubuf"] = self.sb(st, [128, 2, UW], BF16, "ubuf")
        for k in ["tqna", "tkna", "tqg", "tkg", "tvv", "tubuf", "tmix"]:
            BB[k] = Tok(k)
        self.memset("pool", BB["ubuf"][:], 0.0, w=[BB["tubuf"]])
        self.memset("pool", BB["knaA"][64:128, :, :], 0.0, w=[BB["tkna"]])
        self.memset("dve", BB["knaB"][0:64, :, :], 0.0, pw=[BB["tkna"]])
        self.memset("pool", BB["kgA"][64:128, :], 0.0, w=[BB["tkg"]])
        self.memset("dve", BB["kgB"][0:64, :], 0.0, pw=[BB["tkg"]])
        self.memset("pool", BB["vv"][:, :, :, 64:65], 1.0, w=[BB["tvv"]])
        return BB

    def build(self, depth=DEPTH, stages="all"):
        self.declare()
        with ExitStack() as st0:
            self.consts(st0)
            self.P.barrier()
            self._build_body(depth, stages)
            self.P.emit()
        return self.nc

    def _build_body(self, depth, stages):
        if stages == "c":
            return
        self.stage_s0()
        if stages == "s0":
            return
        for l in range(depth):
            do_ctx = l < DEPTH - 1
            self.stage_s1(l)
            if stages == "s1":
                return
            if stages == "pp":
                self.stage_p1(l, ntiles=3)
                self.stage_p0(l)
                self.stage_p2(l, nsg=1)
                return
            for b in range(NB):
                with ExitStack() as stb:
                    BB = self.batch_bufs(stb)
                    self.stage_s2(l, b, stb, BB)
                    if stages == "s2":
                        return
                    with ExitStack() as stm:
                        BB["mixT"] = self.sb(stm, [128, 8, TB], BF16, "mixT")
                        self.stage_s3(l, b, BB, do_ctx)
                        if stages == "s3":
                            return
                        self.stage_s4(l, b, BB, do_ctx)
            if stages == "mix":
                return
            self.stage_p0(l)
            for hb in range(2):
                self.stage_p1(l, ntiles=NTILE // 2, tile0=hb * (NTILE // 2), gbase=hb * (NT // 2))
                self.stage_p2(l, nsg=2, sg0=hb * 2, gbase=hb * (NT // 2))
            if stages == "l0":
                return
        self.stage_final()


OFF_A_VAL, OFF_A_GATE, OFF_NA_Q, OFF_G_Q = 0, 256, 512, 896
OFF_NA_K, OFF_NA_V, OFF_G_K, OFF_G_V = 1280, 1664, 2048, 2176


def _rope_tables():
    t = np.arange(TL, dtype=np.int32)
    row = (t // 64).astype(np.float32)
    col = (t % 64).astype(np.float32)
    n_freq = 16
    inv_freq = (np.float32(10000.0) ** (-np.arange(n_freq, dtype=np.float32) / np.float32(n_freq))).astype(np.float32)
    ang = np.concatenate([row[:, None] * inv_freq, col[:, None] * inv_freq], axis=-1).astype(np.float32)
    cos = np.cos(ang).astype(np.float32)
    sin = np.sin(ang).astype(np.float32)
    C = np.ones((128, TB), np.float32)
    S = np.zeros((128, TB), np.float32)
    for p in range(128):
        d = p % 64
        if d < 32:
            C[p, :TL] = cos[:, d]
            S[p, :TL] = -sin[:, d]
        else:
            C[p, :TL] = cos[:, d - 32]
            S[p, :TL] = sin[:, d - 32]
    return C, S


def _consts():
    C, S = _rope_tables()
    cols = np.arange(64)
    c0 = np.clip(cols - 8, 0, 48)
    cm = np.zeros((64, 64), np.float32)
    for c in range(64):
        cm[c0[c]:c0[c] + 16, c] = 1.0
    colmask2 = np.tile(cm, (2, 2)).astype(np.float32)
    blockmask = np.zeros((128, 8), np.float32)
    for p in range(128):
        blockmask[p, p // 16] = 1.0
    blockones = np.zeros((128, 128), np.float32)
    blockones[:64, :64] = 1.0
    blockones[64:, 64:] = 1.0
    return dict(ropeC=C, ropeS=S, colmask2=colmask2, blockmask=blockmask, blockones=blockones)


def _prep_shared(inp):
    f = np.float32
    L = DEPTH
    sh = {}
    sh["w_ada"] = np.ascontiguousarray(inp["w_ada"], dtype=f)
    sh["b_adaT"] = np.ascontiguousarray(inp["b_ada"].reshape(L, 48, 128).transpose(0, 2, 1))
    sh["n1gT"] = np.ascontiguousarray(inp["norm1_g"].reshape(L, 8, 128).transpose(0, 2, 1))
    sh["n2gT"] = np.ascontiguousarray(inp["norm2_g"].reshape(L, 8, 128).transpose(0, 2, 1))
    sh["fngT"] = np.ascontiguousarray(inp["final_norm_g"].reshape(8, 128).T)
    w_in = inp["w_in"]
    hperm = [0, 3, 1, 4, 2, 5]
    gq_cols = np.concatenate([OFF_G_Q + h * 64 + np.arange(64) for h in hperm])
    swp = np.concatenate([np.arange(32, 64), np.arange(0, 32)])
    gq_sw = np.concatenate([OFF_G_Q + h * 64 + swp for h in hperm])
    gk_cols = OFF_G_K + np.arange(128)
    gk_sw = np.concatenate([OFF_G_K + h * 64 + swp for h in range(2)])
    fm_cols = np.concatenate([np.arange(OFF_A_VAL, OFF_A_VAL + 256), np.arange(OFF_A_GATE, OFF_A_GATE + 256),
                              np.arange(OFF_NA_Q, OFF_NA_Q + 384), gq_cols, np.arange(OFF_NA_K, OFF_NA_K + 384),
                              gk_cols, gq_sw, gk_sw])
    assert fm_cols.shape[0] == 2304
    sh["w_fm"] = np.ascontiguousarray(w_in[:, :, fm_cols])
    v_cols = np.concatenate([np.arange(OFF_NA_V, OFF_NA_V + 384), np.arange(OFF_G_V, OFF_G_V + 128)])
    sh["w_v"] = np.ascontiguousarray(w_in[:, :, v_cols])
    p = np.arange(128)
    d = p % 64
    dsw = (d + 32) % 64
    sh["gq2"] = np.ascontiguousarray(np.stack([inp["gqa_q_norm"][:, d], inp["gqa_q_norm"][:, dsw]], axis=-1))
    sh["gk2"] = np.ascontiguousarray(np.stack([inp["gqa_k_norm"][:, d], inp["gqa_k_norm"][:, dsw]], axis=-1))
    sh["conv_wT"] = np.ascontiguousarray(inp["conv_w"].reshape(L, 31, 2, 128).transpose(0, 3, 2, 1))
    for k_src, k_dst in (("conv_b", "conv_bT"), ("conv_ln_g", "conv_lngT"), ("conv_ln_b", "conv_lnbT")):
        sh[k_dst] = np.ascontiguousarray(inp[k_src].reshape(L, 2, 128).transpose(0, 2, 1))
    cp_ = np.arange(64)[:, None]
    c_ = np.arange(64)[None, :]
    jidx = np.clip(cp_ - c_ + 15, 0, 30)
    sh["rb_exp"] = np.ascontiguousarray(inp["na_rel_bias"][:, :, :, jidx])
    sh["w_out"] = np.ascontiguousarray(inp["w_out"], dtype=f)
    sh["w_q"] = np.ascontiguousarray(inp["peer_w_q"], dtype=f)
    sh["keysT"] = np.ascontiguousarray(inp["peer_keys"].reshape(L, 16, 128, 128).transpose(0, 3, 1, 2))
    sh["uT"] = np.ascontiguousarray(inp["peer_u"].transpose(0, 2, 1))
    sh["pv"] = np.ascontiguousarray(inp["peer_v"], dtype=f)
    sh.update(_consts())
    return sh


def _prep_core(inp, core):
    b0 = core * NB
    cc = np.concatenate([inp["c"][b0:b0 + NB], inp["c_ctx"][None, :]], axis=0)
    m = {}
    m["x"] = np.ascontiguousarray(inp["x"][b0:b0 + NB])
    m["ctx"] = np.ascontiguousarray(inp["ctx"][b0:b0 + NB])
    m["ccT"] = np.ascontiguousarray(cc.reshape(3, 8, 128).transpose(2, 1, 0))
    return m


_NC_CACHE = {}


def kernel(**inputs):
    inp = {k: np.asarray(v, dtype=np.float32) for k, v in inputs.items()}
    n_cores = inp["x"].shape[0] // NB
    if "nc" not in _NC_CACHE:
        nc = bass.Bass("TRN2", target_bir_lowering=False)
        kk = K(nc)
        kk.build()
        _NC_CACHE["nc"] = nc
    nc = _NC_CACHE["nc"]
    sh = _prep_shared(inp)
    in_maps = []
    for core in range(n_cores):
        m = dict(sh)
        m.update(_prep_core(inp, core))
        in_maps.append(m)
    res = run_bass_kernel_spmd(nc, in_maps, core_ids=list(range(n_cores)))
    outs = [np.asarray(r["out"]) for r in res.results]
    return np.concatenate(outs, axis=0).astype(np.float32)
```

```python
import numpy as np
import concourse.bass as bass
import concourse.mybir as mybir
from concourse.bass_utils import run_bass_kernel_spmd
from contextlib import ExitStack

F32 = mybir.dt.float32
BF16 = mybir.dt.bfloat16
U32 = mybir.dt.uint32
I32 = mybir.dt.int32
AF = mybir.ActivationFunctionType
ALU = mybir.AluOpType
AX = mybir.AxisListType


ALLTOK = []


class Tok:
    __slots__ = ("name", "writers", "readers", "base")

    def __init__(self, name):
        self.name = name
        self.writers = []
        self.readers = []
        self.base = []
        ALLTOK.append(self)


class Op:
    __slots__ = ("eng", "fn", "deps", "dma", "signal", "count", "sem", "val",
                 "prev_val", "idx", "barrier")

    def __init__(self, eng, fn, dma):
        self.eng = eng
        self.fn = fn
        self.dma = dma
        self.deps = []
        self.signal = False
        self.count = 0
        self.sem = None
        self.val = 0
        self.prev_val = 0
        self.barrier = False


ENGS = ("pe", "act", "dve", "pool", "sp")
NSLOT = 12


class Prog:
    def __init__(self, nc):
        self.nc = nc
        self.ops = []
        self.last = {e: None for e in ENGS}
        self.dma_outstanding = []

    def tok(self, name="t"):
        return Tok(name)

    def toks(self, name, n):
        return [Tok("%s%d" % (name, i)) for i in range(n)]

    def op(self, eng, fn, reads=(), writes=(), pw=(), dma=False):
        o = Op(eng, fn, dma)
        deps = []
        for t in reads:
            deps.extend(t.writers)
        for t in writes:
            deps.extend(t.writers)
            deps.extend(t.readers)
        for t in pw:
            if t.readers:
                t.base = list(t.readers) + list(t.writers)
                t.writers = []
                t.readers = []
            deps.extend(t.base)
        for t in reads:
            t.readers.append(o)
        for t in writes:
            t.writers = [o]
            t.readers = []
            t.base = [o]
        for t in pw:
            t.writers.append(o)
        o.idx = len(self.ops)
        seen = set()
        best = {}
        for d in deps:
            if d is o or id(d) in seen:
                continue
            seen.add(id(d))
            if d.dma:
                o.deps.append(d)
            else:
                b = best.get(d.eng)
                if b is None or d.idx > b.idx:
                    best[d.eng] = d
        o.deps.extend(best.values())
        self.ops.append(o)
        self.last[eng] = o
        return o

    def barrier(self):
        b = Op(None, None, False)
        b.barrier = True
        b.deps = [o for o in self.ops if False]
        b.idx = len(self.ops)
        self.ops.append(b)
        for t in ALLTOK:
            t.writers = []
            t.readers = []
            t.base = []

    def emit(self):
        nc = self.nc
        ops = self.ops
        last = {e: None for e in ENGS}
        dmas_since = []
        pending = {e: [] for e in ENGS}
        for o in ops:
            if o.barrier:
                extra = [x for x in last.values() if x is not None] + dmas_since
                for e in ENGS:
                    pending[e] = list(extra)
                dmas_since = []
                continue
            if pending[o.eng]:
                seen = set(id(d) for d in o.deps)
                for d in pending[o.eng]:
                    if id(d) not in seen and d is not o:
                        o.deps.append(d)
                pending[o.eng] = []
            last[o.eng] = o
            if o.dma:
                dmas_since.append(o)
        real = [o for o in ops if not o.barrier]
        for o in real:
            for d in o.deps:
                if not d.dma:
                    if d.eng == "pe" and o.eng == "pe" and not o.dma:
                        continue
                    d.signal = True
        cnt = {e: 0 for e in ENGS}
        for o in real:
            if o.dma:
                continue
            if o.signal:
                cnt[o.eng] += 1
            o.count = cnt[o.eng]
        stack = ExitStack()
        sems = {e: stack.enter_context(nc.semaphore("s_" + e)) for e in ENGS}
        dsem = {}
        dval = {}
        dn = {e: 0 for e in ENGS}
        for o in real:
            if not o.dma:
                continue
            q = o.eng
            j = dn[q] % NSLOT
            dn[q] += 1
            key = (q, j)
            if key not in dsem:
                dsem[key] = stack.enter_context(nc.semaphore("d_%s%d" % (q, j)))
                dval[key] = 0
            o.sem = dsem[key]
            o.prev_val = dval[key]
            dval[key] += 16
            o.val = dval[key]
        per_eng = {e: [o for o in real if o.eng == e] for e in ENGS}

        def run(e, eng):
            waited = {}

            def wait(sem, v):
                k = id(sem)
                if waited.get(k, 0) >= v:
                    return
                waited[k] = v
                eng.wait_ge(sem, v)

            for o in per_eng[e]:
                for d in o.deps:
                    if d.dma:
                        wait(d.sem, d.val)
                    else:
                        if d.eng == "pe" and e == "pe" and not o.dma:
                            continue
                        wait(sems[d.eng], d.count)
                if o.dma:
                    if o.prev_val:
                        wait(o.sem, o.prev_val)
                    ins = o.fn(eng)
                    ins.then_inc(o.sem, 16)
                else:
                    ins = o.fn(eng)
                    if o.signal:
                        ins.then_inc(sems[e], 1)
            if e == "sp":
                for key, s in dsem.items():
                    wait(s, dval[key])
                for e2 in ENGS:
                    if e2 != "sp" and cnt[e2]:
                        wait(sems[e2], cnt[e2])

        with nc.Block() as block:
            @block.tensor
            def _(eng):
                run("pe", eng)

            @block.scalar
            def _(eng):
                run("act", eng)

            @block.vector
            def _(eng):
                run("dve", eng)

            @block.gpsimd
            def _(eng):
                run("pool", eng)

            @block.sync
            def _(eng):
                run("sp", eng)
        stack.close()


DEPTH = 2
NB = 2
TL = 2048
TC = 256
TB = TL + TC
NT = NB * TB
NTILE = NT // 128
GT = 256
NGB = TB // GT
EPS = 1e-6
ULAT0 = 15
UCTX0 = 15 + TL + 15 + 15
UW = UCTX0 + TC + 15
PG = 384
NPG = NT // PG
NEG = -1.0e30


def na_tiles(j):
    res = []
    for m in range(16):
        pat = []
        anyv = False
        for a in range(2):
            row = []
            for b in range(2):
                r = 2 * j + b
                r0 = min(max(r - 4, 0), 24)
                kr = 2 * m + a
                v = (r0 <= kr <= r0 + 7)
                row.append(v)
                anyv = anyv or v
            pat.append(tuple(row))
        if anyv:
            res.append((m, tuple(pat)))
    return res


def na_combos():
    combos = []
    for j in range(16):
        for m, pat in na_tiles(j):
            key = (m - j, pat)
            if key not in combos:
                combos.append(key)
    return combos


class K:
    def __init__(self, nc, dbg=()):
        self.nc = nc
        self.P = Prog(nc)
        self.dbg = dbg
        self.D = {}
        self.nm = 0

    def din(self, name, shape, dt=F32):
        self.D[name] = self.nc.dram_tensor(name, list(shape), dt, kind="ExternalInput").ap()

    def sb(self, st, shape, dt, name=None):
        self.nm += 1
        return st.enter_context(self.nc.sbuf_tensor("%s_%d" % (name or "sb", self.nm), list(shape), dt))

    def ps(self, st, shape, dt, name=None):
        self.nm += 1
        return st.enter_context(self.nc.psum_tensor("%s_%d" % (name or "ps", self.nm), list(shape), dt))

    def tok(self, n="t"):
        return Tok(n)

    def mm(self, out, lhsT, rhs, start, stop, reads, w=(), pw=(), sgc=False):
        self.P.op("pe", lambda e: e.matmul(out, lhsT=lhsT, rhs=rhs, start=start, stop=stop, skip_group_check=sgc),
                  reads=reads, writes=w, pw=pw)

    def tr(self, out, in_, ident, reads, w=(), pw=()):
        self.P.op("pe", lambda e: e.transpose(out, in_, ident), reads=reads, writes=w, pw=pw)

    def act(self, out, in_, func, reads, w=(), pw=(), scale=None, bias=None):
        kw = {}
        if scale is not None:
            kw["scale"] = scale
        if bias is not None:
            kw["bias"] = bias
        self.P.op("act", lambda e: e.activation(out=out, in_=in_, func=func, **kw),
                  reads=reads, writes=w, pw=pw)

    def tt(self, eng, out, in0, in1, op, reads, w=(), pw=()):
        self.P.op(eng, lambda e: e.tensor_tensor(out=out, in0=in0, in1=in1, op=op),
                  reads=reads, writes=w, pw=pw)

    def ts(self, eng, out, in0, s1, op0, reads, w=(), pw=(), s2=None, op1=None):
        if op1 is None:
            self.P.op(eng, lambda e: e.tensor_scalar(out=out, in0=in0, scalar1=s1, scalar2=None, op0=op0),
                      reads=reads, writes=w, pw=pw)
        else:
            self.P.op(eng, lambda e: e.tensor_scalar(out=out, in0=in0, scalar1=s1, scalar2=s2, op0=op0, op1=op1),
                      reads=reads, writes=w, pw=pw)

    def stt(self, out, in0, scalar, in1, op0, op1, reads, w=(), pw=()):
        self.P.op("dve", lambda e: e.scalar_tensor_tensor(out=out, in0=in0, scalar=scalar, in1=in1, op0=op0, op1=op1),
                  reads=reads, writes=w, pw=pw)

    def cp(self, eng, out, in_, reads, w=(), pw=()):
        if eng == "act":
            self.P.op("act", lambda e: e.copy(out=out, in_=in_), reads=reads, writes=w, pw=pw)
        else:
            self.P.op(eng, lambda e: e.tensor_copy(out=out, in_=in_), reads=reads, writes=w, pw=pw)

    def recip(self, out, in_, reads, w=(), pw=()):
        self.P.op("dve", lambda e: e.reciprocal(out=out, in_=in_), reads=reads, writes=w, pw=pw)

    def memset(self, eng, ap, val, w=(), pw=(), reads=()):
        self.P.op(eng, lambda e: e.memset(ap, val), reads=reads, writes=w, pw=pw)

    def dma(self, q, out, in_, reads, w=(), pw=()):
        self.P.op(q, lambda e: e.dma_start(out=out, in_=in_), reads=reads, writes=w, pw=pw, dma=True)

    def declare(self):
        din = self.din
        din("x", [NB, TL, 1024]); din("ctx", [NB, TC, 1024]); din("ccT", [128, 8, 3])
        din("w_ada", [DEPTH, 1024, 6144]); din("b_adaT", [DEPTH, 128, 48])
        din("n1gT", [DEPTH, 128, 8]); din("n2gT", [DEPTH, 128, 8]); din("fngT", [128, 8])
        din("w_fm", [DEPTH, 1024, 2304]); din("w_v", [DEPTH, 1024, 512])
        din("gq2", [DEPTH, 128, 2]); din("gk2", [DEPTH, 128, 2])
        din("conv_wT", [DEPTH, 128, 2, 31]); din("conv_bT", [DEPTH, 128, 2])
        din("conv_lngT", [DEPTH, 128, 2]); din("conv_lnbT", [DEPTH, 128, 2])
        din("rb_exp", [DEPTH, 6, 15, 64, 64])
        din("w_out", [DEPTH, 1024, 1024]); din("w_q", [DEPTH, 1024, 2048])
        din("keysT", [DEPTH, 128, 16, 128]); din("uT", [DEPTH, 1024, 16384]); din("pv", [DEPTH, 16384, 1024])
        din("ropeC", [128, TB]); din("ropeS", [128, TB]); din("colmask2", [128, 128])
        din("blockmask", [128, 8]); din("blockones", [128, 128])
        nc = self.nc
        self.out = nc.dram_tensor("out", [NB, TL, 1024], F32, kind="ExternalOutput").ap()
        if self.dbg:
            self.xT_d = nc.dram_tensor("xT_d", [8, 128, NT], F32, kind="ExternalOutput").ap()
        else:
            self.xT_d = nc.dram_tensor("xT_d", [8, 128, NT], F32).ap()
        self.xc_d = nc.dram_tensor("xc_d", [8, 128, NT], BF16).ap()
        self.G_d = nc.dram_tensor("G_d", [16384, NT // 2], BF16).ap()
        self.ub_d = nc.dram_tensor("ub_d", [1024, 16384], BF16).ap()
        self.vb_d = nc.dram_tensor("vb_d", [16384, 1024], BF16).ap()
        self.xT_tok = [Tok("xT%d" % i) for i in range(NTILE)]
        self.xc_tok = [Tok("xc%d" % i) for i in range(NTILE)]
        self.G_tok = [Tok("G%d" % i) for i in range(NTILE)]
        self.dbg_out = {}

    def dbg_tensor(self, name, shape, dt=F32):
        ap = self.nc.dram_tensor(name, list(shape), dt, kind="ExternalOutput").ap()
        self.dbg_out[name] = ap
        return ap

    def consts(self, st):
        D = self.D
        c = {}
        self.c = c
        c["ident_f"] = self.sb(st, [128, 128], F32, "identf")
        c["ident_b"] = self.sb(st, [128, 128], BF16, "identb")
        c["ones_b"] = self.sb(st, [128, 128], BF16, "onesb")
        c["bones_b"] = self.sb(st, [128, 128], BF16, "bonesb")
        c["iota_f"] = self.sb(st, [128, 128], F32, "iotaf")
        c["iota_b"] = self.sb(st, [128, 128], BF16, "iotab")
        c["bmask_b"] = self.sb(st, [128, 8], BF16, "bmaskb")
        c["cmask_b"] = self.sb(st, [128, 128], BF16, "cmaskb")
        c["ropeC"] = self.sb(st, [128, TB], BF16, "ropeC")
        c["ropeS"] = self.sb(st, [128, TB], BF16, "ropeS")
        c["siluT"] = self.sb(st, [128, 8, 3], F32, "siluT")
        c["fng"] = self.sb(st, [128, 8], F32, "fng")
        c["modT"] = self.sb(st, [128, 48, 3], F32, "modT")
        c["A1"] = self.sb(st, [128, 8, 3], F32, "A1")
        c["A2"] = self.sb(st, [128, 8, 3], F32, "A2")
        c["eps"] = self.sb(st, [128, 1], F32, "eps")
        tmpf = self.sb(st, [128, 128], F32, "ctmp")
        tmp8 = self.sb(st, [128, 8], F32, "ctmp8")
        T = self.T = {k: Tok(k) for k in ["const", "modT", "A", "ctmp", "ctmp8", "silu"]}
        ct = [T["const"]]
        self.memset("pool", c["ident_f"][:], 0.0, w=[T["ctmp"]])
        self.P.op("pool", lambda e: e.affine_select(out=c["ident_f"][:], in_=c["ident_f"][:], pattern=[[-1, 128]],
                                                    compare_op=ALU.not_equal, fill=1.0, base=0, channel_multiplier=1),
                  reads=[T["ctmp"]], writes=[T["ctmp"]])
        self.cp("dve", c["ident_b"][:], c["ident_f"][:], reads=[T["ctmp"]], pw=ct)
        self.memset("dve", c["ones_b"][:], 1.0, pw=ct)
        self.memset("dve", c["eps"][:], EPS, pw=ct)
        tio = Tok("iota")
        self.P.op("pool", lambda e: e.iota(c["iota_f"][:], pattern=[[1, 128]], base=0, channel_multiplier=0,
                                           allow_small_or_imprecise_dtypes=True), writes=[tio])
        self.cp("dve", c["iota_b"][:], c["iota_f"][:], reads=[tio], pw=ct)
        self.dma("sp", tmpf[:], D["blockones"][:, :], reads=[], w=[T["ctmp8"]])
        self.cp("dve", c["bones_b"][:], tmpf[:], reads=[T["ctmp8"]], pw=ct)
        tmpf2 = self.sb(st, [128, 128], F32, "ctmp2")
        t2 = Tok("ctmp2")
        self.dma("sp", tmpf2[:], D["colmask2"][:, :], reads=[], w=[t2])
        self.cp("dve", c["cmask_b"][:], tmpf2[:], reads=[t2], pw=ct)
        t3 = Tok("ctmp3")
        self.dma("sp", tmp8[:], D["blockmask"][:, :], reads=[], w=[t3])
        self.cp("dve", c["bmask_b"][:], tmp8[:], reads=[t3], pw=ct)
        with ExitStack() as st2:
            rst = [self.sb(st2, [128, TB], F32, "ropest") for _ in range(2)]
            trst = [Tok("ropest") for _ in range(2)]
            for i, nm in enumerate(("ropeC", "ropeS")):
                self.dma("sp", rst[i][:], D[nm][:, :], reads=[], w=[trst[i]])
                self.cp("dve", c[nm][:], rst[i][:], reads=[trst[i]], pw=ct)
            self.P.barrier()
        self.dma("sp", c["fng"][:], D["fngT"][:, :], reads=[], pw=ct)
        t4 = Tok("cc")
        cct = self.sb(st, [128, 8, 3], F32, "cct")
        self.dma("sp", cct[:], D["ccT"][:, :, :], reads=[], w=[t4])
        self.act(c["siluT"][:], cct[:], AF.Silu, reads=[t4], w=[T["silu"]])

    def stage_s0(self):
        D, c, T = self.D, self.c, self.T
        with ExitStack() as st:
            xin = [self.sb(st, [128, 1024], F32, "xin") for _ in range(2)]
            xo = [self.sb(st, [128, 8, 128], F32, "xo") for _ in range(2)]
            pp = [self.ps(st, [128, 4, 128], F32, "s0p") for _ in range(4)]
            txin = [Tok("xin") for _ in range(2)]
            txo = [Tok("xo") for _ in range(2)]
            tpp = [Tok("pp") for _ in range(4)]
            n = 0
            for b in range(NB):
                for ti in range(TB // 128):
                    s = n % 2
                    if ti < 16:
                        src = D["x"][b, ti * 128:(ti + 1) * 128, :]
                    else:
                        src = D["ctx"][b, (ti - 16) * 128:(ti - 15) * 128, :]
                    self.dma("sp", xin[s][:], src, reads=[], w=[txin[s]])
                    for hf in range(2):
                        pi = (n * 2 + hf) % 4
                        for q in range(4):
                            k = hf * 4 + q
                            self.tr(pp[pi][:, q, :], xin[s][:, k * 128:(k + 1) * 128], c["ident_f"][:],
                                    reads=[txin[s], T["const"], T["ctmp"]],
                                    w=[tpp[pi]] if q == 0 else (), pw=() if q == 0 else [tpp[pi]])
                        self.cp("act" if hf == 0 else "dve", xo[s][:, hf * 4:(hf + 1) * 4, :], pp[pi][:],
                                reads=[tpp[pi]], w=([txo[s]] if hf == 0 else []) + [tpp[pi]], pw=() if hf == 0 else [txo[s]])
                    tile = b * (TB // 128) + ti
                    self.dma("pool", self.xT_d.rearrange("k p t -> p k t")[:, :, tile * 128:(tile + 1) * 128], xo[s][:],
                             reads=[txo[s]], w=[self.xT_tok[tile]])
                    n += 1
        self.P.barrier()

    def stage_s1(self, l):
        D, c, T = self.D, self.c, self.T
        with ExitStack() as st:
            wst = [self.sb(st, [128, 8, 512], F32, "wada") for _ in range(2)]
            twst = [Tok("wada") for _ in range(2)]
            bad = self.sb(st, [128, 48], F32, "bada")
            n1g = self.sb(st, [128, 8], F32, "n1g")
            n2g = self.sb(st, [128, 8], F32, "n2g")
            tsm = Tok("s1small")
            tmpa = self.sb(st, [128, 8, 3], F32, "tmpa")
            ttmp = Tok("tmpa")
            modp = self.ps(st, [128, 128, 4], F32, "modp")
            tmodp = Tok("modp")
            self.dma("pool", bad[:], D["b_adaT"][l], reads=[], pw=[tsm])
            self.dma("pool", n1g[:], D["n1gT"][l], reads=[], pw=[tsm])
            self.dma("pool", n2g[:], D["n2gT"][l], reads=[], pw=[tsm])
            wv = D["w_ada"][l].rearrange("(k p) c -> p k c", p=128)
            for jc in range(12):
                s = jc % 2
                self.dma("sp", wst[s][:], wv[:, :, jc * 512:(jc + 1) * 512], reads=[], w=[twst[s]])
                for q in range(4):
                    j = jc * 4 + q
                    for k in range(8):
                        self.mm(modp[:, j, 0:3], wst[s][:, k, q * 128:(q + 1) * 128], c["siluT"][:, k, :],
                                start=(k == 0), stop=(k == 7), reads=[twst[s], T["silu"]],
                                w=[tmodp] if (j == 0 and k == 0) else (), pw=() if (j == 0 and k == 0) else [tmodp])
            self.tt("dve", c["modT"][:], modp[:, 0:48, 0:3], bad[:].unsqueeze(2).to_broadcast([128, 48, 3]), ALU.add,
                    reads=[tmodp, tsm], w=[T["modT"], tmodp])
            for (dst, nrm, base) in ((c["A1"], n1g, 8), (c["A2"], n2g, 32)):
                self.ts("dve", tmpa[:], c["modT"][:, base:base + 8, :], 1.0, ALU.add, reads=[T["modT"]], w=[ttmp])
                self.tt("dve", dst[:], tmpa[:], nrm[:].unsqueeze(2).to_broadcast([128, 8, 3]), ALU.mult,
                        reads=[ttmp, tsm], pw=[T["A"]])
        self.P.barrier()

    def norm_mod(self, st_bufs, xg, txg, hT, thT, A, sh_base, r, n, pfx):
        c = self.c
        B = st_bufs
        self.act(B["sq"][:, :, 0:n], xg, AF.Square, reads=[txg], w=[B["tsq"]])
        for k in range(8):
            self.mm(B["ssp"][:, 0:n], c["ones_b"][:], B["sq"][:, k, 0:n], start=(k == 0), stop=(k == 7),
                    reads=[B["tsq"]], w=[B["tssp"]] if k == 0 else (), pw=() if k == 0 else [B["tssp"]])
        self.act(B["sd"][:, 0:n], B["ssp"][:, 0:n], AF.Sqrt, reads=[B["tssp"]], w=[B["tsd"], B["tssp"]],
                 scale=1.0 / 1024.0, bias=c["eps"][:])
        self.recip(B["rstd"][:, 0:n], B["sd"][:, 0:n], reads=[B["tsd"]], w=[B["trstd"]])
        self.tt("dve", B["tmp"][:, :, 0:n], xg, B["rstd"][:, 0:n].unsqueeze(1).to_broadcast([128, 8, n]), ALU.mult,
                reads=[txg, B["trstd"]], w=[B["ttmp"]])
        for k in range(8):
            self.act(hT[:, k, :], B["tmp"][:, k, 0:n], AF.Identity, reads=[B["ttmp"]],
                     w=[thT] if k == 0 else (), pw=() if k == 0 else [thT],
                     scale=A[:, k, r:r + 1], bias=c["modT"][:, sh_base + k, r:r + 1])

    def norm_bufs(self, st, n):
        B = {}
        B["sq"] = self.sb(st, [128, 8, n], BF16, "sq")
        B["ssp"] = self.ps(st, [128, 512], F32, "ssp")
        B["sd"] = self.sb(st, [128, n], F32, "sd")
        B["rstd"] = self.sb(st, [128, n], F32, "rstd")
        B["tmp"] = self.sb(st, [128, 8, n], F32, "ntmp")
        for k in ["tsq", "tssp", "tsd", "trstd", "ttmp"]:
            B[k] = Tok(k)
        return B

    def load_cast(self, st, dst, src_view, ncols, piece, engs=("act", "pool", "dve"), stg=None):
        if stg is None:
            stg = ([self.sb(st, [128, 8, piece], F32, "wstg") for _ in range(2)], [Tok("wstg") for _ in range(2)])
        stg, tst = stg
        tdst = Tok("wdst")
        npc = ncols // piece
        for i in range(npc):
            s = i % 2
            self.dma("sp", stg[s][:], src_view[:, :, i * piece:(i + 1) * piece], reads=[], w=[tst[s]])
            self.cp(engs[i % len(engs)], dst[:, :, i * piece:(i + 1) * piece], stg[s][:], reads=[tst[s]], pw=[tdst])
        return tdst

    def stage_s2(self, l, b, st_b, BB):
        D, c = self.D, self.c
        with ExitStack() as st:
            wfm = self.sb(st, [128, 8, 2304], BF16, "wfm")
            wv = self.sb(st, [128, 8, 512], BF16, "wv")
            stg = ([self.sb(st, [128, 8, 128], F32, "wstg") for _ in range(2)], [Tok("wstg") for _ in range(2)])
            twfm = self.load_cast(st, wfm, D["w_fm"][l].rearrange("(k p) c -> p k c", p=128), 2304, 128, stg=stg)
            twv = self.load_cast(st, wv, D["w_v"][l].rearrange("(k p) c -> p k c", p=128), 512, 128, stg=stg)
            gq2 = self.sb(st, [128, 2], F32, "gq2")
            gk2 = self.sb(st, [128, 2], F32, "gk2")
            gq2s = self.sb(st, [128, 2], F32, "gq2s")
            tg = Tok("g2")
            tgs = Tok("g2s")
            self.dma("pool", gq2[:], D["gq2"][l], reads=[], pw=[tg])
            self.dma("pool", gk2[:], D["gk2"][l], reads=[], pw=[tg])
            self.ts("dve", gq2s[:], gq2[:], 0.125, ALU.mult, reads=[tg], w=[tgs])
            NBUF = self.norm_bufs(st, GT)
            xg1 = self.sb(st, [128, 8, GT], F32, "xg")
            xg = [xg1, xg1]
            txg1 = Tok("xg")
            txg = [txg1, txg1]
            hT = [self.sb(st, [128, 8, GT], BF16, "hT") for _ in range(2)]
            thT = [Tok("hT") for _ in range(2)]
            zp = [self.ps(st, [128, 2, GT], F32, "zp") for _ in range(4)]
            tzp = [Tok("zp") for _ in range(4)]
            vtp = [self.ps(st, [128, 512], F32, "vtp") for _ in range(2)]
            tvtp = [Tok("vtp") for _ in range(2)]
            ssz = self.ps(st, [128, 2, GT], F32, "ssz")
            tssz = Tok("ssz")
            sg = [self.sb(st, [128, GT], F32, "sg") for _ in range(2)]
            tsg = [Tok("sg") for _ in range(2)]
            sqz = [self.sb(st, [128, GT], BF16, "sqz") for _ in range(2)]
            tsqz = [Tok("sqz") for _ in range(2)]
            sdz = [self.sb(st, [128, GT], F32, "sdz") for _ in range(2)]
            tsdz = [Tok("sdz") for _ in range(2)]
            rz = [self.sb(st, [128, GT], F32, "rz") for _ in range(2)]
            trz = [Tok("rz") for _ in range(2)]
            t1 = [self.sb(st, [128, GT], F32, "t1") for _ in range(2)]
            tt1 = [Tok("t1") for _ in range(2)]
            t2 = [self.sb(st, [128, GT], F32, "t2") for _ in range(2)]
            tt2 = [Tok("t2") for _ in range(2)]
            xTv = self.xT_d.rearrange("k p t -> p k t")
            zc = [0]

            def zpair(ciA, ciB, s, thTs):
                i = zc[0] % 4
                zc[0] += 1
                tk = tzp[i]
                for h2, ci in enumerate((ciA, ciB)):
                    for k in range(8):
                        first = (h2 == 0 and k == 0)
                        self.mm(zp[i][:, h2, :], wfm[:, k, ci * 128:(ci + 1) * 128], hT[s][:, k, :], start=(k == 0), stop=(k == 7),
                                reads=[twfm, thTs], w=[tk] if first else (), pw=() if first else [tk])
                return zp[i][:, 0, :], zp[i][:, 1, :], tk

            nrope = [0]
            for g in range(NGB):
                s = g % 2
                t0 = b * TB + g * GT
                r = b if g < 8 else 2
                pos0 = g * GT
                self.dma("sp", xg[s][:], xTv[:, :, t0:t0 + GT], reads=[], w=[txg[s]])
                self.norm_mod(NBUF, xg[s][:], txg[s], hT[s], thT[s], c["A1"], 0, r, GT, "s2")
                ucol0 = (ULAT0 + pos0) if g < 8 else UCTX0
                for cc in range(2):
                    zv, zg, tk = zpair(cc, 2 + cc, s, thT[s])
                    self.act(sg[cc][:], zg, AF.Sigmoid, reads=[tk], w=[tsg[cc], tk])
                    self.tt("dve", BB["ubuf"][:, cc, ucol0:ucol0 + GT], zv, sg[cc][:], ALU.mult,
                            reads=[tk, tsg[cc]], w=[tk], pw=[BB["tubuf"]])
                for cc in range(3):
                    zq, zk, tk = zpair(4 + cc, 10 + cc, s, thT[s])
                    self.act(BB["qna"][:, cc, pos0:pos0 + GT], zq, AF.Identity, reads=[tk], w=[tk], pw=[BB["tqna"]], scale=0.125)
                    self.cp("dve", BB["knaA"][0:64, cc, pos0:pos0 + GT], zk[0:64, :], reads=[tk], w=[tk], pw=[BB["tkna"]])
                    self.cp("dve", BB["knaB"][64:128, cc, pos0:pos0 + GT], zk[64:128, :], reads=[tk], w=[tk], pw=[BB["tkna"]])
                for cc in range(4):
                    if cc < 3:
                        ci, cis, gg = 7 + cc, 14 + cc, gq2s
                        dst, tdst = BB["qg"][:, cc, pos0:pos0 + GT], BB["tqg"]
                    else:
                        ci, cis, gg = 13, 17, gk2
                        dst, tdst = None, BB["tkg"]
                    i = nrope[0] % 2
                    nrope[0] += 1
                    z, zs, tk = zpair(ci, cis, s, thT[s])
                    self.act(sqz[i][:], z, AF.Square, reads=[tk], w=[tsqz[i], tk])
                    self.mm(ssz[:, i, :], c["bones_b"][:], sqz[i][:], start=True, stop=True, reads=[tsqz[i]], w=[tssz])
                    self.act(sdz[i][:], ssz[:, i, :], AF.Sqrt, reads=[tssz], w=[tsdz[i], tssz], scale=1.0 / 64.0, bias=c["eps"][:])
                    self.recip(rz[i][:], sdz[i][:], reads=[tsdz[i]], w=[trz[i]])
                    self.stt(t1[i][:], z, gg[:, 0:1], c["ropeC"][:, pos0:pos0 + GT], ALU.mult, ALU.mult,
                             reads=[tk, tg, tgs], w=[tt1[i], tk])
                    self.stt(t2[i][:], zs, gg[:, 1:2], c["ropeS"][:, pos0:pos0 + GT], ALU.mult, ALU.mult,
                             reads=[tk, tg, tgs], w=[tt2[i], tk])
                    self.tt("pool", t1[i][:], t1[i][:], t2[i][:], ALU.add, reads=[tt1[i], tt2[i]], w=[tt1[i]])
                    if dst is not None:
                        self.tt("dve", dst, t1[i][:], rz[i][:], ALU.mult, reads=[tt1[i], trz[i]], pw=[tdst])
                    else:
                        self.tt("dve", BB["kgA"][0:64, pos0:pos0 + GT], t1[i][0:64, :], rz[i][0:64, :], ALU.mult,
                                reads=[tt1[i], trz[i]], pw=[tdst])
                        self.tt("dve", BB["kgB"][64:128, pos0:pos0 + GT], t1[i][64:128, :], rz[i][64:128, :], ALU.mult,
                                reads=[tt1[i], trz[i]], pw=[tdst])
                for tt_ in range(2):
                    vi = (g * 2 + tt_) % 2
                    for k in range(8):
                        self.mm(vtp[vi][:], hT[s][:, k, tt_ * 128:(tt_ + 1) * 128], wv[:, k, :], start=(k == 0), stop=(k == 7),
                                reads=[thT[s], twv], w=[tvtp[vi]] if k == 0 else (), pw=() if k == 0 else [tvtp[vi]])
                    self.cp("act", BB["vv"][:, g * 2 + tt_, :, 0:64], vtp[vi][:].rearrange("p (h d) -> p h d", d=64),
                            reads=[tvtp[vi]], w=[tvtp[vi]], pw=[BB["tvv"]])
        self.P.barrier()

    def stage_s3(self, l, b, BB, do_ctx):
        D, c = self.D, self.c
        mixT, tmix = BB["mixT"], BB["tmix"]
        with ExitStack() as st:
            cw = self.sb(st, [128, 2, 31], F32, "cw")
            cb = self.sb(st, [128, 2], F32, "cb")
            lng = self.sb(st, [128, 2], F32, "lng")
            lnb = self.sb(st, [128, 2], F32, "lnb")
            tcs = Tok("convsmall")
            self.dma("pool", cw[:], D["conv_wT"][l], reads=[], pw=[tcs])
            self.dma("pool", cb[:], D["conv_bT"][l], reads=[], pw=[tcs])
            self.dma("pool", lng[:], D["conv_lngT"][l], reads=[], pw=[tcs])
            self.dma("pool", lnb[:], D["conv_lnbT"][l], reads=[], pw=[tcs])
            dg = self.sb(st, [128, 2, 31, 128], BF16, "dg")
            tdg = Tok("dg")
            n = 0
            for cc in range(2):
                for k in range(31):
                    self.ts("dve" if n % 2 == 0 else "pool", dg[:, cc, k, :], c["ident_b"][:], cw[:, cc, k:k + 1], ALU.mult,
                            reads=[tcs], pw=[tdg])
                    n += 1
            yp = [self.ps(st, [128, 2, GT], F32, "yp") for _ in range(2)]
            typ = [Tok("yp") for _ in range(2)]
            lsp = self.ps(st, [128, 2, GT], F32, "lsp")
            tlsp = Tok("lsp")
            ycb = [self.sb(st, [128, 2, GT], F32, "ycb") for _ in range(2)]
            tycb = [Tok("ycb") for _ in range(2)]
            ysq = [self.sb(st, [128, 2, GT], BF16, "ysq") for _ in range(2)]
            tysq = [Tok("ysq") for _ in range(2)]
            yb = [self.sb(st, [128, 2, GT], BF16, "yb") for _ in range(2)]
            tyb = [Tok("yb") for _ in range(2)]
            mu = self.sb(st, [128, GT], F32, "mu")
            musq = self.sb(st, [128, GT], F32, "musq")
            var = self.sb(st, [128, GT], F32, "var")
            rs = self.sb(st, [128, GT], F32, "rs")
            tmu, tmusq, tvar, trs = Tok("mu"), Tok("musq"), Tok("var"), Tok("rs")
            ct = [self.sb(st, [128, GT], F32, "ct") for _ in range(2)]
            tct = [Tok("ct") for _ in range(2)]
            import os
            parts = os.environ.get("S3PARTS", "cma")
            ngr = NGB if do_ctx else 8
            if "c" not in parts:
                ngr = 0
            for g in range(ngr):
                s = g % 2
                pos0 = g * GT
                ub0 = (pos0 if g < 8 else (UCTX0 - 15))
                for cc in range(2):
                    for k in range(31):
                        self.mm(yp[s][:, cc, :], dg[:, cc, k, :], BB["ubuf"][:, cc, ub0 + k:ub0 + k + GT],
                                start=(k == 0), stop=(k == 30), reads=[tdg, BB["tubuf"]],
                                w=[typ[s]] if (cc == 0 and k == 0) else (), pw=() if (cc == 0 and k == 0) else [typ[s]])
                for cc in range(2):
                    self.act(ycb[s][:, cc, :], yp[s][:, cc, :], AF.Identity, reads=[typ[s], tcs],
                             w=([tycb[s]] if cc == 0 else []) + [typ[s]], pw=() if cc == 0 else [tycb[s]], bias=cb[:, cc:cc + 1])
                    self.act(ysq[s][:, cc, :], yp[s][:, cc, :], AF.Square, reads=[typ[s], tcs],
                             w=([tysq[s]] if cc == 0 else []) + [typ[s]], pw=() if cc == 0 else [tysq[s]], bias=cb[:, cc:cc + 1])
                self.cp("pool", yb[s][:], ycb[s][:], reads=[tycb[s]], w=[tyb[s]])
                for cc in range(2):
                    self.mm(lsp[:, 0, :], c["ones_b"][:], yb[s][:, cc, :], start=(cc == 0), stop=(cc == 1),
                            reads=[tyb[s]], w=[tlsp] if cc == 0 else (), pw=() if cc == 0 else [tlsp])
                for cc in range(2):
                    self.mm(lsp[:, 1, :], c["ones_b"][:], ysq[s][:, cc, :], start=(cc == 0), stop=(cc == 1),
                            reads=[tysq[s]], pw=[tlsp])
                self.act(mu[:], lsp[:, 0, :], AF.Identity, reads=[tlsp], w=[tmu, tlsp], scale=1.0 / 256.0)
                self.tt("dve", musq[:], mu[:], mu[:], ALU.mult, reads=[tmu], w=[tmusq])
                self.stt(var[:], lsp[:, 1, :], 1.0 / 256.0, musq[:], ALU.mult, ALU.subtract, reads=[tlsp, tmusq], w=[tvar, tlsp])
                self.act(var[:], var[:], AF.Sqrt, reads=[tvar], w=[tvar], bias=c["eps"][:])
                self.recip(rs[:], var[:], reads=[tvar], w=[trs])
                for cc in range(2):
                    self.tt("dve", ct[cc][:], ycb[s][:, cc, :], mu[:], ALU.subtract, reads=[tycb[s], tmu], w=[tct[cc]])
                    self.tt("pool", ct[cc][:], ct[cc][:], rs[:], ALU.mult, reads=[tct[cc], trs], w=[tct[cc]])
                    self.act(mixT[:, cc, pos0:pos0 + GT], ct[cc][:], AF.Silu, reads=[tct[cc], tcs], pw=[tmix],
                             scale=lng[:, cc:cc + 1], bias=lnb[:, cc:cc + 1])

            combos = na_combos()
            NCB = len(combos)
            MT = self.sb(st, [128, NCB, 6, 128], BF16, "MT")
            tMT = Tok("MT")
            mst = [self.sb(st, [128, 6, 128], F32, "mst") for _ in range(2)]
            tmst = [Tok("mst") for _ in range(2)]
            for ci, (d, pat) in enumerate(combos):
                s = ci % 2
                self.memset("pool", mst[s][:], -30000.0, w=[tmst[s]])
                for a in range(2):
                    for bq in range(2):
                        if not pat[a][bq]:
                            continue
                        delta = 2 * d + a - bq
                        src = D["rb_exp"][l, :, delta + 7, :, :].rearrange("h c q -> c h q")
                        self.dma("sp", mst[s][a * 64:(a + 1) * 64, :, bq * 64:(bq + 1) * 64], src, reads=[], pw=[tmst[s]])
                self.act(MT[:, ci, :, :], mst[s][:], AF.Exp, reads=[tmst[s]], pw=[tMT])
            self.tt("dve", MT[:].rearrange("p c h q -> p (c h) q"), MT[:].rearrange("p c h q -> p (c h) q"),
                    c["cmask_b"][:].unsqueeze(1).to_broadcast([128, NCB * 6, 128]), ALU.mult, reads=[tMT], w=[tMT])

            sp_ = [self.ps(st, [128, 4, 128], F32, "sT") for _ in range(2)]
            tsp = [Tok("sT") for _ in range(2)]
            op_ = [self.ps(st, [128, 4, 128], F32, "oacc") for _ in range(2)]
            top_ = [Tok("oacc") for _ in range(2)]
            trp = self.ps(st, [128, 8, 128], BF16, "trp")
            ttrp = Tok("trp")
            pT = [self.sb(st, [128, 3, 128], BF16, "pT") for _ in range(3)]
            tpT = [Tok("pT") for _ in range(3)]
            rc = [self.sb(st, [128, 3], F32, "rc") for _ in range(2)]
            trc = [Tok("rc") for _ in range(2)]
            otm = [self.sb(st, [128, 768], BF16, "otm") for _ in range(2)]
            totm = [Tok("otm") for _ in range(2)]
            cnt = {"u": 0, "h": 0}

            att = os.environ.get("ATT", "qpnt")

            def attn_half(qt, heads, keylist, ot, tot_, ocol0):
                hi = cnt["h"] % 2
                cnt["h"] += 1
                nk = len(keylist)

                def qk(ki):
                    kaps, vaps, mask = keylist[ki]
                    u = cnt["u"]
                    cnt["u"] += 1
                    si, pi = u % 2, u % 3
                    for h3 in range(3):
                        self.mm(sp_[si][:, h3, :], kaps[h3], heads[h3], start=True, stop=True,
                                reads=[BB["tqna"], BB["tkna"], BB["tqg"], BB["tkg"]],
                                w=[tsp[si]] if h3 == 0 else (), pw=() if h3 == 0 else [tsp[si]])
                    self.act(pT[pi][:], sp_[si][:, 0:3, :], AF.Exp, reads=[tsp[si]], w=[tpT[pi], tsp[si]])
                    if mask is not None:
                        self.tt("dve", pT[pi][:], pT[pi][:], mask, ALU.mult, reads=[tpT[pi], tMT], w=[tpT[pi]])
                    return pi

                pend = qk(0)
                for ki in range(nk):
                    nxt = qk(ki + 1) if ki + 1 < nk else None
                    pi = pend
                    vaps = keylist[ki][1]
                    for h3 in range(3 if "p" in att else 0):
                        first = (ki == 0 and h3 == 0)
                        self.mm(op_[hi][:, h3, 0:65], pT[pi][:, h3, :], vaps[h3], start=first, stop=(ki == nk - 1), sgc=True,
                                reads=[tpT[pi], BB["tvv"]], w=[top_[hi]] if first else (), pw=() if first else [top_[hi]])
                    pend = nxt
                if "n" not in att:
                    return
                self.recip(rc[hi][:], op_[hi][:, 0:3, 64], reads=[top_[hi]], w=[trc[hi], top_[hi]])
                self.tt("dve", ot[:, ocol0:ocol0 + 192].rearrange("p (h d) -> p h d", d=64), op_[hi][:, 0:3, 0:64],
                        rc[hi][:].unsqueeze(2).to_broadcast([128, 3, 64]), ALU.mult, reads=[top_[hi], trc[hi]], w=[top_[hi]], pw=[tot_])

            vv, qna, qg = BB["vv"], BB["qna"], BB["qg"]
            knaAB = [BB["knaA"], BB["knaB"]]
            kgAB = [BB["kgA"], BB["kgB"]]
            nq = 18 if do_ctx else 16
            if "a" not in parts:
                nq = 0
            if "1" in parts:
                nq = 1
            for qt in range(nq):
                oi = qt % 2
                q0 = qt * 128
                if qt < 16:
                    loc = na_tiles(qt)
                    na_keys = [(m, combos.index((m - qt, pat))) for (m, pat) in loc] + [(16, None), (17, None)]
                    g_keys = list(range(18))
                else:
                    na_keys = [(16, None), (17, None)]
                    g_keys = [16, 17]
                for half in range(2):
                    hs = [3 * half + i for i in range(3)]
                    heads = [qna[:, h // 2, q0:q0 + 128] for h in hs]
                    kl = []
                    for (m, cidx) in na_keys:
                        kaps = [knaAB[h % 2][:, h // 2, m * 128:(m + 1) * 128] for h in hs]
                        vaps = [vv[:, m, h, :] for h in hs]
                        mask = None if cidx is None else MT[:, cidx, 3 * half:3 * half + 3, :]
                        kl.append((kaps, vaps, mask))
                    attn_half(qt, heads, kl, otm[oi], totm[oi], half * 192)
                for kvh in range(2):
                    heads = [qg[:, cc, q0:q0 + 128] for cc in range(3)]
                    kl = []
                    for m in g_keys:
                        kaps = [kgAB[kvh][:, m * 128:(m + 1) * 128]] * 3
                        vaps = [vv[:, m, 6 + kvh, :]] * 3
                        kl.append((kaps, vaps, None))
                    attn_half(qt, heads, kl, otm[oi], totm[oi], 384 + kvh * 192)
                if "t" not in att:
                    continue
                for j in range(6):
                    self.tr(trp[:, j, :], otm[oi][:, j * 128:(j + 1) * 128], c["ident_b"][:], reads=[totm[oi]],
                            w=[ttrp] if j == 0 else (), pw=() if j == 0 else [ttrp])
                self.cp("act", mixT[:, 2:8, q0:q0 + 128], trp[:, 0:6, :], reads=[ttrp], w=[ttrp], pw=[tmix])
        self.P.barrier()

    def stage_s4(self, l, b, BB, do_ctx):
        D, c = self.D, self.c
        with ExitStack() as st:
            wo = self.sb(st, [128, 8, 1024], BF16, "wo")
            two = self.load_cast(st, wo, D["w_out"][l].rearrange("(k p) c -> p k c", p=128), 1024, 256)
            xg = [self.sb(st, [128, 8, GT], F32, "xg4") for _ in range(2)]
            txg = [Tok("xg4") for _ in range(2)]
            opp = [self.ps(st, [128, 512], F32, "opp") for _ in range(4)]
            topp = [Tok("opp") for _ in range(4)]
            xTv = self.xT_d.rearrange("k p t -> p k t")
            ngr = NGB if do_ctx else 8
            n = 0
            for g in range(ngr):
                s = g % 2
                t0 = b * TB + g * GT
                pos0 = g * GT
                r = b if g < 8 else 2
                self.dma("sp", xg[s][:], xTv[:, :, t0:t0 + GT], reads=[], w=[txg[s]])
                for kd in range(8):
                    i = n % 4
                    n += 1
                    ap, tk = opp[i][:, 0:GT], topp[i]
                    for kf in range(8):
                        self.mm(ap, wo[:, kf, kd * 128:(kd + 1) * 128], BB["mixT"][:, kf, pos0:pos0 + GT],
                                start=(kf == 0), stop=(kf == 7), reads=[two, BB["tmix"]],
                                w=[tk] if kf == 0 else (), pw=() if kf == 0 else [tk])
                    self.stt(xg[s][:, kd, :], ap, c["modT"][:, 16 + kd, r:r + 1], xg[s][:, kd, :], ALU.mult, ALU.add,
                             reads=[tk, txg[s]], w=[tk], pw=[txg[s]])
                self.dma("pool", xTv[:, :, t0:t0 + GT], xg[s][:], reads=[txg[s]], w=[])
        self.P.barrier()

    def stage_p1(self, l, ntiles=NTILE, tile0=0, gbase=0):
        D, c = self.D, self.c
        with ExitStack() as st:
            wq = self.sb(st, [128, 8, 2048], BF16, "wq")
            twq = self.load_cast(st, wq, D["w_q"][l].rearrange("(k p) c -> p k c", p=128), 2048, 256)
            keysT = self.sb(st, [128, 16, 128], F32, "keysT")
            tkeys = Tok("keysT")
            self.dma("pool", keysT[:], D["keysT"][l], reads=[], w=[tkeys])
            NBUF = self.norm_bufs(st, 128)
            xt = [self.sb(st, [128, 8, 128], F32, "xt") for _ in range(2)]
            txt = [Tok("xt") for _ in range(2)]
            xc = [self.sb(st, [128, 8, 128], BF16, "xc") for _ in range(2)]
            txc = [Tok("xc") for _ in range(2)]
            qT = self.sb(st, [128, 16, 128], F32, "qT")
            tqT = Tok("qT")
            s_sb = self.sb(st, [128, 16, 128], F32, "s_sb")
            ts_sb = Tok("s_sb")
            tmpk = [self.sb(st, [128, 128], F32, "tmpk") for _ in range(2)]
            ttmpk = [Tok("tmpk") for _ in range(2)]
            top = self.sb(st, [128, 16, 16], F32, "top")
            ttop = Tok("top")
            idx = self.sb(st, [128, 16, 16], U32, "idx")
            tidx = Tok("idx")
            idxf = self.sb(st, [128, 2, 8, 16], F32, "idxf")
            tidxf = Tok("idxf")
            cs = self.sb(st, [128, 8, 256], F32, "cs")
            tcs = Tok("cs")
            tmp2 = [self.sb(st, [128, 256], F32, "tmp2") for _ in range(2)]
            ttmp2 = [Tok("tmp2") for _ in range(2)]
            m8 = self.sb(st, [128, 8, 16], F32, "m8")
            tm8 = Tok("m8")
            ee = self.sb(st, [128, 8, 256], F32, "ee")
            tee = Tok("ee")
            msk = self.sb(st, [128, 8, 256], F32, "msk")
            tmsk = Tok("msk")
            zz = self.sb(st, [128, 8], F32, "zz")
            tzz = Tok("zz")
            idxT = self.sb(st, [128, 2, 128], BF16, "idxT")
            tidxT = Tok("idxT")
            Tw = self.sb(st, [128, 16, 128], BF16, "Tw")
            tTw = Tok("Tw")
            G_t = self.sb(st, [128, 128, 128], BF16, "G_t")
            tG_t = Tok("G_t")
            A4 = [self.sb(st, [128, 4, 128], BF16, "A4") for _ in range(2)]
            tA4 = [Tok("A4") for _ in range(2)]
            B4 = [self.sb(st, [128, 4, 128], BF16, "B4") for _ in range(2)]
            tB4 = [Tok("B4") for _ in range(2)]
            W4 = [self.sb(st, [128, 4, 8, 16], BF16, "W4") for _ in range(2)]
            tW4 = [Tok("W4") for _ in range(2)]
            R4 = [self.sb(st, [128, 4, 128], BF16, "R4") for _ in range(2)]
            tR4 = [Tok("R4") for _ in range(2)]
            qsp = [self.ps(st, [128, 4, 128], F32, "qsp") for _ in range(2)]
            tqsp = [Tok("qsp") for _ in range(2)]
            trq, ttrq = qsp, tqsp
            Rp = [self.ps(st, [128, 4, 128], F32, "Rp") for _ in range(2)]
            tRp = [Tok("Rp") for _ in range(2)]
            Gp = [self.ps(st, [128, 4, 128], F32, "Gp") for _ in range(2)]
            tGp = [Tok("Gp") for _ in range(2)]
            xTv = self.xT_d.rearrange("k p t -> p k t")
            xcv = self.xc_d.rearrange("k p t -> p k t")
            Gv = self.G_d.rearrange("(a i) t -> i a t", i=128)
            nq = 0
            for ti in range(tile0, tile0 + ntiles):
                s = ti % 2
                t0 = ti * 128
                bb = ti // (TB // 128)
                r = bb if (ti % (TB // 128)) < 16 else 2
                self.dma("sp", xt[s][:], xTv[:, :, t0:t0 + 128], reads=[], w=[txt[s]])
                self.norm_mod(NBUF, xt[s][:], txt[s], xc[s], txc[s], c["A2"], 24, r, 128, "p1")
                self.dma("pool", xcv[:, :, t0:t0 + 128], xc[s][:], reads=[txc[s]], w=[])
                for bg in range(4):
                    i = nq % 2
                    nq += 1
                    for bl in range(4):
                        blk = bg * 4 + bl
                        for k in range(8):
                            first = (bl == 0 and k == 0)
                            self.mm(qsp[i][:, bl, :], wq[:, k, blk * 128:(blk + 1) * 128], xc[s][:, k, :],
                                    start=(k == 0), stop=(k == 7), reads=[twq, txc[s]],
                                    w=[tqsp[i]] if first else (), pw=() if first else [tqsp[i]])
                    self.cp("act" if bg % 2 == 0 else "dve", qT[:, bg * 4:(bg + 1) * 4, :], qsp[i][:], reads=[tqsp[i]],
                            w=([tqT] if bg == 0 else []) + [tqsp[i]], pw=() if bg == 0 else [tqT])
                for bg in range(4):
                    i = nq % 2
                    nq += 1
                    for bl in range(4):
                        blk = bg * 4 + bl
                        self.mm(qsp[i][:, bl, :], qT[:, blk, :], keysT[:, blk, :], start=True, stop=True,
                                reads=[tqT, tkeys], w=[tqsp[i]] if bl == 0 else (), pw=() if bl == 0 else [tqsp[i]])
                    self.cp("act" if bg % 2 == 0 else "dve", s_sb[:, bg * 4:(bg + 1) * 4, :], qsp[i][:], reads=[tqsp[i]],
                            w=([ts_sb] if bg == 0 else []) + [tqsp[i]], pw=() if bg == 0 else [ts_sb])
                for blk in range(16):
                    j = blk % 2
                    first = blk == 0
                    wtop = dict(w=[ttop]) if first else dict(pw=[ttop])
                    widx = dict(w=[tidx]) if first else dict(pw=[tidx])
                    self.P.op("dve", (lambda e, blk=blk: e.max(out=top[:, blk, 0:8], in_=s_sb[:, blk, :])),
                              reads=[ts_sb], writes=wtop.get("w", ()), pw=wtop.get("pw", ()))
                    self.P.op("dve", (lambda e, blk=blk: e.max_index(out=idx[:, blk, 0:8], in_max=top[:, blk, 0:8],
                                                                    in_values=s_sb[:, blk, :])),
                              reads=[ts_sb, ttop], writes=widx.get("w", ()), pw=widx.get("pw", ()))
                    self.P.op("dve", (lambda e, blk=blk, j=j: e.match_replace(out=tmpk[j][:], in_to_replace=top[:, blk, 0:8],
                                                                            in_values=s_sb[:, blk, :], imm_value=NEG)),
                              reads=[ts_sb, ttop], writes=[ttmpk[j]])
                    self.P.op("dve", (lambda e, blk=blk, j=j: e.max(out=top[:, blk, 8:16], in_=tmpk[j][:])),
                              reads=[ttmpk[j]], pw=[ttop])
                    self.P.op("dve", (lambda e, blk=blk, j=j: e.max_index(out=idx[:, blk, 8:16], in_max=top[:, blk, 8:16],
                                                                         in_values=tmpk[j][:])),
                              reads=[ttmpk[j], ttop], pw=[tidx])
                self.cp("dve", idxf[:].rearrange("p q h i -> p h q i"), idx[:].rearrange("p (h q) i -> p h q i", q=2),
                        reads=[tidx], w=[tidxf])
                top4 = top[:].rearrange("p (h q) i -> p h q i", q=2)
                self.tt("dve", cs[:].rearrange("p h (i j) -> p h i j", j=16),
                        top4[:, :, 0, :].unsqueeze(3).to_broadcast([128, 8, 16, 16]),
                        top4[:, :, 1, :].unsqueeze(2).to_broadcast([128, 8, 16, 16]), ALU.add, reads=[ttop], w=[tcs])
                for h in range(8):
                    j = h % 2
                    self.P.op("dve", (lambda e, h=h: e.max(out=m8[:, h, 0:8], in_=cs[:, h, :])), reads=[tcs],
                              writes=[tm8] if h == 0 else (), pw=() if h == 0 else [tm8])
                    self.P.op("dve", (lambda e, h=h, j=j: e.match_replace(out=tmp2[j][:], in_to_replace=m8[:, h, 0:8],
                                                                        in_values=cs[:, h, :], imm_value=NEG)),
                              reads=[tcs, tm8], writes=[ttmp2[j]])
                    self.P.op("dve", (lambda e, h=h, j=j: e.max(out=m8[:, h, 8:16], in_=tmp2[j][:])), reads=[ttmp2[j]], pw=[tm8])
                self.tt("dve", ee[:], cs[:], m8[:, :, 0:1].to_broadcast([128, 8, 256]), ALU.subtract, reads=[tcs, tm8], w=[tee])
                self.act(ee[:], ee[:], AF.Exp, reads=[tee], w=[tee])
                self.tt("dve", msk[:], cs[:], m8[:, :, 15:16].to_broadcast([128, 8, 256]), ALU.is_ge, reads=[tcs, tm8], w=[tmsk])
                self.tt("dve", ee[:], ee[:], msk[:], ALU.mult, reads=[tee, tmsk], w=[tee])
                self.P.op("dve", lambda e: e.tensor_reduce(out=zz[:], in_=ee[:], axis=AX.X, op=ALU.add), reads=[tee], writes=[tzz])
                self.recip(zz[:], zz[:], reads=[tzz], w=[tzz])
                Wc2 = msk[:].rearrange("p a b -> p (a b)").rearrange("p (i h j) -> p i h j", i=16, h=8)
                self.tt("dve", Wc2.rearrange("p i h j -> p h i j"), ee[:].rearrange("p h (i j) -> p h i j", j=16),
                        zz[:].unsqueeze(2).unsqueeze(3).to_broadcast([128, 8, 16, 16]), ALU.mult, reads=[tee, tzz], w=[tmsk])
                for q2 in range(2):
                    self.tr(trq[0][:, q2, :], idxf[:, q2, :, :].rearrange("p h i -> p (h i)"), c["ident_f"][:], reads=[tidxf],
                            w=[ttrq[0]] if q2 == 0 else (), pw=() if q2 == 0 else [ttrq[0]])
                self.cp("act", idxT[:], trq[0][:, 0:2, :], reads=[ttrq[0]], w=[tidxT, ttrq[0]])
                for ig in range(4):
                    pi = (ig + 1) % 2
                    for i4 in range(4):
                        i = ig * 4 + i4
                        self.tr(trq[pi][:, i4, :], Wc2[:, i, :, :].rearrange("p h j -> p (h j)"), c["ident_f"][:], reads=[tmsk],
                                w=[ttrq[pi]] if i4 == 0 else (), pw=() if i4 == 0 else [ttrq[pi]])
                    self.cp("act" if ig % 2 == 0 else "dve", Tw[:, ig * 4:(ig + 1) * 4, :], trq[pi][:], reads=[ttrq[pi]],
                            w=([tTw] if ig == 0 else []) + [ttrq[pi]], pw=() if ig == 0 else [tTw])
                for qd in range(32):
                    u = qd % 2
                    tq0 = qd * 4
                    self.tt("dve", A4[u][:], c["iota_b"][:].unsqueeze(1).to_broadcast([128, 4, 128]),
                            idxT[:, 0, tq0:tq0 + 4].unsqueeze(2).to_broadcast([128, 4, 128]), ALU.is_equal,
                            reads=[tidxT], w=[tA4[u]])
                    self.tt("dve", B4[u][:], c["iota_b"][:].unsqueeze(1).to_broadcast([128, 4, 128]),
                            idxT[:, 1, tq0:tq0 + 4].unsqueeze(2).to_broadcast([128, 4, 128]), ALU.is_equal,
                            reads=[tidxT], w=[tB4[u]])
                    self.tt("pool", W4[u][:], c["bmask_b"][:].unsqueeze(1).unsqueeze(3).to_broadcast([128, 4, 8, 16]),
                            Tw[:, :, tq0:tq0 + 4].rearrange("p i t -> p t i").unsqueeze(2).to_broadcast([128, 4, 8, 16]),
                            ALU.mult, reads=[tTw], w=[tW4[u]])
                    for t4 in range(4):
                        self.mm(Rp[u][:, t4, :], W4[u][:, t4, :, :].rearrange("p h i -> p (h i)"), B4[u][:, t4, :],
                                start=True, stop=True, reads=[tW4[u], tB4[u]],
                                w=[tRp[u]] if t4 == 0 else (), pw=() if t4 == 0 else [tRp[u]])
                    self.cp("act", R4[u][:], Rp[u][:], reads=[tRp[u]], w=[tR4[u], tRp[u]])
                    for t4 in range(4):
                        self.mm(Gp[u][:, t4, :], R4[u][:, t4, :], A4[u][:, t4, :], start=True, stop=True,
                                reads=[tR4[u], tA4[u]], w=[tGp[u]] if t4 == 0 else (), pw=() if t4 == 0 else [tGp[u]])
                    self.cp("act", G_t[:, :, tq0:tq0 + 4], Gp[u][:].rearrange("p t i -> p i t"),
                            reads=[tGp[u]], w=([tG_t] if qd == 0 else []) + [tGp[u]], pw=() if qd == 0 else [tG_t])
                for a8 in range(8):
                    self.dma("pool", Gv[:, a8 * 16:(a8 + 1) * 16, t0 - gbase:t0 - gbase + 128], G_t[:, a8 * 16:(a8 + 1) * 16, :],
                             reads=[tG_t], w=[])
        self.P.barrier()

    def stage_p0(self, l):
        D = self.D
        with ExitStack() as st:
            ust = [self.sb(st, [128, 8, 512], F32, "ust0") for _ in range(3)]
            tust = [Tok("ust0") for _ in range(3)]
            ubf = [self.sb(st, [128, 8, 512], BF16, "ubf0") for _ in range(3)]
            tubf = [Tok("ubf0") for _ in range(3)]
            uv = D["uT"][l].rearrange("(k p) e -> p k e", p=128)
            vview = D["pv"][l].rearrange("(a e) d -> e a d", e=128)
            ubv = self.ub_d.rearrange("(k p) e -> p k e", p=128)
            vbv = self.vb_d.rearrange("(a e) d -> e a d", e=128)
            engs = ("act", "dve", "act", "dve", "act", "pool")
            n = 0
            for pc in range(32):
                for which in range(2):
                    s = n % 3
                    if which == 0:
                        src = uv[:, :, pc * 512:(pc + 1) * 512]
                        dst = ubv[:, :, pc * 512:(pc + 1) * 512]
                        sview = ust[s][:]
                        bview = ubf[s][:]
                    else:
                        src = vview[:, pc * 4:(pc + 1) * 4, :]
                        dst = vbv[:, pc * 4:(pc + 1) * 4, :]
                        sview = ust[s][:].rearrange("p k e -> p (k e)").rearrange("p (a d) -> p a d", a=4)
                        bview = ubf[s][:].rearrange("p k e -> p (k e)").rearrange("p (a d) -> p a d", a=4)
                    self.dma("sp", sview, src, reads=[], w=[tust[s]])
                    self.cp(engs[n % 6], bview, sview, reads=[tust[s]], w=[tubf[s]])
                    self.dma("pool" if n % 2 == 0 else "act", dst, bview, reads=[tubf[s]], w=[])
                    n += 1
        self.P.barrier()

    def stage_p2(self, l, nsg=2, sg0=0, gbase=0):
        D, c = self.D, self.c
        NSUB = 3
        SG = NSUB * PG
        NTL = SG // 128
        with ExitStack() as st:
            ubf = [self.sb(st, [128, 8, 512], BF16, "ubf") for _ in range(3)]
            tubf = [Tok("ubf") for _ in range(3)]
            vbf = [self.sb(st, [128, 4, 1024], BF16, "vbf") for _ in range(3)]
            tvbf = [Tok("vbf") for _ in range(3)]
            gpc = [self.sb(st, [128, 4, SG], BF16, "gpc") for _ in range(3)]
            tgpc = [Tok("gpc") for _ in range(3)]
            xcg = self.sb(st, [128, 8, SG], BF16, "xcg")
            txcg = Tok("xcg")
            a_sb = [self.sb(st, [128, PG], BF16, "a_sb") for _ in range(2)]
            ta_sb = [Tok("a_sb") for _ in range(2)]
            ga = [self.sb(st, [128, PG], BF16, "ga") for _ in range(2)]
            tga = [Tok("ga") for _ in range(2)]
            acc = self.sb(st, [128, NTL, 1024], F32, "acc")
            tacc = [Tok("acc") for _ in range(NTL)]
            xt = [self.sb(st, [128, 8, 128], F32, "xt2") for _ in range(2)]
            txt = [Tok("xt2") for _ in range(2)]
            app = [self.ps(st, [128, 512], F32, "app") for _ in range(2)]
            tapp = [Tok("app") for _ in range(2)]
            oacc = [self.ps(st, [128, 1024], F32, "oacc2") for _ in range(3)]
            toacc = [Tok("oacc2") for _ in range(3)]
            uv = self.ub_d.rearrange("(k p) e -> p k e", p=128)
            vview = self.vb_d.rearrange("(a e) d -> e a d", e=128)
            Gv = self.G_d.rearrange("(a i) t -> i a t", i=128)
            xTv = self.xT_d.rearrange("k p t -> p k t")
            xcv = self.xc_d.rearrange("k p t -> p k t")
            NPC = 32
            self._npc = 0
            self._nch = 0
            nch = 0
            ntile = 0
            for sg in range(sg0, sg0 + nsg):
                tok0 = sg * SG
                self.dma("sp", xcg[:], xcv[:, :, tok0:tok0 + SG], reads=[], w=[txcg])
                steps = [(pc, sub, cc) for pc in range(NPC) for sub in range(NSUB) for cc in range(4)]
                pbuf = {}

                def u_phase(step):
                    pc, sub, cc = step
                    if sub == 0 and cc == 0:
                        s = self._npc % 3
                        self._npc += 1
                        pbuf[pc] = s
                        e0 = pc * 512
                        self.dma("sp", ubf[s][:], uv[:, :, e0:e0 + 512], reads=[], w=[tubf[s]])
                        self.dma("sp", vbf[s][:], vview[:, pc * 4:(pc + 1) * 4, :], reads=[], w=[tvbf[s]])
                        self.dma("sp", gpc[s][:], Gv[:, pc * 4:(pc + 1) * 4, tok0 - gbase:tok0 - gbase + SG], reads=[], w=[tgpc[s]])
                    s = pbuf[pc]
                    c0 = sub * PG
                    i = self._nch % 2
                    self._nch += 1
                    for k in range(8):
                        self.mm(app[i][:, 0:PG], ubf[s][:, k, cc * 128:(cc + 1) * 128], xcg[:, k, c0:c0 + PG],
                                start=(k == 0), stop=(k == 7), reads=[tubf[s], txcg],
                                w=[tapp[i]] if k == 0 else (), pw=() if k == 0 else [tapp[i]])
                    self.act(a_sb[i][:], app[i][:, 0:PG], AF.Gelu, reads=[tapp[i]], w=[ta_sb[i], tapp[i]])
                    self.tt("dve", ga[i][:], a_sb[i][:], gpc[s][:, cc, c0:c0 + PG], ALU.mult,
                            reads=[ta_sb[i], tgpc[s]], w=[tga[i]])
                    return i

                def v_phase(step, i):
                    pc, sub, cc = step
                    s = pbuf[pc]
                    for t3 in range(3):
                        for hf in range(2):
                            self.mm(oacc[t3][:, hf * 512:(hf + 1) * 512], ga[i][:, t3 * 128:(t3 + 1) * 128],
                                    vbf[s][:, cc, hf * 512:(hf + 1) * 512], start=(cc == 0), stop=(cc == 3),
                                    reads=[tga[i], tvbf[s]],
                                    w=[toacc[t3]] if (cc == 0 and hf == 0) else (),
                                    pw=() if (cc == 0 and hf == 0) else [toacc[t3]])
                    if cc == 3:
                        for t3 in range(3):
                            tl = sub * 3 + t3
                            if pc == 0:
                                self.cp("dve" if t3 != 1 else "act", acc[:, tl, :], oacc[t3][:], reads=[toacc[t3]],
                                        w=[tacc[tl], toacc[t3]])
                            else:
                                self.tt("dve", acc[:, tl, :], oacc[t3][:], acc[:, tl, :], ALU.add,
                                        reads=[toacc[t3], tacc[tl]], w=[tacc[tl], toacc[t3]])

                pend = u_phase(steps[0])
                for n_ in range(len(steps)):
                    nxt = u_phase(steps[n_ + 1]) if n_ + 1 < len(steps) else None
                    v_phase(steps[n_], pend)
                    pend = nxt
                nch = self._nch
                for tl in range(NTL):
                    ti = sg * NTL + tl
                    s2 = ntile % 2
                    ntile += 1
                    t0 = ti * 128
                    bb = ti // (TB // 128)
                    r = bb if (ti % (TB // 128)) < 16 else 2
                    self.dma("sp", xt[s2][:], xTv[:, :, t0:t0 + 128], reads=[], w=[txt[s2]])
                    for hf in range(2):
                        i = self._nch % 2
                        self._nch += 1
                        for q in range(4):
                            k = hf * 4 + q
                            self.tr(app[i][:, q * 128:(q + 1) * 128], acc[:, tl, k * 128:(k + 1) * 128], c["ident_f"][:],
                                    reads=[tacc[tl]], w=[tapp[i]] if q == 0 else (), pw=() if q == 0 else [tapp[i]])
                        for q in range(4):
                            k = hf * 4 + q
                            self.stt(xt[s2][:, k, :], app[i][:, q * 128:(q + 1) * 128], c["modT"][:, 40 + k, r:r + 1],
                                     xt[s2][:, k, :], ALU.mult, ALU.add, reads=[tapp[i], txt[s2]], w=[tapp[i]], pw=[txt[s2]])
                    self.dma("pool", xTv[:, :, t0:t0 + 128], xt[s2][:], reads=[txt[s2]], w=[])
        self.P.barrier()

    def stage_final(self):
        D, c = self.D, self.c
        with ExitStack() as st:
            xt = [self.sb(st, [128, 8, 128], F32, "xtf") for _ in range(2)]
            txt = [Tok("xtf") for _ in range(2)]
            sq = self.sb(st, [128, 8, 128], BF16, "sqf")
            tsq = Tok("sqf")
            ssp = self.ps(st, [128, 512], F32, "sspf")
            tssp = Tok("sspf")
            sd = self.sb(st, [128, 128], F32, "sdf")
            tsd = Tok("sdf")
            yt = [self.sb(st, [128, 8, 128], F32, "ytf") for _ in range(2)]
            tyt = [Tok("ytf") for _ in range(2)]
            trp = [self.ps(st, [128, 1024], F32, "trpf") for _ in range(2)]
            ttrp = [Tok("trpf") for _ in range(2)]
            yo = [self.sb(st, [128, 1024], F32, "yo") for _ in range(2)]
            tyo = [Tok("yo") for _ in range(2)]
            xTv = self.xT_d.rearrange("k p t -> p k t")
            n = 0
            for b in range(NB):
                for ti in range(16):
                    s = n % 2
                    n += 1
                    t0 = b * TB + ti * 128
                    self.dma("sp", xt[s][:], xTv[:, :, t0:t0 + 128], reads=[], w=[txt[s]])
                    self.act(sq[:], xt[s][:], AF.Square, reads=[txt[s]], w=[tsq])
                    for k in range(8):
                        self.mm(ssp[:, 0:128], c["ones_b"][:], sq[:, k, :], start=(k == 0), stop=(k == 7), reads=[tsq],
                                w=[tssp] if k == 0 else (), pw=() if k == 0 else [tssp])
                    self.act(sd[:], ssp[:, 0:128], AF.Sqrt, reads=[tssp], w=[tsd, tssp], scale=1.0 / 1024.0, bias=c["eps"][:])
                    self.recip(sd[:], sd[:], reads=[tsd], w=[tsd])
                    self.tt("dve", yt[s][:], xt[s][:], sd[:].unsqueeze(1).to_broadcast([128, 8, 128]), ALU.mult,
                            reads=[txt[s], tsd], w=[tyt[s]])
                    self.tt("pool", yt[s][:], yt[s][:], c["fng"][:].unsqueeze(2).to_broadcast([128, 8, 128]), ALU.mult,
                            reads=[tyt[s]], w=[tyt[s]])
                    for k in range(8):
                        self.tr(trp[s][:, k * 128:(k + 1) * 128], yt[s][:, k, :], c["ident_f"][:], reads=[tyt[s]],
                                w=[ttrp[s]] if k == 0 else (), pw=() if k == 0 else [ttrp[s]])
                    self.cp("act", yo[s][:], trp[s][:], reads=[ttrp[s]], w=[tyo[s], ttrp[s]])
                    self.dma("pool", self.out[b, ti * 128:(ti + 1) * 128, :], yo[s][:], reads=[tyo[s]], w=[])

    def batch_bufs(self, st):
        BB = {}
        BB["qna"] = self.sb(st, [128, 3, TB], BF16, "qna")
        BB["knaA"] = self.sb(st, [128, 3, TB], BF16, "knaA")
        BB["knaB"] = self.sb(st, [128, 3, TB], BF16, "knaB")
        BB["qg"] = self.sb(st, [128, 3, TB], BF16, "qg")
        BB["kgA"] = self.sb(st, [128, TB], BF16, "kgA")
        BB["kgB"] = self.sb(st, [128, TB], BF16, "kgB")
        BB["vv"] = self.sb(st, [128, TB // 128, 8, 65], BF16, "vv")
        BB["ubuf"] = self.sb(st, [128, 2, UW], BF16, "ubuf")
        for k in ["tqna", "tkna", "tqg", "tkg", "tvv", "tubuf", "tmix"]:
            BB[k] = Tok(k)
        self.memset("pool", BB["ubuf"][:], 0.0, w=[BB["tubuf"]])
        self.memset("pool", BB["knaA"][64:128, :, :], 0.0, w=[BB["tkna"]])
        self.memset("dve", BB["knaB"][0:64, :, :], 0.0, pw=[BB["tkna"]])
        self.memset("pool", BB["kgA"][64:128, :], 0.0, w=[BB["tkg"]])
        self.memset("dve", BB["kgB"][0:64, :], 0.0, pw=[BB["tkg"]])
        self.memset("pool", BB["vv"][:, :, :, 64:65], 1.0, w=[BB["tvv"]])
        return BB

    def build(self, depth=DEPTH, stages="all"):
        self.declare()
        with ExitStack() as st0:
            self.consts(st0)
            self.P.barrier()
            self._build_body(depth, stages)
            self.P.emit()
        return self.nc

    def _build_body(self, depth, stages):
        if stages == "c":
            return
        self.stage_s0()
        if stages == "s0":
            return
        for l in range(depth):
            do_ctx = l < DEPTH - 1
            self.stage_s1(l)
            if stages == "s1":
                return
            if stages == "pp":
                self.stage_p1(l, ntiles=3)
                self.stage_p0(l)
                self.stage_p2(l, nsg=1)
                return
            for b in range(NB):
                with ExitStack() as stb:
                    BB = self.batch_bufs(stb)
                    self.stage_s2(l, b, stb, BB)
                    if stages == "s2":
                        return
                    with ExitStack() as stm:
                        BB["mixT"] = self.sb(stm, [128, 8, TB], BF16, "mixT")
                        self.stage_s3(l, b, BB, do_ctx)
                        if stages == "s3":
                            return
                        self.stage_s4(l, b, BB, do_ctx)
            if stages == "mix":
                return
            self.stage_p0(l)
            for hb in range(2):
                self.stage_p1(l, ntiles=NTILE // 2, tile0=hb * (NTILE // 2), gbase=hb * (NT // 2))
                self.stage_p2(l, nsg=2, sg0=hb * 2, gbase=hb * (NT // 2))
            if stages == "l0":
                return
        self.stage_final()


OFF_A_VAL, OFF_A_GATE, OFF_NA_Q, OFF_G_Q = 0, 256, 512, 896
OFF_NA_K, OFF_NA_V, OFF_G_K, OFF_G_V = 1280, 1664, 2048, 2176


def _rope_tables():
    t = np.arange(TL, dtype=np.int32)
    row = (t // 64).astype(np.float32)
    col = (t % 64).astype(np.float32)
    n_freq = 16
    inv_freq = (np.float32(10000.0) ** (-np.arange(n_freq, dtype=np.float32) / np.float32(n_freq))).astype(np.float32)
    ang = np.concatenate([row[:, None] * inv_freq, col[:, None] * inv_freq], axis=-1).astype(np.float32)
    cos = np.cos(ang).astype(np.float32)
    sin = np.sin(ang).astype(np.float32)
    C = np.ones((128, TB), np.float32)
    S = np.zeros((128, TB), np.float32)
    for p in range(128):
        d = p % 64
        if d < 32:
            C[p, :TL] = cos[:, d]
            S[p, :TL] = -sin[:, d]
        else:
            C[p, :TL] = cos[:, d - 32]
            S[p, :TL] = sin[:, d - 32]
    return C, S


def _consts():
    C, S = _rope_tables()
    cols = np.arange(64)
    c0 = np.clip(cols - 8, 0, 48)
    cm = np.zeros((64, 64), np.float32)
    for c in range(64):
        cm[c0[c]:c0[c] + 16, c] = 1.0
    colmask2 = np.tile(cm, (2, 2)).astype(np.float32)
    blockmask = np.zeros((128, 8), np.float32)
    for p in range(128):
        blockmask[p, p // 16] = 1.0
    blockones = np.zeros((128, 128), np.float32)
    blockones[:64, :64] = 1.0
    blockones[64:, 64:] = 1.0
    return dict(ropeC=C, ropeS=S, colmask2=colmask2, blockmask=blockmask, blockones=blockones)


def _prep_shared(inp):
    f = np.float32
    L = DEPTH
    sh = {}
    sh["w_ada"] = np.ascontiguousarray(inp["w_ada"], dtype=f)
    sh["b_adaT"] = np.ascontiguousarray(inp["b_ada"].reshape(L, 48, 128).transpose(0, 2, 1))
    sh["n1gT"] = np.ascontiguousarray(inp["norm1_g"].reshape(L, 8, 128).transpose(0, 2, 1))
    sh["n2gT"] = np.ascontiguousarray(inp["norm2_g"].reshape(L, 8, 128).transpose(0, 2, 1))
    sh["fngT"] = np.ascontiguousarray(inp["final_norm_g"].reshape(8, 128).T)
    w_in = inp["w_in"]
    hperm = [0, 3, 1, 4, 2, 5]
    gq_cols = np.concatenate([OFF_G_Q + h * 64 + np.arange(64) for h in hperm])
    swp = np.concatenate([np.arange(32, 64), np.arange(0, 32)])
    gq_sw = np.concatenate([OFF_G_Q + h * 64 + swp for h in hperm])
    gk_cols = OFF_G_K + np.arange(128)
    gk_sw = np.concatenate([OFF_G_K + h * 64 + swp for h in range(2)])
    fm_cols = np.concatenate([np.arange(OFF_A_VAL, OFF_A_VAL + 256), np.arange(OFF_A_GATE, OFF_A_GATE + 256),
                              np.arange(OFF_NA_Q, OFF_NA_Q + 384), gq_cols, np.arange(OFF_NA_K, OFF_NA_K + 384),
                              gk_cols, gq_sw, gk_sw])
    assert fm_cols.shape[0] == 2304
    sh["w_fm"] = np.ascontiguousarray(w_in[:, :, fm_cols])
    v_cols = np.concatenate([np.arange(OFF_NA_V, OFF_NA_V + 384), np.arange(OFF_G_V, OFF_G_V + 128)])
    sh["w_v"] = np.ascontiguousarray(w_in[:, :, v_cols])
    p = np.arange(128)
    d = p % 64
    dsw = (d + 32) % 64
    sh["gq2"] = np.ascontiguousarray(np.stack([inp["gqa_q_norm"][:, d], inp["gqa_q_norm"][:, dsw]], axis=-1))
    sh["gk2"] = np.ascontiguousarray(np.stack([inp["gqa_k_norm"][:, d], inp["gqa_k_norm"][:, dsw]], axis=-1))
    sh["conv_wT"] = np.ascontiguousarray(inp["conv_w"].reshape(L, 31, 2, 128).transpose(0, 3, 2, 1))
    for k_src, k_dst in (("conv_b", "conv_bT"), ("conv_ln_g", "conv_lngT"), ("conv_ln_b", "conv_lnbT")):
        sh[k_dst] = np.ascontiguousarray(inp[k_src].reshape(L, 2, 128).transpose(0, 2, 1))
    cp_ = np.arange(64)[:, None]
    c_ = np.arange(64)[None, :]
    jidx = np.clip(cp_ - c_ + 15, 0, 30)
    sh["rb_exp"] = np.ascontiguousarray(inp["na_rel_bias"][:, :, :, jidx])
    sh["w_out"] = np.ascontiguousarray(inp["w_out"], dtype=f)
    sh["w_q"] = np.ascontiguousarray(inp["peer_w_q"], dtype=f)
    sh["keysT"] = np.ascontiguousarray(inp["peer_keys"].reshape(L, 16, 128, 128).transpose(0, 3, 1, 2))
    sh["uT"] = np.ascontiguousarray(inp["peer_u"].transpose(0, 2, 1))
    sh["pv"] = np.ascontiguousarray(inp["peer_v"], dtype=f)
    sh.update(_consts())
    return sh


def _prep_core(inp, core):
    b0 = core * NB
    cc = np.concatenate([inp["c"][b0:b0 + NB], inp["c_ctx"][None, :]], axis=0)
    m = {}
    m["x"] = np.ascontiguousarray(inp["x"][b0:b0 + NB])
    m["ctx"] = np.ascontiguousarray(inp["ctx"][b0:b0 + NB])
    m["ccT"] = np.ascontiguousarray(cc.reshape(3, 8, 128).transpose(2, 1, 0))
    return m


_NC_CACHE = {}


def kernel(**inputs):
    inp = {k: np.asarray(v, dtype=np.float32) for k, v in inputs.items()}
    n_cores = inp["x"].shape[0] // NB
    if "nc" not in _NC_CACHE:
        nc = bass.Bass("TRN2", target_bir_lowering=False)
        kk = K(nc)
        kk.build()
        _NC_CACHE["nc"] = nc
    nc = _NC_CACHE["nc"]
    sh = _prep_shared(inp)
    in_maps = []
    for core in range(n_cores):
        m = dict(sh)
        m.update(_prep_core(inp, core))
        in_maps.append(m)
    res = run_bass_kernel_spmd(nc, in_maps, core_ids=list(range(n_cores)))
    outs = [np.asarray(r["out"]) for r in res.results]
    return np.concatenate(outs, axis=0).astype(np.float32)
```

```python
import numpy as np
import concourse.bass as bass
import concourse.mybir as mybir
from concourse.bass_utils import run_bass_kernel_spmd
from contextlib import ExitStack

F32 = mybir.dt.float32
BF16 = mybir.dt.bfloat16
U32 = mybir.dt.uint32
I32 = mybir.dt.int32
AF = mybir.ActivationFunctionType
ALU = mybir.AluOpType
AX = mybir.AxisListType


ALLTOK = []


class Tok:
    __slots__ = ("name", "writers", "readers", "base")

    def __init__(self, name):
        self.name = name
        self.writers = []
        self.readers = []
        self.base = []
        ALLTOK.append(self)


class Op:
    __slots__ = ("eng", "fn", "deps", "dma", "signal", "count", "sem", "val",
                 "prev_val", "idx", "barrier")

    def __init__(self, eng, fn, dma):
        self.eng = eng
        self.fn = fn
        self.dma = dma
        self.deps = []
        self.signal = False
        self.count = 0
        self.sem = None
        self.val = 0
        self.prev_val = 0
        self.barrier = False


ENGS = ("pe", "act", "dve", "pool", "sp")
NSLOT = 12


class Prog:
    def __init__(self, nc):
        self.nc = nc
        self.ops = []
        self.last = {e: None for e in ENGS}
        self.dma_outstanding = []

    def tok(self, name="t"):
        return Tok(name)

    def toks(self, name, n):
        return [Tok("%s%d" % (name, i)) for i in range(n)]

    def op(self, eng, fn, reads=(), writes=(), pw=(), dma=False):
        o = Op(eng, fn, dma)
        deps = []
        for t in reads:
            deps.extend(t.writers)
        for t in writes:
            deps.extend(t.writers)
            deps.extend(t.readers)
        for t in pw:
            if t.readers:
                t.base = list(t.readers) + list(t.writers)
                t.writers = []
                t.readers = []
            deps.extend(t.base)
        for t in reads:
            t.readers.append(o)
        for t in writes:
            t.writers = [o]
            t.readers = []
            t.base = [o]
        for t in pw:
            t.writers.append(o)
        o.idx = len(self.ops)
        seen = set()
        best = {}
        for d in deps:
            if d is o or id(d) in seen:
                continue
            seen.add(id(d))
            if d.dma:
                o.deps.append(d)
            else:
                b = best.get(d.eng)
                if b is None or d.idx > b.idx:
                    best[d.eng] = d
        o.deps.extend(best.values())
        self.ops.append(o)
        self.last[eng] = o
        return o

    def barrier(self):
        b = Op(None, None, False)
        b.barrier = True
        b.deps = [o for o in self.ops if False]
        b.idx = len(self.ops)
        self.ops.append(b)
        for t in ALLTOK:
            t.writers = []
            t.readers = []
            t.base = []

    def emit(self):
        nc = self.nc
        ops = self.ops
        last = {e: None for e in ENGS}
        dmas_since = []
        pending = {e: [] for e in ENGS}
        for o in ops:
            if o.barrier:
                extra = [x for x in last.values() if x is not None] + dmas_since
                for e in ENGS:
                    pending[e] = list(extra)
                dmas_since = []
                continue
            if pending[o.eng]:
                seen = set(id(d) for d in o.deps)
                for d in pending[o.eng]:
                    if id(d) not in seen and d is not o:
                        o.deps.append(d)
                pending[o.eng] = []
            last[o.eng] = o
            if o.dma:
                dmas_since.append(o)
        real = [o for o in ops if not o.barrier]
        for o in real:
            for d in o.deps:
                if not d.dma:
                    if d.eng == "pe" and o.eng == "pe" and not o.dma:
                        continue
                    d.signal = True
        cnt = {e: 0 for e in ENGS}
        for o in real:
            if o.dma:
                continue
            if o.signal:
                cnt[o.eng] += 1
            o.count = cnt[o.eng]
        stack = ExitStack()
        sems = {e: stack.enter_context(nc.semaphore("s_" + e)) for e in ENGS}
        dsem = {}
        dval = {}
        dn = {e: 0 for e in ENGS}
        for o in real:
            if not o.dma:
                continue
            q = o.eng
            j = dn[q] % NSLOT
            dn[q] += 1
            key = (q, j)
            if key not in dsem:
                dsem[key] = stack.enter_context(nc.semaphore("d_%s%d" % (q, j)))
                dval[key] = 0
            o.sem = dsem[key]
            o.prev_val = dval[key]
            dval[key] += 16
            o.val = dval[key]
        per_eng = {e: [o for o in real if o.eng == e] for e in ENGS}

        def run(e, eng):
            waited = {}

            def wait(sem, v):
                k = id(sem)
                if waited.get(k, 0) >= v:
                    return
                waited[k] = v
                eng.wait_ge(sem, v)

            for o in per_eng[e]:
                for d in o.deps:
                    if d.dma:
                        wait(d.sem, d.val)
                    else:
                        if d.eng == "pe" and e == "pe" and not o.dma:
                            continue
                        wait(sems[d.eng], d.count)
                if o.dma:
                    if o.prev_val:
                        wait(o.sem, o.prev_val)
                    ins = o.fn(eng)
                    ins.then_inc(o.sem, 16)
                else:
                    ins = o.fn(eng)
                    if o.signal:
                        ins.then_inc(sems[e], 1)
            if e == "sp":
                for key, s in dsem.items():
                    wait(s, dval[key])
                for e2 in ENGS:
                    if e2 != "sp" and cnt[e2]:
                        wait(sems[e2], cnt[e2])

        with nc.Block() as block:
            @block.tensor
            def _(eng):
                run("pe", eng)

            @block.scalar
            def _(eng):
                run("act", eng)

            @block.vector
            def _(eng):
                run("dve", eng)

            @block.gpsimd
            def _(eng):
                run("pool", eng)

            @block.sync
            def _(eng):
                run("sp", eng)
        stack.close()


DEPTH = 2
NB = 2
TL = 2048
TC = 256
TB = TL + TC
NT = NB * TB
NTILE = NT // 128
GT = 256
NGB = TB // GT
EPS = 1e-6
ULAT0 = 15
UCTX0 = 15 + TL + 15 + 15
UW = UCTX0 + TC + 15
PG = 384
NPG = NT // PG
NEG = -1.0e30


def na_tiles(j):
    res = []
    for m in range(16):
        pat = []
        anyv = False
        for a in range(2):
            row = []
            for b in range(2):
                r = 2 * j + b
                r0 = min(max(r - 4, 0), 24)
                kr = 2 * m + a
                v = (r0 <= kr <= r0 + 7)
                row.append(v)
                anyv = anyv or v
            pat.append(tuple(row))
        if anyv:
            res.append((m, tuple(pat)))
    return res


def na_combos():
    combos = []
    for j in range(16):
        for m, pat in na_tiles(j):
            key = (m - j, pat)
            if key not in combos:
                combos.append(key)
    return combos


class K:
    def __init__(self, nc, dbg=()):
        self.nc = nc
        self.P = Prog(nc)
        self.dbg = dbg
        self.D = {}
        self.nm = 0

    def din(self, name, shape, dt=F32):
        self.D[name] = self.nc.dram_tensor(name, list(shape), dt, kind="ExternalInput").ap()

    def sb(self, st, shape, dt, name=None):
        self.nm += 1
        return st.enter_context(self.nc.sbuf_tensor("%s_%d" % (name or "sb", self.nm), list(shape), dt))

    def ps(self, st, shape, dt, name=None):
        self.nm += 1
        return st.enter_context(self.nc.psum_tensor("%s_%d" % (name or "ps", self.nm), list(shape), dt))

    def tok(self, n="t"):
        return Tok(n)

    def mm(self, out, lhsT, rhs, start, stop, reads, w=(), pw=(), sgc=False):
        self.P.op("pe", lambda e: e.matmul(out, lhsT=lhsT, rhs=rhs, start=start, stop=stop, skip_group_check=sgc),
                  reads=reads, writes=w, pw=pw)

    def tr(self, out, in_, ident, reads, w=(), pw=()):
        self.P.op("pe", lambda e: e.transpose(out, in_, ident), reads=reads, writes=w, pw=pw)

    def act(self, out, in_, func, reads, w=(), pw=(), scale=None, bias=None):
        kw = {}
        if scale is not None:
            kw["scale"] = scale
        if bias is not None:
            kw["bias"] = bias
        self.P.op("act", lambda e: e.activation(out=out, in_=in_, func=func, **kw),
                  reads=reads, writes=w, pw=pw)

    def tt(self, eng, out, in0, in1, op, reads, w=(), pw=()):
        self.P.op(eng, lambda e: e.tensor_tensor(out=out, in0=in0, in1=in1, op=op),
                  reads=reads, writes=w, pw=pw)

    def ts(self, eng, out, in0, s1, op0, reads, w=(), pw=(), s2=None, op1=None):
        if op1 is None:
            self.P.op(eng, lambda e: e.tensor_scalar(out=out, in0=in0, scalar1=s1, scalar2=None, op0=op0),
                      reads=reads, writes=w, pw=pw)
        else:
            self.P.op(eng, lambda e: e.tensor_scalar(out=out, in0=in0, scalar1=s1, scalar2=s2, op0=op0, op1=op1),
                      reads=reads, writes=w, pw=pw)

    def stt(self, out, in0, scalar, in1, op0, op1, reads, w=(), pw=()):
        self.P.op("dve", lambda e: e.scalar_tensor_tensor(out=out, in0=in0, scalar=scalar, in1=in1, op0=op0, op1=op1),
                  reads=reads, writes=w, pw=pw)

    def cp(self, eng, out, in_, reads, w=(), pw=()):
        if eng == "act":
            self.P.op("act", lambda e: e.copy(out=out, in_=in_), reads=reads, writes=w, pw=pw)
        else:
            self.P.op(eng, lambda e: e.tensor_copy(out=out, in_=in_), reads=reads, writes=w, pw=pw)

    def recip(self, out, in_, reads, w=(), pw=()):
        self.P.op("dve", lambda e: e.reciprocal(out=out, in_=in_), reads=reads, writes=w, pw=pw)

    def memset(self, eng, ap, val, w=(), pw=(), reads=()):
        self.P.op(eng, lambda e: e.memset(ap, val), reads=reads, writes=w, pw=pw)

    def dma(self, q, out, in_, reads, w=(), pw=()):
        self.P.op(q, lambda e: e.dma_start(out=out, in_=in_), reads=reads, writes=w, pw=pw, dma=True)

    def declare(self):
        din = self.din
        din("x", [NB, TL, 1024]); din("ctx", [NB, TC, 1024]); din("ccT", [128, 8, 3])
        din("w_ada", [DEPTH, 1024, 6144]); din("b_adaT", [DEPTH, 128, 48])
        din("n1gT", [DEPTH, 128, 8]); din("n2gT", [DEPTH, 128, 8]); din("fngT", [128, 8])
        din("w_fm", [DEPTH, 1024, 2304]); din("w_v", [DEPTH, 1024, 512])
        din("gq2", [DEPTH, 128, 2]); din("gk2", [DEPTH, 128, 2])
        din("conv_wT", [DEPTH, 128, 2, 31]); din("conv_bT", [DEPTH, 128, 2])
        din("conv_lngT", [DEPTH, 128, 2]); din("conv_lnbT", [DEPTH, 128, 2])
        din("rb_exp", [DEPTH, 6, 15, 64, 64])
        din("w_out", [DEPTH, 1024, 1024]); din("w_q", [DEPTH, 1024, 2048])
        din("keysT", [DEPTH, 128, 16, 128]); din("uT", [DEPTH, 1024, 16384]); din("pv", [DEPTH, 16384, 1024])
        din("ropeC", [128, TB]); din("ropeS", [128, TB]); din("colmask2", [128, 128])
        din("blockmask", [128, 8]); din("blockones", [128, 128])
        nc = self.nc
        self.out = nc.dram_tensor("out", [NB, TL, 1024], F32, kind="ExternalOutput").ap()
        if self.dbg:
            self.xT_d = nc.dram_tensor("xT_d", [8, 128, NT], F32, kind="ExternalOutput").ap()
        else:
            self.xT_d = nc.dram_tensor("xT_d", [8, 128, NT], F32).ap()
        self.xc_d = nc.dram_tensor("xc_d", [8, 128, NT], BF16).ap()
        self.G_d = nc.dram_tensor("G_d", [16384, NT // 2], BF16).ap()
        self.ub_d = nc.dram_tensor("ub_d", [1024, 16384], BF16).ap()
        self.vb_d = nc.dram_tensor("vb_d", [16384, 1024], BF16).ap()
        self.xT_tok = [Tok("xT%d" % i) for i in range(NTILE)]
        self.xc_tok = [Tok("xc%d" % i) for i in range(NTILE)]
        self.G_tok = [Tok("G%d" % i) for i in range(NTILE)]
        self.dbg_out = {}

    def dbg_tensor(self, name, shape, dt=F32):
        ap = self.nc.dram_tensor(name, list(shape), dt, kind="ExternalOutput").ap()
        self.dbg_out[name] = ap
        return ap

    def consts(self, st):
        D = self.D
        c = {}
        self.c = c
        c["ident_f"] = self.sb(st, [128, 128], F32, "identf")
        c["ident_b"] = self.sb(st, [128, 128], BF16, "identb")
        c["ones_b"] = self.sb(st, [128, 128], BF16, "onesb")
        c["bones_b"] = self.sb(st, [128, 128], BF16, "bonesb")
        c["iota_f"] = self.sb(st, [128, 128], F32, "iotaf")
        c["iota_b"] = self.sb(st, [128, 128], BF16, "iotab")
        c["bmask_b"] = self.sb(st, [128, 8], BF16, "bmaskb")
        c["cmask_b"] = self.sb(st, [128, 128], BF16, "cmaskb")
        c["ropeC"] = self.sb(st, [128, TB], BF16, "ropeC")
        c["ropeS"] = self.sb(st, [128, TB], BF16, "ropeS")
        c["siluT"] = self.sb(st, [128, 8, 3], F32, "siluT")
        c["fng"] = self.sb(st, [128, 8], F32, "fng")
        c["modT"] = self.sb(st, [128, 48, 3], F32, "modT")
        c["A1"] = self.sb(st, [128, 8, 3], F32, "A1")
        c["A2"] = self.sb(st, [128, 8, 3], F32, "A2")
        c["eps"] = self.sb(st, [128, 1], F32, "eps")
        tmpf = self.sb(st, [128, 128], F32, "ctmp")
        tmp8 = self.sb(st, [128, 8], F32, "ctmp8")
        T = self.T = {k: Tok(k) for k in ["const", "modT", "A", "ctmp", "ctmp8", "silu"]}
        ct = [T["const"]]
        self.memset("pool", c["ident_f"][:], 0.0, w=[T["ctmp"]])
        self.P.op("pool", lambda e: e.affine_select(out=c["ident_f"][:], in_=c["ident_f"][:], pattern=[[-1, 128]],
                                                    compare_op=ALU.not_equal, fill=1.0, base=0, channel_multiplier=1),
                  reads=[T["ctmp"]], writes=[T["ctmp"]])
        self.cp("dve", c["ident_b"][:], c["ident_f"][:], reads=[T["ctmp"]], pw=ct)
        self.memset("dve", c["ones_b"][:], 1.0, pw=ct)
        self.memset("dve", c["eps"][:], EPS, pw=ct)
        tio = Tok("iota")
        self.P.op("pool", lambda e: e.iota(c["iota_f"][:], pattern=[[1, 128]], base=0, channel_multiplier=0,
                                           allow_small_or_imprecise_dtypes=True), writes=[tio])
        self.cp("dve", c["iota_b"][:], c["iota_f"][:], reads=[tio], pw=ct)
        self.dma("sp", tmpf[:], D["blockones"][:, :], reads=[], w=[T["ctmp8"]])
        self.cp("dve", c["bones_b"][:], tmpf[:], reads=[T["ctmp8"]], pw=ct)
        tmpf2 = self.sb(st, [128, 128], F32, "ctmp2")
        t2 = Tok("ctmp2")
        self.dma("sp", tmpf2[:], D["colmask2"][:, :], reads=[], w=[t2])
        self.cp("dve", c["cmask_b"][:], tmpf2[:], reads=[t2], pw=ct)
        t3 = Tok("ctmp3")
        self.dma("sp", tmp8[:], D["blockmask"][:, :], reads=[], w=[t3])
        self.cp("dve", c["bmask_b"][:], tmp8[:], reads=[t3], pw=ct)
        with ExitStack() as st2:
            rst = [self.sb(st2, [128, TB], F32, "ropest") for _ in range(2)]
            trst = [Tok("ropest") for _ in range(2)]
            for i, nm in enumerate(("ropeC", "ropeS")):
                self.dma("sp", rst[i][:], D[nm][:, :], reads=[], w=[trst[i]])
                self.cp("dve", c[nm][:], rst[i][:], reads=[trst[i]], pw=ct)
            self.P.barrier()
        self.dma("sp", c["fng"][:], D["fngT"][:, :], reads=[], pw=ct)
        t4 = Tok("cc")
        cct = self.sb(st, [128, 8, 3], F32, "cct")
        self.dma("sp", cct[:], D["ccT"][:, :, :], reads=[], w=[t4])
        self.act(c["siluT"][:], cct[:], AF.Silu, reads=[t4], w=[T["silu"]])

    def stage_s0(self):
        D, c, T = self.D, self.c, self.T
        with ExitStack() as st:
            xin = [self.sb(st, [128, 1024], F32, "xin") for _ in range(2)]
            xo = [self.sb(st, [128, 8, 128], F32, "xo") for _ in range(2)]
            pp = [self.ps(st, [128, 4, 128], F32, "s0p") for _ in range(4)]
            txin = [Tok("xin") for _ in range(2)]
            txo = [Tok("xo") for _ in range(2)]
            tpp = [Tok("pp") for _ in range(4)]
            n = 0
            for b in range(NB):
                for ti in range(TB // 128):
                    s = n % 2
                    if ti < 16:
                        src = D["x"][b, ti * 128:(ti + 1) * 128, :]
                    else:
                        src = D["ctx"][b, (ti - 16) * 128:(ti - 15) * 128, :]
                    self.dma("sp", xin[s][:], src, reads=[], w=[txin[s]])
                    for hf in range(2):
                        pi = (n * 2 + hf) % 4
                        for q in range(4):
                            k = hf * 4 + q
                            self.tr(pp[pi][:, q, :], xin[s][:, k * 128:(k + 1) * 128], c["ident_f"][:],
                                    reads=[txin[s], T["const"], T["ctmp"]],
                                    w=[tpp[pi]] if q == 0 else (), pw=() if q == 0 else [tpp[pi]])
                        self.cp("act" if hf == 0 else "dve", xo[s][:, hf * 4:(hf + 1) * 4, :], pp[pi][:],
                                reads=[tpp[pi]], w=([txo[s]] if hf == 0 else []) + [tpp[pi]], pw=() if hf == 0 else [txo[s]])
                    tile = b * (TB // 128) + ti
                    self.dma("pool", self.xT_d.rearrange("k p t -> p k t")[:, :, tile * 128:(tile + 1) * 128], xo[s][:],
                             reads=[txo[s]], w=[self.xT_tok[tile]])
                    n += 1
        self.P.barrier()

    def stage_s1(self, l):
        D, c, T = self.D, self.c, self.T
        with ExitStack() as st:
            wst = [self.sb(st, [128, 8, 512], F32, "wada") for _ in range(2)]
            twst = [Tok("wada") for _ in range(2)]
            bad = self.sb(st, [128, 48], F32, "bada")
            n1g = self.sb(st, [128, 8], F32, "n1g")
            n2g = self.sb(st, [128, 8], F32, "n2g")
            tsm = Tok("s1small")
            tmpa = self.sb(st, [128, 8, 3], F32, "tmpa")
            ttmp = Tok("tmpa")
            modp = self.ps(st, [128, 128, 4], F32, "modp")
            tmodp = Tok("modp")
            self.dma("pool", bad[:], D["b_adaT"][l], reads=[], pw=[tsm])
            self.dma("pool", n1g[:], D["n1gT"][l], reads=[], pw=[tsm])
            self.dma("pool", n2g[:], D["n2gT"][l], reads=[], pw=[tsm])
            wv = D["w_ada"][l].rearrange("(k p) c -> p k c", p=128)
            for jc in range(12):
                s = jc % 2
                self.dma("sp", wst[s][:], wv[:, :, jc * 512:(jc + 1) * 512], reads=[], w=[twst[s]])
                for q in range(4):
                    j = jc * 4 + q
                    for k in range(8):
                        self.mm(modp[:, j, 0:3], wst[s][:, k, q * 128:(q + 1) * 128], c["siluT"][:, k, :],
                                start=(k == 0), stop=(k == 7), reads=[twst[s], T["silu"]],
                                w=[tmodp] if (j == 0 and k == 0) else (), pw=() if (j == 0 and k == 0) else [tmodp])
            self.tt("dve", c["modT"][:], modp[:, 0:48, 0:3], bad[:].unsqueeze(2).to_broadcast([128, 48, 3]), ALU.add,
                    reads=[tmodp, tsm], w=[T["modT"], tmodp])
            for (dst, nrm, base) in ((c["A1"], n1g, 8), (c["A2"], n2g, 32)):
                self.ts("dve", tmpa[:], c["modT"][:, base:base + 8, :], 1.0, ALU.add, reads=[T["modT"]], w=[ttmp])
                self.tt("dve", dst[:], tmpa[:], nrm[:].unsqueeze(2).to_broadcast([128, 8, 3]), ALU.mult,
                        reads=[ttmp, tsm], pw=[T["A"]])
        self.P.barrier()

    def norm_mod(self, st_bufs, xg, txg, hT, thT, A, sh_base, r, n, pfx):
        c = self.c
        B = st_bufs
        self.act(B["sq"][:, :, 0:n], xg, AF.Square, reads=[txg], w=[B["tsq"]])
        for k in range(8):
            self.mm(B["ssp"][:, 0:n], c["ones_b"][:], B["sq"][:, k, 0:n], start=(k == 0), stop=(k == 7),
                    reads=[B["tsq"]], w=[B["tssp"]] if k == 0 else (), pw=() if k == 0 else [B["tssp"]])
        self.act(B["sd"][:, 0:n], B["ssp"][:, 0:n], AF.Sqrt, reads=[B["tssp"]], w=[B["tsd"], B["tssp"]],
                 scale=1.0 / 1024.0, bias=c["eps"][:])
        self.recip(B["rstd"][:, 0:n], B["sd"][:, 0:n], reads=[B["tsd"]], w=[B["trstd"]])
        self.tt("dve", B["tmp"][:, :, 0:n], xg, B["rstd"][:, 0:n].unsqueeze(1).to_broadcast([128, 8, n]), ALU.mult,
                reads=[txg, B["trstd"]], w=[B["ttmp"]])
        for k in range(8):
            self.act(hT[:, k, :], B["tmp"][:, k, 0:n], AF.Identity, reads=[B["ttmp"]],
                     w=[thT] if k == 0 else (), pw=() if k == 0 else [thT],
                     scale=A[:, k, r:r + 1], bias=c["modT"][:, sh_base + k, r:r + 1])

    def norm_bufs(self, st, n):
        B = {}
        B["sq"] = self.sb(st, [128, 8, n], BF16, "sq")
        B["ssp"] = self.ps(st, [128, 512], F32, "ssp")
        B["sd"] = self.sb(st, [128, n], F32, "sd")
        B["rstd"] = self.sb(st, [128, n], F32, "rstd")
        B["tmp"] = self.sb(st, [128, 8, n], F32, "ntmp")
        for k in ["tsq", "tssp", "tsd", "trstd", "ttmp"]:
            B[k] = Tok(k)
        return B

    def load_cast(self, st, dst, src_view, ncols, piece, engs=("act", "pool", "dve"), stg=None):
        if stg is None:
            stg = ([self.sb(st, [128, 8, piece], F32, "wstg") for _ in range(2)], [Tok("wstg") for _ in range(2)])
        stg, tst = stg
        tdst = Tok("wdst")
        npc = ncols // piece
        for i in range(npc):
            s = i % 2
            self.dma("sp", stg[s][:], src_view[:, :, i * piece:(i + 1) * piece], reads=[], w=[tst[s]])
            self.cp(engs[i % len(engs)], dst[:, :, i * piece:(i + 1) * piece], stg[s][:], reads=[tst[s]], pw=[tdst])
        return tdst

    def stage_s2(self, l, b, st_b, BB):
        D, c = self.D, self.c
        with ExitStack() as st:
            wfm = self.sb(st, [128, 8, 2304], BF16, "wfm")
            wv = self.sb(st, [128, 8, 512], BF16, "wv")
            stg = ([self.sb(st, [128, 8, 128], F32, "wstg") for _ in range(2)], [Tok("wstg") for _ in range(2)])
            twfm = self.load_cast(st, wfm, D["w_fm"][l].rearrange("(k p) c -> p k c", p=128), 2304, 128, stg=stg)
            twv = self.load_cast(st, wv, D["w_v"][l].rearrange("(k p) c -> p k c", p=128), 512, 128, stg=stg)
            gq2 = self.sb(st, [128, 2], F32, "gq2")
            gk2 = self.sb(st, [128, 2], F32, "gk2")
            gq2s = self.sb(st, [128, 2], F32, "gq2s")
            tg = Tok("g2")
            tgs = Tok("g2s")
            self.dma("pool", gq2[:], D["gq2"][l], reads=[], pw=[tg])
            self.dma("pool", gk2[:], D["gk2"][l], reads=[], pw=[tg])
            self.ts("dve", gq2s[:], gq2[:], 0.125, ALU.mult, reads=[tg], w=[tgs])
            NBUF = self.norm_bufs(st, GT)
            xg1 = self.sb(st, [128, 8, GT], F32, "xg")
            xg = [xg1, xg1]
            txg1 = Tok("xg")
            txg = [txg1, txg1]
            hT = [self.sb(st, [128, 8, GT], BF16, "hT") for _ in range(2)]
            thT = [Tok("hT") for _ in range(2)]
            zp = [self.ps(st, [128, 2, GT], F32, "zp") for _ in range(4)]
            tzp = [Tok("zp") for _ in range(4)]
            vtp = [self.ps(st, [128, 512], F32, "vtp") for _ in range(2)]
            tvtp = [Tok("vtp") for _ in range(2)]
            ssz = self.ps(st, [128, 2, GT], F32, "ssz")
            tssz = Tok("ssz")
            sg = [self.sb(st, [128, GT], F32, "sg") for _ in range(2)]
            tsg = [Tok("sg") for _ in range(2)]
            sqz = [self.sb(st, [128, GT], BF16, "sqz") for _ in range(2)]
            tsqz = [Tok("sqz") for _ in range(2)]
            sdz = [self.sb(st, [128, GT], F32, "sdz") for _ in range(2)]
            tsdz = [Tok("sdz") for _ in range(2)]
            rz = [self.sb(st, [128, GT], F32, "rz") for _ in range(2)]
            trz = [Tok("rz") for _ in range(2)]
            t1 = [self.sb(st, [128, GT], F32, "t1") for _ in range(2)]
            tt1 = [Tok("t1") for _ in range(2)]
            t2 = [self.sb(st, [128, GT], F32, "t2") for _ in range(2)]
            tt2 = [Tok("t2") for _ in range(2)]
            xTv = self.xT_d.rearrange("k p t -> p k t")
            zc = [0]

            def zpair(ciA, ciB, s, thTs):
                i = zc[0] % 4
                zc[0] += 1
                tk = tzp[i]
                for h2, ci in enumerate((ciA, ciB)):
                    for k in range(8):
                        first = (h2 == 0 and k == 0)
                        self.mm(zp[i][:, h2, :], wfm[:, k, ci * 128:(ci + 1) * 128], hT[s][:, k, :], start=(k == 0), stop=(k == 7),
                                reads=[twfm, thTs], w=[tk] if first else (), pw=() if first else [tk])
                return zp[i][:, 0, :], zp[i][:, 1, :], tk

            nrope = [0]
            for g in range(NGB):
                s = g % 2
                t0 = b * TB + g * GT
                r = b if g < 8 else 2
                pos0 = g * GT
                self.dma("sp", xg[s][:], xTv[:, :, t0:t0 + GT], reads=[], w=[txg[s]])
                self.norm_mod(NBUF, xg[s][:], txg[s], hT[s], thT[s], c["A1"], 0, r, GT, "s2")
                ucol0 = (ULAT0 + pos0) if g < 8 else UCTX0
                for cc in range(2):
                    zv, zg, tk = zpair(cc, 2 + cc, s, thT[s])
                    self.act(sg[cc][:], zg, AF.Sigmoid, reads=[tk], w=[tsg[cc], tk])
                    self.tt("dve", BB["ubuf"][:, cc, ucol0:ucol0 + GT], zv, sg[cc][:], ALU.mult,
                            reads=[tk, tsg[cc]], w=[tk], pw=[BB["tubuf"]])
                for cc in range(3):
                    zq, zk, tk = zpair(4 + cc, 10 + cc, s, thT[s])
                    self.act(BB["qna"][:, cc, pos0:pos0 + GT], zq, AF.Identity, reads=[tk], w=[tk], pw=[BB["tqna"]], scale=0.125)
                    self.cp("dve", BB["knaA"][0:64, cc, pos0:pos0 + GT], zk[0:64, :], reads=[tk], w=[tk], pw=[BB["tkna"]])
                    self.cp("dve", BB["knaB"][64:128, cc, pos0:pos0 + GT], zk[64:128, :], reads=[tk], w=[tk], pw=[BB["tkna"]])
                for cc in range(4):
                    if cc < 3:
                        ci, cis, gg = 7 + cc, 14 + cc, gq2s
                        dst, tdst = BB["qg"][:, cc, pos0:pos0 + GT], BB["tqg"]
                    else:
                        ci, cis, gg = 13, 17, gk2
                        dst, tdst = None, BB["tkg"]
                    i = nrope[0] % 2
                    nrope[0] += 1
                    z, zs, tk = zpair(ci, cis, s, thT[s])
                    self.act(sqz[i][:], z, AF.Square, reads=[tk], w=[tsqz[i], tk])
                    self.mm(ssz[:, i, :], c["bones_b"][:], sqz[i][:], start=True, stop=True, reads=[tsqz[i]], w=[tssz])
                    self.act(sdz[i][:], ssz[:, i, :], AF.Sqrt, reads=[tssz], w=[tsdz[i], tssz], scale=1.0 / 64.0, bias=c["eps"][:])
                    self.recip(rz[i][:], sdz[i][:], reads=[tsdz[i]], w=[trz[i]])
                    self.stt(t1[i][:], z, gg[:, 0:1], c["ropeC"][:, pos0:pos0 + GT], ALU.mult, ALU.mult,
                             reads=[tk, tg, tgs], w=[tt1[i], tk])
                    self.stt(t2[i][:], zs, gg[:, 1:2], c["ropeS"][:, pos0:pos0 + GT], ALU.mult, ALU.mult,
                             reads=[tk, tg, tgs], w=[tt2[i], tk])
                    self.tt("pool", t1[i][:], t1[i][:], t2[i][:], ALU.add, reads=[tt1[i], tt2[i]], w=[tt1[i]])
                    if dst is not None:
                        self.tt("dve", dst, t1[i][:], rz[i][:], ALU.mult, reads=[tt1[i], trz[i]], pw=[tdst])
                    else:
                        self.tt("dve", BB["kgA"][0:64, pos0:pos0 + GT], t1[i][0:64, :], rz[i][0:64, :], ALU.mult,
                                reads=[tt1[i], trz[i]], pw=[tdst])
                        self.tt("dve", BB["kgB"][64:128, pos0:pos0 + GT], t1[i][64:128, :], rz[i][64:128, :], ALU.mult,
                                reads=[tt1[i], trz[i]], pw=[tdst])
                for tt_ in range(2):
                    vi = (g * 2 + tt_) % 2
                    for k in range(8):
                        self.mm(vtp[vi][:], hT[s][:, k, tt_ * 128:(tt_ + 1) * 128], wv[:, k, :], start=(k == 0), stop=(k == 7),
                                reads=[thT[s], twv], w=[tvtp[vi]] if k == 0 else (), pw=() if k == 0 else [tvtp[vi]])
                    self.cp("act", BB["vv"][:, g * 2 + tt_, :, 0:64], vtp[vi][:].rearrange("p (h d) -> p h d", d=64),
                            reads=[tvtp[vi]], w=[tvtp[vi]], pw=[BB["tvv"]])
        self.P.barrier()

    def stage_s3(self, l, b, BB, do_ctx):
        D, c = self.D, self.c
        mixT, tmix = BB["mixT"], BB["tmix"]
        with ExitStack() as st:
            cw = self.sb(st, [128, 2, 31], F32, "cw")
            cb = self.sb(st, [128, 2], F32, "cb")
            lng = self.sb(st, [128, 2], F32, "lng")
            lnb = self.sb(st, [128, 2], F32, "lnb")
            tcs = Tok("convsmall")
            self.dma("pool", cw[:], D["conv_wT"][l], reads=[], pw=[tcs])
            self.dma("pool", cb[:], D["conv_bT"][l], reads=[], pw=[tcs])
            self.dma("pool", lng[:], D["conv_lngT"][l], reads=[], pw=[tcs])
            self.dma("pool", lnb[:], D["conv_lnbT"][l], reads=[], pw=[tcs])
            dg = self.sb(st, [128, 2, 31, 128], BF16, "dg")
            tdg = Tok("dg")
            n = 0
            for cc in range(2):
                for k in range(31):
                    self.ts("dve" if n % 2 == 0 else "pool", dg[:, cc, k, :], c["ident_b"][:], cw[:, cc, k:k + 1], ALU.mult,
                            reads=[tcs], pw=[tdg])
                    n += 1
            yp = [self.ps(st, [128, 2, GT], F32, "yp") for _ in range(2)]
            typ = [Tok("yp") for _ in range(2)]
            lsp = self.ps(st, [128, 2, GT], F32, "lsp")
            tlsp = Tok("lsp")
            ycb = [self.sb(st, [128, 2, GT], F32, "ycb") for _ in range(2)]
            tycb = [Tok("ycb") for _ in range(2)]
            ysq = [self.sb(st, [128, 2, GT], BF16, "ysq") for _ in range(2)]
            tysq = [Tok("ysq") for _ in range(2)]
            yb = [self.sb(st, [128, 2, GT], BF16, "yb") for _ in range(2)]
            tyb = [Tok("yb") for _ in range(2)]
            mu = self.sb(st, [128, GT], F32, "mu")
            musq = self.sb(st, [128, GT], F32, "musq")
            var = self.sb(st, [128, GT], F32, "var")
            rs = self.sb(st, [128, GT], F32, "rs")
            tmu, tmusq, tvar, trs = Tok("mu"), Tok("musq"), Tok("var"), Tok("rs")
            ct = [self.sb(st, [128, GT], F32, "ct") for _ in range(2)]
            tct = [Tok("ct") for _ in range(2)]
            import os
            parts = os.environ.get("S3PARTS", "cma")
            ngr = NGB if do_ctx else 8
            if "c" not in parts:
                ngr = 0
            for g in range(ngr):
                s = g % 2
                pos0 = g * GT
                ub0 = (pos0 if g < 8 else (UCTX0 - 15))
                for cc in range(2):
                    for k in range(31):
                        self.mm(yp[s][:, cc, :], dg[:, cc, k, :], BB["ubuf"][:, cc, ub0 + k:ub0 + k + GT],
                                start=(k == 0), stop=(k == 30), reads=[tdg, BB["tubuf"]],
                                w=[typ[s]] if (cc == 0 and k == 0) else (), pw=() if (cc == 0 and k == 0) else [typ[s]])
                for cc in range(2):
                    self.act(ycb[s][:, cc, :], yp[s][:, cc, :], AF.Identity, reads=[typ[s], tcs],
                             w=([tycb[s]] if cc == 0 else []) + [typ[s]], pw=() if cc == 0 else [tycb[s]], bias=cb[:, cc:cc + 1])
                    self.act(ysq[s][:, cc, :], yp[s][:, cc, :], AF.Square, reads=[typ[s], tcs],
                             w=([tysq[s]] if cc == 0 else []) + [typ[s]], pw=() if cc == 0 else [tysq[s]], bias=cb[:, cc:cc + 1])
                self.cp("pool", yb[s][:], ycb[s][:], reads=[tycb[s]], w=[tyb[s]])
                for cc in range(2):
                    self.mm(lsp[:, 0, :], c["ones_b"][:], yb[s][:, cc, :], start=(cc == 0), stop=(cc == 1),
                            reads=[tyb[s]], w=[tlsp] if cc == 0 else (), pw=() if cc == 0 else [tlsp])
                for cc in range(2):
                    self.mm(lsp[:, 1, :], c["ones_b"][:], ysq[s][:, cc, :], start=(cc == 0), stop=(cc == 1),
                            reads=[tysq[s]], pw=[tlsp])
                self.act(mu[:], lsp[:, 0, :], AF.Identity, reads=[tlsp], w=[tmu, tlsp], scale=1.0 / 256.0)
                self.tt("dve", musq[:], mu[:], mu[:], ALU.mult, reads=[tmu], w=[tmusq])
                self.stt(var[:], lsp[:, 1, :], 1.0 / 256.0, musq[:], ALU.mult, ALU.subtract, reads=[tlsp, tmusq], w=[tvar, tlsp])
                self.act(var[:], var[:], AF.Sqrt, reads=[tvar], w=[tvar], bias=c["eps"][:])
                self.recip(rs[:], var[:], reads=[tvar], w=[trs])
                for cc in range(2):
                    self.tt("dve", ct[cc][:], ycb[s][:, cc, :], mu[:], ALU.subtract, reads=[tycb[s], tmu], w=[tct[cc]])
                    self.tt("pool", ct[cc][:], ct[cc][:], rs[:], ALU.mult, reads=[tct[cc], trs], w=[tct[cc]])
                    self.act(mixT[:, cc, pos0:pos0 + GT], ct[cc][:], AF.Silu, reads=[tct[cc], tcs], pw=[tmix],
                             scale=lng[:, cc:cc + 1], bias=lnb[:, cc:cc + 1])

            combos = na_combos()
            NCB = len(combos)
            MT = self.sb(st, [128, NCB, 6, 128], BF16, "MT")
            tMT = Tok("MT")
            mst = [self.sb(st, [128, 6, 128], F32, "mst") for _ in range(2)]
            tmst = [Tok("mst") for _ in range(2)]
            for ci, (d, pat) in enumerate(combos):
                s = ci % 2
                self.memset("pool", mst[s][:], -30000.0, w=[tmst[s]])
                for a in range(2):
                    for bq in range(2):
                        if not pat[a][bq]:
                            continue
                        delta = 2 * d + a - bq
                        src = D["rb_exp"][l, :, delta + 7, :, :].rearrange("h c q -> c h q")
                        self.dma("sp", mst[s][a * 64:(a + 1) * 64, :, bq * 64:(bq + 1) * 64], src, reads=[], pw=[tmst[s]])
                self.act(MT[:, ci, :, :], mst[s][:], AF.Exp, reads=[tmst[s]], pw=[tMT])
            self.tt("dve", MT[:].rearrange("p c h q -> p (c h) q"), MT[:].rearrange("p c h q -> p (c h) q"),
                    c["cmask_b"][:].unsqueeze(1).to_broadcast([128, NCB * 6, 128]), ALU.mult, reads=[tMT], w=[tMT])

            sp_ = [self.ps(st, [128, 4, 128], F32, "sT") for _ in range(2)]
            tsp = [Tok("sT") for _ in range(2)]
            op_ = [self.ps(st, [128, 4, 128], F32, "oacc") for _ in range(2)]
            top_ = [Tok("oacc") for _ in range(2)]
            trp = self.ps(st, [128, 8, 128], BF16, "trp")
            ttrp = Tok("trp")
            pT = [self.sb(st, [128, 3, 128], BF16, "pT") for _ in range(3)]
            tpT = [Tok("pT") for _ in range(3)]
            rc = [self.sb(st, [128, 3], F32, "rc") for _ in range(2)]
            trc = [Tok("rc") for _ in range(2)]
            otm = [self.sb(st, [128, 768], BF16, "otm") for _ in range(2)]
            totm = [Tok("otm") for _ in range(2)]
            cnt = {"u": 0, "h": 0}

            att = os.environ.get("ATT", "qpnt")

            def attn_half(qt, heads, keylist, ot, tot_, ocol0):
                hi = cnt["h"] % 2
                cnt["h"] += 1
                nk = len(keylist)

                def qk(ki):
                    kaps, vaps, mask = keylist[ki]
                    u = cnt["u"]
                    cnt["u"] += 1
                    si, pi = u % 2, u % 3
                    for h3 in range(3):
                        self.mm(sp_[si][:, h3, :], kaps[h3], heads[h3], start=True, stop=True,
                                reads=[BB["tqna"], BB["tkna"], BB["tqg"], BB["tkg"]],
                                w=[tsp[si]] if h3 == 0 else (), pw=() if h3 == 0 else [tsp[si]])
                    self.act(pT[pi][:], sp_[si][:, 0:3, :], AF.Exp, reads=[tsp[si]], w=[tpT[pi], tsp[si]])
                    if mask is not None:
                        self.tt("dve", pT[pi][:], pT[pi][:], mask, ALU.mult, reads=[tpT[pi], tMT], w=[tpT[pi]])
                    return pi

                pend = qk(0)
                for ki in range(nk):
                    nxt = qk(ki + 1) if ki + 1 < nk else None
                    pi = pend
                    vaps = keylist[ki][1]
                    for h3 in range(3 if "p" in att else 0):
                        first = (ki == 0 and h3 == 0)
                        self.mm(op_[hi][:, h3, 0:65], pT[pi][:, h3, :], vaps[h3], start=first, stop=(ki == nk - 1), sgc=True,
                                reads=[tpT[pi], BB["tvv"]], w=[top_[hi]] if first else (), pw=() if first else [top_[hi]])
                    pend = nxt
                if "n" not in att:
                    return
                self.recip(rc[hi][:], op_[hi][:, 0:3, 64], reads=[top_[hi]], w=[trc[hi], top_[hi]])
                self.tt("dve", ot[:, ocol0:ocol0 + 192].rearrange("p (h d) -> p h d", d=64), op_[hi][:, 0:3, 0:64],
                        rc[hi][:].unsqueeze(2).to_broadcast([128, 3, 64]), ALU.mult, reads=[top_[hi], trc[hi]], w=[top_[hi]], pw=[tot_])

            vv, qna, qg = BB["vv"], BB["qna"], BB["qg"]
            knaAB = [BB["knaA"], BB["knaB"]]
            kgAB = [BB["kgA"], BB["kgB"]]
            nq = 18 if do_ctx else 16
            if "a" not in parts:
                nq = 0
            if "1" in parts:
                nq = 1
            for qt in range(nq):
                oi = qt % 2
                q0 = qt * 128
                if qt < 16:
                    loc = na_tiles(qt)
                    na_keys = [(m, combos.index((m - qt, pat))) for (m, pat) in loc] + [(16, None), (17, None)]
                    g_keys = list(range(18))
                else:
                    na_keys = [(16, None), (17, None)]
                    g_keys = [16, 17]
                for half in range(2):
                    hs = [3 * half + i for i in range(3)]
                    heads = [qna[:, h // 2, q0:q0 + 128] for h in hs]
                    kl = []
                    for (m, cidx) in na_keys:
                        kaps = [knaAB[h % 2][:, h // 2, m * 128:(m + 1) * 128] for h in hs]
                        vaps = [vv[:, m, h, :] for h in hs]
                        mask = None if cidx is None else MT[:, cidx, 3 * half:3 * half + 3, :]
                        kl.append((kaps, vaps, mask))
                    attn_half(qt, heads, kl, otm[oi], totm[oi], half * 192)
                for kvh in range(2):
                    heads = [qg[:, cc, q0:q0 + 128] for cc in range(3)]
                    kl = []
                    for m in g_keys:
                        kaps = [kgAB[kvh][:, m * 128:(m + 1) * 128]] * 3
                        vaps = [vv[:, m, 6 + kvh, :]] * 3
                        kl.append((kaps, vaps, None))
                    attn_half(qt, heads, kl, otm[oi], totm[oi], 384 + kvh * 192)
                if "t" not in att:
                    continue
                for j in range(6):
                    self.tr(trp[:, j, :], otm[oi][:, j * 128:(j + 1) * 128], c["ident_b"][:], reads=[totm[oi]],
                            w=[ttrp] if j == 0 else (), pw=() if j == 0 else [ttrp])
                self.cp("act", mixT[:, 2:8, q0:q0 + 128], trp[:, 0:6, :], reads=[ttrp], w=[ttrp], pw=[tmix])
        self.P.barrier()

    def stage_s4(self, l, b, BB, do_ctx):
        D, c = self.D, self.c
        with ExitStack() as st:
            wo = self.sb(st, [128, 8, 1024], BF16, "wo")
            two = self.load_cast(st, wo, D["w_out"][l].rearrange("(k p) c -> p k c", p=128), 1024, 256)
            xg = [self.sb(st, [128, 8, GT], F32, "xg4") for _ in range(2)]
            txg = [Tok("xg4") for _ in range(2)]
            opp = [self.ps(st, [128, 512], F32, "opp") for _ in range(4)]
            topp = [Tok("opp") for _ in range(4)]
            xTv = self.xT_d.rearrange("k p t -> p k t")
            ngr = NGB if do_ctx else 8
            n = 0
            for g in range(ngr):
                s = g % 2
                t0 = b * TB + g * GT
                pos0 = g * GT
                r = b if g < 8 else 2
                self.dma("sp", xg[s][:], xTv[:, :, t0:t0 + GT], reads=[], w=[txg[s]])
                for kd in range(8):
                    i = n % 4
                    n += 1
                    ap, tk = opp[i][:, 0:GT], topp[i]
                    for kf in range(8):
                        self.mm(ap, wo[:, kf, kd * 128:(kd + 1) * 128], BB["mixT"][:, kf, pos0:pos0 + GT],
                                start=(kf == 0), stop=(kf == 7), reads=[two, BB["tmix"]],
                                w=[tk] if kf == 0 else (), pw=() if kf == 0 else [tk])
                    self.stt(xg[s][:, kd, :], ap, c["modT"][:, 16 + kd, r:r + 1], xg[s][:, kd, :], ALU.mult, ALU.add,
                             reads=[tk, txg[s]], w=[tk], pw=[txg[s]])
                self.dma("pool", xTv[:, :, t0:t0 + GT], xg[s][:], reads=[txg[s]], w=[])
        self.P.barrier()

    def stage_p1(self, l, ntiles=NTILE, tile0=0, gbase=0):
        D, c = self.D, self.c
        with ExitStack() as st:
            wq = self.sb(st, [128, 8, 2048], BF16, "wq")
            twq = self.load_cast(st, wq, D["w_q"][l].rearrange("(k p) c -> p k c", p=128), 2048, 256)
            keysT = self.sb(st, [128, 16, 128], F32, "keysT")
            tkeys = Tok("keysT")
            self.dma("pool", keysT[:], D["keysT"][l], reads=[], w=[tkeys])
            NBUF = self.norm_bufs(st, 128)
            xt = [self.sb(st, [128, 8, 128], F32, "xt") for _ in range(2)]
            txt = [Tok("xt") for _ in range(2)]
            xc = [self.sb(st, [128, 8, 128], BF16, "xc") for _ in range(2)]
            txc = [Tok("xc") for _ in range(2)]
            qT = self.sb(st, [128, 16, 128], F32, "qT")
            tqT = Tok("qT")
            s_sb = self.sb(st, [128, 16, 128], F32, "s_sb")
            ts_sb = Tok("s_sb")
            tmpk = [self.sb(st, [128, 128], F32, "tmpk") for _ in range(2)]
            ttmpk = [Tok("tmpk") for _ in range(2)]
            top = self.sb(st, [128, 16, 16], F32, "top")
            ttop = Tok("top")
            idx = self.sb(st, [128, 16, 16], U32, "idx")
            tidx = Tok("idx")
            idxf = self.sb(st, [128, 2, 8, 16], F32, "idxf")
            tidxf = Tok("idxf")
            cs = self.sb(st, [128, 8, 256], F32, "cs")
            tcs = Tok("cs")
            tmp2 = [self.sb(st, [128, 256], F32, "tmp2") for _ in range(2)]
            ttmp2 = [Tok("tmp2") for _ in range(2)]
            m8 = self.sb(st, [128, 8, 16], F32, "m8")
            tm8 = Tok("m8")
            ee = self.sb(st, [128, 8, 256], F32, "ee")
            tee = Tok("ee")
            msk = self.sb(st, [128, 8, 256], F32, "msk")
            tmsk = Tok("msk")
            zz = self.sb(st, [128, 8], F32, "zz")
            tzz = Tok("zz")
            idxT = self.sb(st, [128, 2, 128], BF16, "idxT")
            tidxT = Tok("idxT")
            Tw = self.sb(st, [128, 16, 128], BF16, "Tw")
            tTw = Tok("Tw")
            G_t = self.sb(st, [128, 128, 128], BF16, "G_t")
            tG_t = Tok("G_t")
            A4 = [self.sb(st, [128, 4, 128], BF16, "A4") for _ in range(2)]
            tA4 = [Tok("A4") for _ in range(2)]
            B4 = [self.sb(st, [128, 4, 128], BF16, "B4") for _ in range(2)]
            tB4 = [Tok("B4") for _ in range(2)]
            W4 = [self.sb(st, [128, 4, 8, 16], BF16, "W4") for _ in range(2)]
            tW4 = [Tok("W4") for _ in range(2)]
            R4 = [self.sb(st, [128, 4, 128], BF16, "R4") for _ in range(2)]
            tR4 = [Tok("R4") for _ in range(2)]
            qsp = [self.ps(st, [128, 4, 128], F32, "qsp") for _ in range(2)]
            tqsp = [Tok("qsp") for _ in range(2)]
            trq, ttrq = qsp, tqsp
            Rp = [self.ps(st, [128, 4, 128], F32, "Rp") for _ in range(2)]
            tRp = [Tok("Rp") for _ in range(2)]
            Gp = [self.ps(st, [128, 4, 128], F32, "Gp") for _ in range(2)]
            tGp = [Tok("Gp") for _ in range(2)]
            xTv = self.xT_d.rearrange("k p t -> p k t")
            xcv = self.xc_d.rearrange("k p t -> p k t")
            Gv = self.G_d.rearrange("(a i) t -> i a t", i=128)
            nq = 0
            for ti in range(tile0, tile0 + ntiles):
                s = ti % 2
                t0 = ti * 128
                bb = ti // (TB // 128)
                r = bb if (ti % (TB // 128)) < 16 else 2
                self.dma("sp", xt[s][:], xTv[:, :, t0:t0 + 128], reads=[], w=[txt[s]])
                self.norm_mod(NBUF, xt[s][:], txt[s], xc[s], txc[s], c["A2"], 24, r, 128, "p1")
                self.dma("pool", xcv[:, :, t0:t0 + 128], xc[s][:], reads=[txc[s]], w=[])
                for bg in range(4):
                    i = nq % 2
                    nq += 1
                    for bl in range(4):
                        blk = bg * 4 + bl
                        for k in range(8):
                            first = (bl == 0 and k == 0)
                            self.mm(qsp[i][:, bl, :], wq[:, k, blk * 128:(blk + 1) * 128], xc[s][:, k, :],
                                    start=(k == 0), stop=(k == 7), reads=[twq, txc[s]],
                                    w=[tqsp[i]] if first else (), pw=() if first else [tqsp[i]])
                    self.cp("act", qT[:, bg * 4:(bg + 1) * 4, :], qsp[i][:], reads=[tqsp[i]],
                            w=([tqT] if bg == 0 else []) + [tqsp[i]], pw=() if bg == 0 else [tqT])
                for bg in range(4):
                    i = nq % 2
                    nq += 1
                    for bl in range(4):
                        blk = bg * 4 + bl
                        self.mm(qsp[i][:, bl, :], qT[:, blk, :], keysT[:, blk, :], start=True, stop=True,
                                reads=[tqT, tkeys], w=[tqsp[i]] if bl == 0 else (), pw=() if bl == 0 else [tqsp[i]])
                    self.cp("act", s_sb[:, bg * 4:(bg + 1) * 4, :], qsp[i][:], reads=[tqsp[i]],
                            w=([ts_sb] if bg == 0 else []) + [tqsp[i]], pw=() if bg == 0 else [ts_sb])
                for blk in range(16):
                    j = blk % 2
                    first = blk == 0
                    wtop = dict(w=[ttop]) if first else dict(pw=[ttop])
                    widx = dict(w=[tidx]) if first else dict(pw=[tidx])
                    self.P.op("dve", (lambda e, blk=blk: e.max(out=top[:, blk, 0:8], in_=s_sb[:, blk, :])),
                              reads=[ts_sb], writes=wtop.get("w", ()), pw=wtop.get("pw", ()))
                    self.P.op("dve", (lambda e, blk=blk: e.max_index(out=idx[:, blk, 0:8], in_max=top[:, blk, 0:8],
                                                                    in_values=s_sb[:, blk, :])),
                              reads=[ts_sb, ttop], writes=widx.get("w", ()), pw=widx.get("pw", ()))
                    self.P.op("dve", (lambda e, blk=blk, j=j: e.match_replace(out=tmpk[j][:], in_to_replace=top[:, blk, 0:8],
                                                                            in_values=s_sb[:, blk, :], imm_value=NEG)),
                              reads=[ts_sb, ttop], writes=[ttmpk[j]])
                    self.P.op("dve", (lambda e, blk=blk, j=j: e.max(out=top[:, blk, 8:16], in_=tmpk[j][:])),
                              reads=[ttmpk[j]], pw=[ttop])
                    self.P.op("dve", (lambda e, blk=blk, j=j: e.max_index(out=idx[:, blk, 8:16], in_max=top[:, blk, 8:16],
                                                                         in_values=tmpk[j][:])),
                              reads=[ttmpk[j], ttop], pw=[tidx])
                self.cp("dve", idxf[:].rearrange("p q h i -> p h q i"), idx[:].rearrange("p (h q) i -> p h q i", q=2),
                        reads=[tidx], w=[tidxf])
                top4 = top[:].rearrange("p (h q) i -> p h q i", q=2)
                self.tt("dve", cs[:].rearrange("p h (i j) -> p h i j", j=16),
                        top4[:, :, 0, :].unsqueeze(3).to_broadcast([128, 8, 16, 16]),
                        top4[:, :, 1, :].unsqueeze(2).to_broadcast([128, 8, 16, 16]), ALU.add, reads=[ttop], w=[tcs])
                for h in range(8):
                    j = h % 2
                    self.P.op("dve", (lambda e, h=h: e.max(out=m8[:, h, 0:8], in_=cs[:, h, :])), reads=[tcs],
                              writes=[tm8] if h == 0 else (), pw=() if h == 0 else [tm8])
                    self.P.op("dve", (lambda e, h=h, j=j: e.match_replace(out=tmp2[j][:], in_to_replace=m8[:, h, 0:8],
                                                                        in_values=cs[:, h, :], imm_value=NEG)),
                              reads=[tcs, tm8], writes=[ttmp2[j]])
                    self.P.op("dve", (lambda e, h=h, j=j: e.max(out=m8[:, h, 8:16], in_=tmp2[j][:])), reads=[ttmp2[j]], pw=[tm8])
                self.tt("dve", ee[:], cs[:], m8[:, :, 0:1].to_broadcast([128, 8, 256]), ALU.subtract, reads=[tcs, tm8], w=[tee])
                self.act(ee[:], ee[:], AF.Exp, reads=[tee], w=[tee])
                self.tt("dve", msk[:], cs[:], m8[:, :, 15:16].to_broadcast([128, 8, 256]), ALU.is_ge, reads=[tcs, tm8], w=[tmsk])
                self.tt("dve", ee[:], ee[:], msk[:], ALU.mult, reads=[tee, tmsk], w=[tee])
                self.P.op("dve", lambda e: e.tensor_reduce(out=zz[:], in_=ee[:], axis=AX.X, op=ALU.add), reads=[tee], writes=[tzz])
                self.recip(zz[:], zz[:], reads=[tzz], w=[tzz])
                Wc2 = msk[:].rearrange("p a b -> p (a b)").rearrange("p (i h j) -> p i h j", i=16, h=8)
                self.tt("dve", Wc2.rearrange("p i h j -> p h i j"), ee[:].rearrange("p h (i j) -> p h i j", j=16),
                        zz[:].unsqueeze(2).unsqueeze(3).to_broadcast([128, 8, 16, 16]), ALU.mult, reads=[tee, tzz], w=[tmsk])
                for q2 in range(2):
                    self.tr(trq[0][:, q2, :], idxf[:, q2, :, :].rearrange("p h i -> p (h i)"), c["ident_f"][:], reads=[tidxf],
                            w=[ttrq[0]] if q2 == 0 else (), pw=() if q2 == 0 else [ttrq[0]])
                self.cp("act", idxT[:], trq[0][:, 0:2, :], reads=[ttrq[0]], w=[tidxT, ttrq[0]])
                for ig in range(4):
                    pi = (ig + 1) % 2
                    for i4 in range(4):
                        i = ig * 4 + i4
                        self.tr(trq[pi][:, i4, :], Wc2[:, i, :, :].rearrange("p h j -> p (h j)"), c["ident_f"][:], reads=[tmsk],
                                w=[ttrq[pi]] if i4 == 0 else (), pw=() if i4 == 0 else [ttrq[pi]])
                    self.cp("act", Tw[:, ig * 4:(ig + 1) * 4, :], trq[pi][:], reads=[ttrq[pi]],
                            w=([tTw] if ig == 0 else []) + [ttrq[pi]], pw=() if ig == 0 else [tTw])
                for qd in range(32):
                    u = qd % 2
                    tq0 = qd * 4
                    self.tt("dve", A4[u][:], c["iota_b"][:].unsqueeze(1).to_broadcast([128, 4, 128]),
                            idxT[:, 0, tq0:tq0 + 4].unsqueeze(2).to_broadcast([128, 4, 128]), ALU.is_equal,
                            reads=[tidxT], w=[tA4[u]])
                    self.tt("dve", B4[u][:], c["iota_b"][:].unsqueeze(1).to_broadcast([128, 4, 128]),
                            idxT[:, 1, tq0:tq0 + 4].unsqueeze(2).to_broadcast([128, 4, 128]), ALU.is_equal,
                            reads=[tidxT], w=[tB4[u]])
                    self.tt("pool", W4[u][:], c["bmask_b"][:].unsqueeze(1).unsqueeze(3).to_broadcast([128, 4, 8, 16]),
                            Tw[:, :, tq0:tq0 + 4].rearrange("p i t -> p t i").unsqueeze(2).to_broadcast([128, 4, 8, 16]),
                            ALU.mult, reads=[tTw], w=[tW4[u]])
                    for t4 in range(4):
                        self.mm(Rp[u][:, t4, :], W4[u][:, t4, :, :].rearrange("p h i -> p (h i)"), B4[u][:, t4, :],
                                start=True, stop=True, reads=[tW4[u], tB4[u]],
                                w=[tRp[u]] if t4 == 0 else (), pw=() if t4 == 0 else [tRp[u]])
                    self.cp("act", R4[u][:], Rp[u][:], reads=[tRp[u]], w=[tR4[u], tRp[u]])
                    for t4 in range(4):
                        self.mm(Gp[u][:, t4, :], R4[u][:, t4, :], A4[u][:, t4, :], start=True, stop=True,
                                reads=[tR4[u], tA4[u]], w=[tGp[u]] if t4 == 0 else (), pw=() if t4 == 0 else [tGp[u]])
                    self.cp("act", G_t[:, :, tq0:tq0 + 4], Gp[u][:].rearrange("p t i -> p i t"),
                            reads=[tGp[u]], w=([tG_t] if qd == 0 else []) + [tGp[u]], pw=() if qd == 0 else [tG_t])
                for a8 in range(8):
                    self.dma("pool", Gv[:, a8 * 16:(a8 + 1) * 16, t0 - gbase:t0 - gbase + 128], G_t[:, a8 * 16:(a8 + 1) * 16, :],
                             reads=[tG_t], w=[])
        self.P.barrier()

    def stage_p0(self, l):
        D = self.D
        with ExitStack() as st:
            ust = [self.sb(st, [128, 8, 512], F32, "ust0") for _ in range(3)]
            tust = [Tok("ust0") for _ in range(3)]
            ubf = [self.sb(st, [128, 8, 512], BF16, "ubf0") for _ in range(3)]
            tubf = [Tok("ubf0") for _ in range(3)]
            uv = D["uT"][l].rearrange("(k p) e -> p k e", p=128)
            vview = D["pv"][l].rearrange("(a e) d -> e a d", e=128)
            ubv = self.ub_d.rearrange("(k p) e -> p k e", p=128)
            vbv = self.vb_d.rearrange("(a e) d -> e a d", e=128)
            engs = ("act", "dve", "act", "dve", "act", "pool")
            n = 0
            for pc in range(32):
                for which in range(2):
                    s = n % 3
                    if which == 0:
                        src = uv[:, :, pc * 512:(pc + 1) * 512]
                        dst = ubv[:, :, pc * 512:(pc + 1) * 512]
                        sview = ust[s][:]
                        bview = ubf[s][:]
                    else:
                        src = vview[:, pc * 4:(pc + 1) * 4, :]
                        dst = vbv[:, pc * 4:(pc + 1) * 4, :]
                        sview = ust[s][:].rearrange("p k e -> p (k e)").rearrange("p (a d) -> p a d", a=4)
                        bview = ubf[s][:].rearrange("p k e -> p (k e)").rearrange("p (a d) -> p a d", a=4)
                    self.dma("sp", sview, src, reads=[], w=[tust[s]])
                    self.cp(engs[n % 6], bview, sview, reads=[tust[s]], w=[tubf[s]])
                    self.dma("pool" if n % 2 == 0 else "act", dst, bview, reads=[tubf[s]], w=[])
                    n += 1
        self.P.barrier()

    def stage_p2(self, l, nsg=2, sg0=0, gbase=0):
        D, c = self.D, self.c
        NSUB = 3
        SG = NSUB * PG
        NTL = SG // 128
        with ExitStack() as st:
            ubf = [self.sb(st, [128, 8, 512], BF16, "ubf") for _ in range(3)]
            tubf = [Tok("ubf") for _ in range(3)]
            vbf = [self.sb(st, [128, 4, 1024], BF16, "vbf") for _ in range(3)]
            tvbf = [Tok("vbf") for _ in range(3)]
            gpc = [self.sb(st, [128, 4, SG], BF16, "gpc") for _ in range(3)]
            tgpc = [Tok("gpc") for _ in range(3)]
            xcg = self.sb(st, [128, 8, SG], BF16, "xcg")
            txcg = Tok("xcg")
            a_sb = [self.sb(st, [128, PG], BF16, "a_sb") for _ in range(2)]
            ta_sb = [Tok("a_sb") for _ in range(2)]
            ga = [self.sb(st, [128, PG], BF16, "ga") for _ in range(2)]
            tga = [Tok("ga") for _ in range(2)]
            acc = self.sb(st, [128, NTL, 1024], F32, "acc")
            tacc = [Tok("acc") for _ in range(NTL)]
            xt = [self.sb(st, [128, 8, 128], F32, "xt2") for _ in range(2)]
            txt = [Tok("xt2") for _ in range(2)]
            app = [self.ps(st, [128, 512], F32, "app") for _ in range(2)]
            tapp = [Tok("app") for _ in range(2)]
            oacc = [self.ps(st, [128, 1024], F32, "oacc2") for _ in range(3)]
            toacc = [Tok("oacc2") for _ in range(3)]
            uv = self.ub_d.rearrange("(k p) e -> p k e", p=128)
            vview = self.vb_d.rearrange("(a e) d -> e a d", e=128)
            Gv = self.G_d.rearrange("(a i) t -> i a t", i=128)
            xTv = self.xT_d.rearrange("k p t -> p k t")
            xcv = self.xc_d.rearrange("k p t -> p k t")
            NPC = 32
            self._npc = 0
            self._nch = 0
            nch = 0
            ntile = 0
            for sg in range(sg0, sg0 + nsg):
                tok0 = sg * SG
                self.dma("sp", xcg[:], xcv[:, :, tok0:tok0 + SG], reads=[], w=[txcg])
                steps = [(pc, sub, cc) for pc in range(NPC) for sub in range(NSUB) for cc in range(4)]
                pbuf = {}

                def u_phase(step):
                    pc, sub, cc = step
                    if sub == 0 and cc == 0:
                        s = self._npc % 3
                        self._npc += 1
                        pbuf[pc] = s
                        e0 = pc * 512
                        self.dma("sp", ubf[s][:], uv[:, :, e0:e0 + 512], reads=[], w=[tubf[s]])
                        self.dma("sp", vbf[s][:], vview[:, pc * 4:(pc + 1) * 4, :], reads=[], w=[tvbf[s]])
                        self.dma("sp", gpc[s][:], Gv[:, pc * 4:(pc + 1) * 4, tok0 - gbase:tok0 - gbase + SG], reads=[], w=[tgpc[s]])
                    s = pbuf[pc]
                    c0 = sub * PG
                    i = self._nch % 2
                    self._nch += 1
                    for k in range(8):
                        self.mm(app[i][:, 0:PG], ubf[s][:, k, cc * 128:(cc + 1) * 128], xcg[:, k, c0:c0 + PG],
                                start=(k == 0), stop=(k == 7), reads=[tubf[s], txcg],
                                w=[tapp[i]] if k == 0 else (), pw=() if k == 0 else [tapp[i]])
                    self.act(a_sb[i][:], app[i][:, 0:PG], AF.Gelu, reads=[tapp[i]], w=[ta_sb[i], tapp[i]])
                    self.tt("dve", ga[i][:], a_sb[i][:], gpc[s][:, cc, c0:c0 + PG], ALU.mult,
                            reads=[ta_sb[i], tgpc[s]], w=[tga[i]])
                    return i

                def v_phase(step, i):
                    pc, sub, cc = step
                    s = pbuf[pc]
                    for t3 in range(3):
                        for hf in range(2):
                            self.mm(oacc[t3][:, hf * 512:(hf + 1) * 512], ga[i][:, t3 * 128:(t3 + 1) * 128],
                                    vbf[s][:, cc, hf * 512:(hf + 1) * 512], start=(cc == 0), stop=(cc == 3),
                                    reads=[tga[i], tvbf[s]],
                                    w=[toacc[t3]] if (cc == 0 and hf == 0) else (),
                                    pw=() if (cc == 0 and hf == 0) else [toacc[t3]])
                    if cc == 3:
                        for t3 in range(3):
                            tl = sub * 3 + t3
                            if pc == 0:
                                self.cp("dve" if t3 != 1 else "act", acc[:, tl, :], oacc[t3][:], reads=[toacc[t3]],
                                        w=[tacc[tl], toacc[t3]])
                            else:
                                self.tt("dve", acc[:, tl, :], oacc[t3][:], acc[:, tl, :], ALU.add,
                                        reads=[toacc[t3], tacc[tl]], w=[tacc[tl], toacc[t3]])

                pend = u_phase(steps[0])
                for n_ in range(len(steps)):
                    nxt = u_phase(steps[n_ + 1]) if n_ + 1 < len(steps) else None
                    v_phase(steps[n_], pend)
                    pend = nxt
                nch = self._nch
                for tl in range(NTL):
                    ti = sg * NTL + tl
                    s2 = ntile % 2
                    ntile += 1
                    t0 = ti * 128
                    bb = ti // (TB // 128)
                    r = bb if (ti % (TB // 128)) < 16 else 2
                    self.dma("sp", xt[s2][:], xTv[:, :, t0:t0 + 128], reads=[], w=[txt[s2]])
                    for hf in range(2):
                        i = self._nch % 2
                        self._nch += 1
                        for q in range(4):
                            k = hf * 4 + q
                            self.tr(app[i][:, q * 128:(q + 1) * 128], acc[:, tl, k * 128:(k + 1) * 128], c["ident_f"][:],
                                    reads=[tacc[tl]], w=[tapp[i]] if q == 0 else (), pw=() if q == 0 else [tapp[i]])
                        for q in range(4):
                            k = hf * 4 + q
                            self.stt(xt[s2][:, k, :], app[i][:, q * 128:(q + 1) * 128], c["modT"][:, 40 + k, r:r + 1],
                                     xt[s2][:, k, :], ALU.mult, ALU.add, reads=[tapp[i], txt[s2]], w=[tapp[i]], pw=[txt[s2]])
                    self.dma("pool", xTv[:, :, t0:t0 + 128], xt[s2][:], reads=[txt[s2]], w=[])
        self.P.barrier()

    def stage_final(self):
        D, c = self.D, self.c
        with ExitStack() as st:
            xt = [self.sb(st, [128, 8, 128], F32, "xtf") for _ in range(2)]
            txt = [Tok("xtf") for _ in range(2)]
            sq = self.sb(st, [128, 8, 128], BF16, "sqf")
            tsq = Tok("sqf")
            ssp = self.ps(st, [128, 512], F32, "sspf")
            tssp = Tok("sspf")
            sd = self.sb(st, [128, 128], F32, "sdf")
            tsd = Tok("sdf")
            yt = [self.sb(st, [128, 8, 128], F32, "ytf") for _ in range(2)]
            tyt = [Tok("ytf") for _ in range(2)]
            trp = [self.ps(st, [128, 1024], F32, "trpf") for _ in range(2)]
            ttrp = [Tok("trpf") for _ in range(2)]
            yo = [self.sb(st, [128, 1024], F32, "yo") for _ in range(2)]
            tyo = [Tok("yo") for _ in range(2)]
            xTv = self.xT_d.rearrange("k p t -> p k t")
            n = 0
            for b in range(NB):
                for ti in range(16):
                    s = n % 2
                    n += 1
                    t0 = b * TB + ti * 128
                    self.dma("sp", xt[s][:], xTv[:, :, t0:t0 + 128], reads=[], w=[txt[s]])
                    self.act(sq[:], xt[s][:], AF.Square, reads=[txt[s]], w=[tsq])
                    for k in range(8):
                        self.mm(ssp[:, 0:128], c["ones_b"][:], sq[:, k, :], start=(k == 0), stop=(k == 7), reads=[tsq],
                                w=[tssp] if k == 0 else (), pw=() if k == 0 else [tssp])
                    self.act(sd[:], ssp[:, 0:128], AF.Sqrt, reads=[tssp], w=[tsd, tssp], scale=1.0 / 1024.0, bias=c["eps"][:])
                    self.recip(sd[:], sd[:], reads=[tsd], w=[tsd])
                    self.tt("dve", yt[s][:], xt[s][:], sd[:].unsqueeze(1).to_broadcast([128, 8, 128]), ALU.mult,
                            reads=[txt[s], tsd], w=[tyt[s]])
                    self.tt("pool", yt[s][:], yt[s][:], c["fng"][:].unsqueeze(2).to_broadcast([128, 8, 128]), ALU.mult,
                            reads=[tyt[s]], w=[tyt[s]])
                    for k in range(8):
                        self.tr(trp[s][:, k * 128:(k + 1) * 128], yt[s][:, k, :], c["ident_f"][:], reads=[tyt[s]],
                                w=[ttrp[s]] if k == 0 else (), pw=() if k == 0 else [ttrp[s]])
                    self.cp("act", yo[s][:], trp[s][:], reads=[ttrp[s]], w=[tyo[s], ttrp[s]])
                    self.dma("pool", self.out[b, ti * 128:(ti + 1) * 128, :], yo[s][:], reads=[tyo[s]], w=[])

    def batch_bufs(self, st):
        BB = {}
        BB["qna"] = self.sb(st, [128, 3, TB], BF16, "qna")
        BB["knaA"] = self.sb(st, [128, 3, TB], BF16, "knaA")
        BB["knaB"] = self.sb(st, [128, 3, TB], BF16, "knaB")
        BB["qg"] = self.sb(st, [128, 3, TB], BF16, "qg")
        BB["kgA"] = self.sb(st, [128, TB], BF16, "kgA")
        BB["kgB"] = self.sb(st, [128, TB], BF16, "kgB")
        BB["vv"] = self.sb(st, [128, TB // 128, 8, 65], BF16, "vv")
        BB["ubuf"] = self.sb(st, [128, 2, UW], BF16, "ubuf")
        for k in ["tqna", "tkna", "tqg", "tkg", "tvv", "tubuf", "tmix"]:
            BB[k] = Tok(k)
        self.memset("pool", BB["ubuf"][:], 0.0, w=[BB["tubuf"]])
        self.memset("pool", BB["knaA"][64:128, :, :], 0.0, w=[BB["tkna"]])
        self.memset("dve", BB["knaB"][0:64, :, :], 0.0, pw=[BB["tkna"]])
        self.memset("pool", BB["kgA"][64:128, :], 0.0, w=[BB["tkg"]])
        self.memset("dve", BB["kgB"][0:64, :], 0.0, pw=[BB["tkg"]])
        self.memset("pool", BB["vv"][:, :, :, 64:65], 1.0, w=[BB["tvv"]])
        return BB

    def build(self, depth=DEPTH, stages="all"):
        self.declare()
        with ExitStack() as st0:
            self.consts(st0)
            self.P.barrier()
            self._build_body(depth, stages)
            self.P.emit()
        return self.nc

    def _build_body(self, depth, stages):
        if stages == "c":
            return
        self.stage_s0()
        if stages == "s0":
            return
        for l in range(depth):
            do_ctx = l < DEPTH - 1
            self.stage_s1(l)
            if stages == "s1":
                return
            if stages == "pp":
                self.stage_p1(l, ntiles=3)
                self.stage_p0(l)
                self.stage_p2(l, nsg=1)
                return
            for b in range(NB):
                with ExitStack() as stb:
                    BB = self.batch_bufs(stb)
                    self.stage_s2(l, b, stb, BB)
                    if stages == "s2":
                        return
                    with ExitStack() as stm:
                        BB["mixT"] = self.sb(stm, [128, 8, TB], BF16, "mixT")
                        self.stage_s3(l, b, BB, do_ctx)
                        if stages == "s3":
                            return
                        self.stage_s4(l, b, BB, do_ctx)
            if stages == "mix":
                return
            self.stage_p0(l)
            for hb in range(2):
                self.stage_p1(l, ntiles=NTILE // 2, tile0=hb * (NTILE // 2), gbase=hb * (NT // 2))
                self.stage_p2(l, nsg=2, sg0=hb * 2, gbase=hb * (NT // 2))
            if stages == "l0":
                return
        self.stage_final()


OFF_A_VAL, OFF_A_GATE, OFF_NA_Q, OFF_G_Q = 0, 256, 512, 896
OFF_NA_K, OFF_NA_V, OFF_G_K, OFF_G_V = 1280, 1664, 2048, 2176


def _rope_tables():
    t = np.arange(TL, dtype=np.int32)
    row = (t // 64).astype(np.float32)
    col = (t % 64).astype(np.float32)
    n_freq = 16
    inv_freq = (np.float32(10000.0) ** (-np.arange(n_freq, dtype=np.float32) / np.float32(n_freq))).astype(np.float32)
    ang = np.concatenate([row[:, None] * inv_freq, col[:, None] * inv_freq], axis=-1).astype(np.float32)
    cos = np.cos(ang).astype(np.float32)
    sin = np.sin(ang).astype(np.float32)
    C = np.ones((128, TB), np.float32)
    S = np.zeros((128, TB), np.float32)
    for p in range(128):
        d = p % 64
        if d < 32:
            C[p, :TL] = cos[:, d]
            S[p, :TL] = -sin[:, d]
        else:
            C[p, :TL] = cos[:, d - 32]
            S[p, :TL] = sin[:, d - 32]
    return C, S


def _consts():
    C, S = _rope_tables()
    cols = np.arange(64)
    c0 = np.clip(cols - 8, 0, 48)
    cm = np.zeros((64, 64), np.float32)
    for c in range(64):
        cm[c0[c]:c0[c] + 16, c] = 1.0
    colmask2 = np.tile(cm, (2, 2)).astype(np.float32)
    blockmask = np.zeros((128, 8), np.float32)
    for p in range(128):
        blockmask[p, p // 16] = 1.0
    blockones = np.zeros((128, 128), np.float32)
    blockones[:64, :64] = 1.0
    blockones[64:, 64:] = 1.0
    return dict(ropeC=C, ropeS=S, colmask2=colmask2, blockmask=blockmask, blockones=blockones)


def _prep_shared(inp):
    f = np.float32
    L = DEPTH
    sh = {}
    sh["w_ada"] = np.ascontiguousarray(inp["w_ada"], dtype=f)
    sh["b_adaT"] = np.ascontiguousarray(inp["b_ada"].reshape(L, 48, 128).transpose(0, 2, 1))
    sh["n1gT"] = np.ascontiguousarray(inp["norm1_g"].reshape(L, 8, 128).transpose(0, 2, 1))
    sh["n2gT"] = np.ascontiguousarray(inp["norm2_g"].reshape(L, 8, 128).transpose(0, 2, 1))
    sh["fngT"] = np.ascontiguousarray(inp["final_norm_g"].reshape(8, 128).T)
    w_in = inp["w_in"]
    hperm = [0, 3, 1, 4, 2, 5]
    gq_cols = np.concatenate([OFF_G_Q + h * 64 + np.arange(64) for h in hperm])
    swp = np.concatenate([np.arange(32, 64), np.arange(0, 32)])
    gq_sw = np.concatenate([OFF_G_Q + h * 64 + swp for h in hperm])
    gk_cols = OFF_G_K + np.arange(128)
    gk_sw = np.concatenate([OFF_G_K + h * 64 + swp for h in range(2)])
    fm_cols = np.concatenate([np.arange(OFF_A_VAL, OFF_A_VAL + 256), np.arange(OFF_A_GATE, OFF_A_GATE + 256),
                              np.arange(OFF_NA_Q, OFF_NA_Q + 384), gq_cols, np.arange(OFF_NA_K, OFF_NA_K + 384),
                              gk_cols, gq_sw, gk_sw])
    assert fm_cols.shape[0] == 2304
    sh["w_fm"] = np.ascontiguousarray(w_in[:, :, fm_cols])
    v_cols = np.concatenate([np.arange(OFF_NA_V, OFF_NA_V + 384), np.arange(OFF_G_V, OFF_G_V + 128)])
    sh["w_v"] = np.ascontiguousarray(w_in[:, :, v_cols])
    p = np.arange(128)
    d = p % 64
    dsw = (d + 32) % 64
    sh["gq2"] = np.ascontiguousarray(np.stack([inp["gqa_q_norm"][:, d], inp["gqa_q_norm"][:, dsw]], axis=-1))
    sh["gk2"] = np.ascontiguousarray(np.stack([inp["gqa_k_norm"][:, d], inp["gqa_k_norm"][:, dsw]], axis=-1))
    sh["conv_wT"] = np.ascontiguousarray(inp["conv_w"].reshape(L, 31, 2, 128).transpose(0, 3, 2, 1))
    for k_src, k_dst in (("conv_b", "conv_bT"), ("conv_ln_g", "conv_lngT"), ("conv_ln_b", "conv_lnbT")):
        sh[k_dst] = np.ascontiguousarray(inp[k_src].reshape(L, 2, 128).transpose(0, 2, 1))
    cp_ = np.arange(64)[:, None]
    c_ = np.arange(64)[None, :]
    jidx = np.clip(cp_ - c_ + 15, 0, 30)
    sh["rb_exp"] = np.ascontiguousarray(inp["na_rel_bias"][:, :, :, jidx])
    sh["w_out"] = np.ascontiguousarray(inp["w_out"], dtype=f)
    sh["w_q"] = np.ascontiguousarray(inp["peer_w_q"], dtype=f)
    sh["keysT"] = np.ascontiguousarray(inp["peer_keys"].reshape(L, 16, 128, 128).transpose(0, 3, 1, 2))
    sh["uT"] = np.ascontiguousarray(inp["peer_u"].transpose(0, 2, 1))
    sh["pv"] = np.ascontiguousarray(inp["peer_v"], dtype=f)
    sh.update(_consts())
    return sh


def _prep_core(inp, core):
    b0 = core * NB
    cc = np.concatenate([inp["c"][b0:b0 + NB], inp["c_ctx"][None, :]], axis=0)
    m = {}
    m["x"] = np.ascontiguousarray(inp["x"][b0:b0 + NB])
    m["ctx"] = np.ascontiguousarray(inp["ctx"][b0:b0 + NB])
    m["ccT"] = np.ascontiguousarray(cc.reshape(3, 8, 128).transpose(2, 1, 0))
    return m


_NC_CACHE = {}


def kernel(**inputs):
    inp = {k: np.asarray(v, dtype=np.float32) for k, v in inputs.items()}
    n_cores = inp["x"].shape[0] // NB
    if "nc" not in _NC_CACHE:
        nc = bass.Bass("TRN2", target_bir_lowering=False)
        kk = K(nc)
        kk.build()
        _NC_CACHE["nc"] = nc
    nc = _NC_CACHE["nc"]
    sh = _prep_shared(inp)
    in_maps = []
    for core in range(n_cores):
        m = dict(sh)
        m.update(_prep_core(inp, core))
        in_maps.append(m)
    res = run_bass_kernel_spmd(nc, in_maps, core_ids=list(range(n_cores)))
    outs = [np.asarray(r["out"]) for r in res.results]
    return np.concatenate(outs, axis=0).astype(np.float32)
```
